# Optimizing a Trainium2 kernel written in Bass

```python
import math
import jax, jax.numpy as jnp
from jax import lax
import numpy as np

D_MODEL = 2048
BATCH = 4
SEQ = 4096
DEPTH = 2

N_MIXERS = 2
N_A_LAYERS = (DEPTH + 1) // 2
N_B_LAYERS = DEPTH // 2
BLOCK_Q = 128
ROPE_THETA = 10000.0

DA_HEAD_DIM = 128
DA_HEADS = D_MODEL // (2 * DA_HEAD_DIM)
DA_V_DIM = 2 * DA_HEAD_DIM
DA_QK_WIDTH = DA_HEADS * 2 * DA_HEAD_DIM
DA_V_WIDTH = DA_HEADS * DA_V_DIM
DA_IN = 2 * DA_QK_WIDTH + DA_V_WIDTH

FX_HEAD_DIM = 128
FX_HEADS = D_MODEL // FX_HEAD_DIM
FX_WIDTH = FX_HEADS * FX_HEAD_DIM
FX_IN = 3 * FX_WIDTH + FX_HEADS
FORGET_BIAS_INIT = 3.0

N_GROUPS = 4
EXPERTS_PER_GROUP = 8
EXPERT_DFF = D_MODEL // 4
TOP_K_INNER = 2

LN_EPS = 1e-5
RMS_EPS = 1e-5
DEEPNORM_ALPHA = (2.0 * DEPTH) ** 0.25
DEEPNORM_BETA = (8.0 * DEPTH) ** -0.25

kernel_name = "hybrid_diffattn_fox_hmoe_deepnorm_adaln"


def layer_norm(x, g, b):
    x32 = x.astype(jnp.float32)
    mu = jnp.mean(x32, axis=-1, keepdims=True)
    var = jnp.mean(jnp.square(x32 - mu), axis=-1, keepdims=True)
    y = (x32 - mu) * lax.rsqrt(var + LN_EPS)
    return (y * g.astype(jnp.float32) + b.astype(jnp.float32)).astype(x.dtype)


def rms_norm(x, g):
    x32 = x.astype(jnp.float32)
    y = x32 * lax.rsqrt(jnp.mean(jnp.square(x32), axis=-1, keepdims=True) + RMS_EPS)
    return (y * g.astype(jnp.float32)).astype(x.dtype)


def adaln_modulation(c, w_mod, b_mod):
    m = jax.nn.silu(c) @ w_mod + b_mod
    shift, scale, g = jnp.split(m, 3, axis=-1)
    return shift[:, None, :], scale[:, None, :], 1.0 + g[:, None, :]


def apply_rope(x, positions):
    d = x.shape[-1]
    inv_freq = jnp.power(ROPE_THETA, -jnp.arange(0, d, 2, dtype=jnp.float32) / d)
    ang = positions.astype(jnp.float32)[:, None] * inv_freq[None, :]
    cos = jnp.cos(ang)[:, None, :]
    sin = jnp.sin(ang)[:, None, :]
    x32 = x.astype(jnp.float32)
    x1, x2 = jnp.split(x32, 2, axis=-1)
    out = jnp.concatenate([x1 * cos - x2 * sin, x2 * cos + x1 * sin], axis=-1)
    return out.astype(x.dtype)


def causal_mask(i, length):
    q_pos = i * BLOCK_Q + jnp.arange(BLOCK_Q)
    k_pos = jnp.arange(length)
    return q_pos[:, None] >= k_pos[None, :]


def diff_lambda_init(layer_idx):
    return 0.8 - 0.6 * math.exp(-0.3 * (layer_idx - 1))


def differential_attention(h, positions, w_in, lam_q1, lam_k1, lam_q2, lam_k2, subln_w, w_out, lam_init):
    B, S, _ = h.shape
    proj = h @ w_in
    q, k, v = jnp.split(proj, [DA_QK_WIDTH, 2 * DA_QK_WIDTH], axis=-1)
    q = apply_rope(q.reshape(B, S, 2 * DA_HEADS, DA_HEAD_DIM), positions)
    k = apply_rope(k.reshape(B, S, 2 * DA_HEADS, DA_HEAD_DIM), positions)
    q = q.reshape(B, S, DA_HEADS, 2, DA_HEAD_DIM).transpose(0, 2, 3, 1, 4)
    k = k.reshape(B, S, DA_HEADS, 2, DA_HEAD_DIM).transpose(0, 2, 3, 1, 4)
    v = v.reshape(B, S, DA_HEADS, DA_V_DIM).transpose(0, 2, 1, 3)
    f32 = jnp.float32
    lam = (jnp.exp(jnp.sum(lam_q1.astype(f32) * lam_k1.astype(f32)))
           - jnp.exp(jnp.sum(lam_q2.astype(f32) * lam_k2.astype(f32))) + lam_init)
    scale = DA_HEAD_DIM ** -0.5
    outs = []
    for i in range(S // BLOCK_Q):
        L = (i + 1) * BLOCK_Q
        q_blk = q[:, :, :, i * BLOCK_Q:L]
        s = jnp.einsum('bhcqd,bhckd->bhcqk', q_blk, k[:, :, :, :L]).astype(f32) * scale
        s = jnp.where(causal_mask(i, L), s, -jnp.inf)
        p = jax.nn.softmax(s, axis=-1)
        a = p[:, :, 0] - lam * p[:, :, 1]
        outs.append(jnp.einsum('bhqk,bhkd->bhqd', a.astype(v.dtype), v[:, :, :L]))
    o = jnp.concatenate(outs, axis=2)
    o = rms_norm(o, subln_w) * (1.0 - lam_init)
    o = o.transpose(0, 2, 1, 3).reshape(B, S, DA_V_WIDTH)
    return o @ w_out


def forgetting_attention(h, w_in, forget_bias, w_out):
    B, S, _ = h.shape
    proj = h @ w_in
    q, k, v, f_logit = jnp.split(proj, [FX_WIDTH, 2 * FX_WIDTH, 3 * FX_WIDTH], axis=-1)
    q = q.reshape(B, S, FX_HEADS, FX_HEAD_DIM).transpose(0, 2, 1, 3)
    k = k.reshape(B, S, FX_HEADS, FX_HEAD_DIM).transpose(0, 2, 1, 3)
    v = v.reshape(B, S, FX_HEADS, FX_HEAD_DIM).transpose(0, 2, 1, 3)
    f32 = jnp.float32
    log_f = jax.nn.log_sigmoid(f_logit.astype(f32) + forget_bias.astype(f32))
    cum = jnp.cumsum(log_f, axis=1).transpose(0, 2, 1)
    scale = FX_HEAD_DIM ** -0.5
    outs = []
    for i in range(S // BLOCK_Q):
        L = (i + 1) * BLOCK_Q
        q_blk = q[:, :, i * BLOCK_Q:L]
        s = jnp.einsum('bhqd,bhkd->bhqk', q_blk, k[:, :, :L]).astype(f32) * scale
        s = s + (cum[:, :, i * BLOCK_Q:L, None] - cum[:, :, None, :L])
        s = jnp.where(causal_mask(i, L), s, -jnp.inf)
        p = jax.nn.softmax(s, axis=-1)
        outs.append(jnp.einsum('bhqk,bhkd->bhqd', p.astype(v.dtype), v[:, :, :L]))
    o = jnp.concatenate(outs, axis=2)
    o = o.transpose(0, 2, 1, 3).reshape(B, S, FX_WIDTH)
    return o @ w_out


def hierarchical_moe(h, w_group, b_group, w_router, b_router, w_gate, w_up, w_down):
    B, S, D = h.shape
    t = h.reshape(B * S, D)
    f32 = jnp.float32
    g_logits = (t @ w_group + b_group).astype(f32)
    g_probs = jax.nn.softmax(g_logits, axis=-1)
    g_idx = jnp.argmax(g_logits, axis=-1)
    g_w = jnp.take_along_axis(g_probs, g_idx[:, None], axis=1)[:, 0]
    e_all = (jnp.einsum('nd,gde->nge', t, w_router) + b_router).astype(f32)
    e_logits = jnp.take_along_axis(e_all, g_idx[:, None, None], axis=1)[:, 0]
    top_v, top_i = lax.top_k(e_logits, TOP_K_INNER)
    top_p = jax.nn.softmax(top_v, axis=-1)
    combine = jnp.sum(jax.nn.one_hot(top_i, EXPERTS_PER_GROUP, dtype=f32) * top_p[..., None], axis=1)
    weights = (g_w[:, None] * combine).astype(t.dtype)
    out = jnp.zeros_like(t)
    for g in range(N_GROUPS):
        wg = jnp.where((g_idx == g)[:, None], weights, jnp.zeros_like(weights))
        a = jnp.einsum('nd,edf->nef', t, w_gate[g])
        u = jnp.einsum('nd,edf->nef', t, w_up[g])
        hid = jax.nn.silu(a) * u * wg[:, :, None]
        out = out + jnp.einsum('nef,efd->nd', hid, w_down[g])
    return out.reshape(B, S, D)


def setup_inputs(seed: int = 0) -> dict:
    key = jax.random.key(seed)
    ks = jax.random.split(key, 32)
    nrm = jax.random.normal
    D = D_MODEL
    s_d = D ** -0.5
    x = nrm(ks[0], (BATCH, SEQ, D), jnp.float32)
    c = nrm(ks[1], (BATCH, D), jnp.float32)
    mix_mod_w = nrm(ks[2], (DEPTH, D, 3 * D)) * s_d * 0.1
    mix_mod_b = nrm(ks[3], (DEPTH, 3 * D)) * 0.02
    mix_ln_g = 1.0 + 0.02 * nrm(ks[4], (DEPTH, D))
    mix_ln_b = 0.02 * nrm(ks[5], (DEPTH, D))
    a_w_qk = nrm(ks[6], (N_A_LAYERS, D, 2 * DA_QK_WIDTH)) * s_d
    a_w_v = nrm(ks[7], (N_A_LAYERS, D, DA_V_WIDTH)) * s_d * DEEPNORM_BETA
    a_w_in = jnp.concatenate([a_w_qk, a_w_v], axis=-1)
    a_lam_q1 = 0.1 * nrm(ks[8], (N_A_LAYERS, DA_HEAD_DIM))
    a_lam_k1 = 0.1 * nrm(ks[9], (N_A_LAYERS, DA_HEAD_DIM))
    a_lam_q2 = 0.1 * nrm(ks[10], (N_A_LAYERS, DA_HEAD_DIM))
    a_lam_k2 = 0.1 * nrm(ks[11], (N_A_LAYERS, DA_HEAD_DIM))
    a_subln_w = 1.0 + 0.02 * nrm(ks[12], (N_A_LAYERS, DA_V_DIM))
    a_w_out = nrm(ks[13], (N_A_LAYERS, DA_V_WIDTH, D)) * DA_V_WIDTH ** -0.5 * DEEPNORM_BETA
    b_w_qk = nrm(ks[14], (N_B_LAYERS, D, 2 * FX_WIDTH)) * s_d
    b_w_v = nrm(ks[15], (N_B_LAYERS, D, FX_WIDTH)) * s_d * DEEPNORM_BETA
    b_w_f = nrm(ks[16], (N_B_LAYERS, D, FX_HEADS)) * s_d * 0.5
    b_w_in = jnp.concatenate([b_w_qk, b_w_v, b_w_f], axis=-1)
    b_forget_bias = FORGET_BIAS_INIT + 0.5 * nrm(ks[17], (N_B_LAYERS, FX_HEADS))
    b_w_out = nrm(ks[18], (N_B_LAYERS, FX_WIDTH, D)) * FX_WIDTH ** -0.5 * DEEPNORM_BETA
    ffn_mod_w = nrm(ks[19], (DEPTH, D, 3 * D)) * s_d * 0.1
    ffn_mod_b = nrm(ks[20], (DEPTH, 3 * D)) * 0.02
    ffn_ln_g = 1.0 + 0.02 * nrm(ks[21], (DEPTH, D))
    ffn_ln_b = 0.02 * nrm(ks[22], (DEPTH, D))
    moe_w_group = nrm(ks[23], (DEPTH, D, N_GROUPS)) * s_d
    moe_b_group = 0.01 * nrm(ks[24], (DEPTH, N_GROUPS))
    moe_w_router = nrm(ks[25], (DEPTH, N_GROUPS, D, EXPERTS_PER_GROUP)) * s_d
    moe_b_router = 0.01 * nrm(ks[26], (DEPTH, N_GROUPS, EXPERTS_PER_GROUP))
    moe_w_gate = nrm(ks[27], (DEPTH, N_GROUPS, EXPERTS_PER_GROUP, D, EXPERT_DFF)) * s_d
    moe_w_up = nrm(ks[28], (DEPTH, N_GROUPS, EXPERTS_PER_GROUP, D, EXPERT_DFF)) * s_d * DEEPNORM_BETA
    moe_w_down = nrm(ks[29], (DEPTH, N_GROUPS, EXPERTS_PER_GROUP, EXPERT_DFF, D)) * EXPERT_DFF ** -0.5 * DEEPNORM_BETA
    return {
        "x": x, "c": c,
        "mix_mod_w": mix_mod_w, "mix_mod_b": mix_mod_b, "mix_ln_g": mix_ln_g, "mix_ln_b": mix_ln_b,
        "a_w_in": a_w_in, "a_lam_q1": a_lam_q1, "a_lam_k1": a_lam_k1, "a_lam_q2": a_lam_q2,
        "a_lam_k2": a_lam_k2, "a_subln_w": a_subln_w, "a_w_out": a_w_out,
        "b_w_in": b_w_in, "b_forget_bias": b_forget_bias, "b_w_out": b_w_out,
        "ffn_mod_w": ffn_mod_w, "ffn_mod_b": ffn_mod_b, "ffn_ln_g": ffn_ln_g, "ffn_ln_b": ffn_ln_b,
        "moe_w_group": moe_w_group, "moe_b_group": moe_b_group,
        "moe_w_router": moe_w_router, "moe_b_router": moe_b_router,
        "moe_w_gate": moe_w_gate, "moe_w_up": moe_w_up, "moe_w_down": moe_w_down,
    }


def reference(x, c, mix_mod_w, mix_mod_b, mix_ln_g, mix_ln_b,
              a_w_in, a_lam_q1, a_lam_k1, a_lam_q2, a_lam_k2, a_subln_w, a_w_out,
              b_w_in, b_forget_bias, b_w_out,
              ffn_mod_w, ffn_mod_b, ffn_ln_g, ffn_ln_b,
              moe_w_group, moe_b_group, moe_w_router, moe_b_router,
              moe_w_gate, moe_w_up, moe_w_down):
    S = x.shape[1]
    positions = jnp.arange(S, dtype=jnp.int32)
    for i in range(DEPTH):
        j = i // N_MIXERS
        shift, scale, gate = adaln_modulation(c, mix_mod_w[i], mix_mod_b[i])
        h = x * (1.0 + scale) + shift
        if i % N_MIXERS == 0:
            y = differential_attention(h, positions, a_w_in[j], a_lam_q1[j], a_lam_k1[j],
                                       a_lam_q2[j], a_lam_k2[j], a_subln_w[j], a_w_out[j],
                                       diff_lambda_init(i + 1))
        else:
            y = forgetting_attention(h, b_w_in[j], b_forget_bias[j], b_w_out[j])
        x = layer_norm(DEEPNORM_ALPHA * x + gate * y, mix_ln_g[i], mix_ln_b[i])
        shift, scale, gate = adaln_modulation(c, ffn_mod_w[i], ffn_mod_b[i])
        h = x * (1.0 + scale) + shift
        y = hierarchical_moe(h, moe_w_group[i], moe_b_group[i], moe_w_router[i], moe_b_router[i],
                             moe_w_gate[i], moe_w_up[i], moe_w_down[i])
        x = layer_norm(DEEPNORM_ALPHA * x + gate * y, ffn_ln_g[i], ffn_ln_b[i])
    return x
```

```python
import math
import os
import numpy as np
import concourse.bass as bass
import concourse.mybir as mybir
from concourse.bass_utils import run_bass_kernel_spmd

F32 = mybir.dt.float32
BF16 = mybir.dt.bfloat16
AF = mybir.ActivationFunctionType
ALU = mybir.AluOpType
AX = mybir.AxisListType

D = 2048
KC = 16
DEPTH = 2
ALPHA = (2.0 * DEPTH) ** 0.25
LN_EPS = 1e-5
RMS_EPS = 1e-5
NEG = -30000.0
COMPUTE = ("tensor", "vector", "scalar", "gpsimd")
NDMASEM = 8


class Buf:
    _n = 0

    def __init__(self, h, name, psum=False):
        self.h = h
        self.psum = psum
        Buf._n += 1
        self.key = name + "#" + str(Buf._n)

    def __getitem__(self, idx):
        return self.h[idx]


def _k(x):
    return x.key if isinstance(x, Buf) else x


class _View:
    def __init__(self, ap, key):
        self.ap = ap
        self.key = key

    def __getitem__(self, idx):
        return self.ap


class Prog:
    def __init__(self, nc):
        self.nc = nc
        self.ops = []
        self.last_w = {}
        self.readers = {}
        self.last_eng = {}
        self.last_dma = {}
        self.cc_ops = []

    def sbuf(self, name, shape, dtype):
        return Buf(self.nc.alloc_sbuf_tensor(name, list(shape), dtype), name)

    def psum(self, name, shape, dtype=F32):
        return Buf(self.nc.alloc_psum_tensor(name, list(shape), dtype), name, psum=True)

    def op(self, eng, fn, reads=(), writes=(), dma=False, cc=False):
        i = len(self.ops)
        deps = set()
        pr = [r for r in reads if isinstance(r, Buf) and r.psum]
        if pr:
            reads = [r for r in reads if not (isinstance(r, Buf) and r.psum)]
            writes = list(writes) + [r for r in pr if r not in writes]
        for r in reads:
            k = _k(r)
            if k in self.last_w:
                deps.add(self.last_w[k])
        for w in writes:
            k = _k(w)
            if k in self.last_w:
                deps.add(self.last_w[k])
            for rd in self.readers.get(k, ()):
                deps.add(rd)
        for r in reads:
            self.readers.setdefault(_k(r), []).append(i)
        for w in writes:
            k = _k(w)
            self.last_w[k] = i
            self.readers[k] = []
        deps.discard(i)
        self.ops.append(dict(eng=eng, fn=fn, deps=deps, dma=dma, signal=False, cc=cc))
        if cc:
            self.cc_ops.append(i)
        elif dma:
            self.last_dma.setdefault(eng, []).append(i)
            self.last_dma[eng] = self.last_dma[eng][-NDMASEM:]
        else:
            self.last_eng[eng] = i
        return i

    def fence(self):
        deps = set(self.last_eng.values())
        for v in self.last_dma.values():
            deps.update(v)
        deps.update(self.cc_ops[-1:])
        for e in COMPUTE + ("sync",):
            self.ops.append(dict(eng=e, fn=None, deps=set(deps), dma=False, signal=False, cc=False))
        self.last_w = {}
        self.readers = {}

    def emit(self):
        nc = self.nc
        ops = self.ops

        def dom(o):
            if o["cc"]:
                return "cc"
            return ("dma", o["eng"]) if o["dma"] else o["eng"]

        ccsem = nc.alloc_semaphore(name="s_cc") if self.cc_ops else None
        ncc = 0
        for o in ops:
            if o["cc"]:
                ncc += 1
            o["ncc_before"] = ncc
        for o in ops:
            for d in o["deps"]:
                od = ops[d]
                if od["cc"]:
                    continue
                if (not od["dma"]) and (not o["dma"]) and od["eng"] == o["eng"] == "tensor":
                    continue
                od["signal"] = True
        cnt = {}
        dma_idx = {}
        for o in ops:
            dm = dom(o)
            if o["cc"]:
                continue
            if o["dma"]:
                n = dma_idx.get(dm, 0)
                o["dslot"] = n % NDMASEM
                o["dround"] = n // NDMASEM
                dma_idx[dm] = n + 1
            elif o["signal"] and o["fn"] is not None:
                cnt[dm] = cnt.get(dm, 0) + 1
                o["cnt"] = cnt[dm]
        sems = {e: nc.alloc_semaphore(name="s_" + e) for e in COMPUTE}
        dsems = {dm: [nc.alloc_semaphore(name="d_%s_%d" % (dm[1], k)) for k in range(NDMASEM)]
                 for dm in dma_idx}
        streams = {}
        for i, o in enumerate(ops):
            streams.setdefault(o["eng"], []).append(i)

        def run_stream(eng_name, engine):
            waited = {}

            def wait(sem, key, val):
                if waited.get(key, -1) >= val:
                    return
                engine.wait_ge(sem, val)
                waited[key] = val

            for i in streams.get(eng_name, []):
                o = ops[i]
                for d in sorted(o["deps"]):
                    od = ops[d]
                    if od["cc"]:
                        wait(ccsem, "cc", o["ncc_before"] - (1 if o["cc"] else 0))
                        continue
                    if od["dma"]:
                        dm = dom(od)
                        wait(dsems[dm][od["dslot"]], (dm, od["dslot"]), 16 * (od["dround"] + 1))
                    else:
                        if "cnt" not in od:
                            continue
                        if od["eng"] == eng_name == "tensor" and not o["dma"]:
                            continue
                        wait(sems[od["eng"]], od["eng"], od["cnt"])
                if o["dma"] and not o["cc"] and o["dround"] > 0:
                    dm = dom(o)
                    wait(dsems[dm][o["dslot"]], (dm, o["dslot"]), 16 * o["dround"])
                if o["fn"] is None:
                    continue
                ins = o["fn"](engine)
                if o["cc"]:
                    ins.then_inc(ccsem, 1)
                elif o["dma"]:
                    ins.then_inc(dsems[dom(o)][o["dslot"]], 16)
                elif "cnt" in o:
                    ins.then_inc(sems[eng_name], 1)

        with nc.Block() as block:
            @block.tensor
            def _(e):
                run_stream("tensor", e)

            @block.vector
            def _(e):
                run_stream("vector", e)

            @block.scalar
            def _(e):
                run_stream("scalar", e)

            @block.gpsimd
            def _(e):
                run_stream("gpsimd", e)

            @block.sync
            def _(e):
                run_stream("sync", e)


class Builder:
    def __init__(self, T, TP, layers, n_exp_groups=4):
        self.T, self.TP, self.TC = T, TP, T + TP
        self.layers = layers
        nc = self.nc = bass.Bass("TRN2", target_bir_lowering=False)
        P = self.P = Prog(nc)
        self.inputs = {}
        self.BIGA = P.sbuf("BIGA", [128, 32768], BF16)
        self.BIGB = P.sbuf("BIGB", [128, 16384], BF16)
        self.W = [P.sbuf("W%d" % i, [128, 8192], BF16) for i in range(5)]
        self.BC = [P.sbuf("BC%d" % i, [128, 2048], F32) for i in range(3)]
        self.ident = P.sbuf("ident", [128, 128], F32)
        self.identb = P.sbuf("identb", [128, 128], BF16)
        self.tri = P.sbuf("tri", [128, 128], F32)
        self.trib = P.sbuf("trib", [128, 128], BF16)
        self.rotT = P.sbuf("rotT", [128, 128], BF16)
        self.ones = P.sbuf("ones", [128, 128], F32)
        self.pbias = P.sbuf("pbias", [128, 1], F32)
        self.zero = P.sbuf("zero", [128, 1], F32)
        self.cvec = P.sbuf("cvec", [128, 16], F32)
        self.rowbuf = _View(self.BIGA.h.bitcast(F32)[0:1, 0:6144], self.BIGA.key)
        self.small = P.sbuf("small", [128, 64], F32)
        self.lam = P.sbuf("lam", [128, 8], F32)
        self.subw = P.sbuf("subw", [128, 256], F32)
        self.w32 = P.sbuf("w32", [128, 8, 32], F32)
        self.rt = P.sbuf("rt", [128, 128], F32)
        self.wr = P.sbuf("wr", [128, 16, 36], F32)
        self.brt = P.sbuf("brt", [128, 36], F32)
        bf = self.BIGB.h.bitcast(F32)
        self.lf = bf[:, 0:512].rearrange("p (t h) -> p t h", h=16)
        self.cum = bf[:, 512:1024].rearrange("p (t h) -> p t h", h=16)
        self.anc = bf[:, 1024:1088].rearrange("p (t h) -> p t h", h=16)
        self.fb = P.sbuf("fb", [128, 16], F32)
        self.wf = _View(self.BIGB[:, 4096:4352].rearrange("p (k n) -> p k n", k=16), "wfkey")
        self.PS = [P.psum("ps%d" % i, [128, 512], F32) for i in range(8)]

    def din(self, name, shape, dt=F32):
        t = self.nc.dram_tensor(name, list(shape), dt, kind="ExternalInput").ap()
        self.inputs[name] = t
        return t

    def dscr(self, name, shape, dt):
        return self.nc.dram_tensor(name, list(shape), dt).ap()

    def dma(self, eng, out, in_, reads=(), writes=()):
        self.P.op(eng, lambda e: e.dma_start(out=out, in_=in_), reads=reads, writes=writes, dma=True)

    def f32view(self, buf, off_f32, n):
        return buf.h.bitcast(F32)[:, off_f32:off_f32 + n]

    def load_consts(self):
        P = self.P
        c_ident = self.din("c_ident", [128, 128])
        c_tri = self.din("c_tri", [128, 128])
        c_rotT = self.din("c_rotT", [128, 128])
        c_pbias = self.din("c_pbias", [128, 1])
        self.dma("sync", self.ident[:], c_ident, writes=[self.ident])
        self.dma("sync", self.tri[:], c_tri, writes=[self.tri])
        self.dma("sync", self.pbias[:], c_pbias, writes=[self.pbias])
        self.dma("gpsimd", self.identb[:], c_ident, writes=[self.identb])
        self.dma("gpsimd", self.trib[:], c_tri, writes=[self.trib])
        self.dma("gpsimd", self.rotT[:], c_rotT, writes=[self.rotT])
        P.op("vector", lambda e: e.memset(self.ones[:], 1.0), writes=[self.ones])
        P.op("vector", lambda e: e.memset(self.zero[:], 0.0), writes=[self.zero])
        cv = self.din("cvec_in", [128, 16])
        self.dma("sync", self.cvec[:], cv, writes=[self.cvec])
        P.op("scalar", lambda e: e.activation(out=self.cvec[:], in_=self.cvec[:], func=AF.Silu),
             reads=[self.cvec], writes=[self.cvec])

    def modulation(self, wmod, bmod):
        P = self.P
        wst = [self.f32view(self.W[0], 0, 4096), self.f32view(self.W[1], 0, 4096),
               self.f32view(self.W[2], 0, 4096), self.f32view(self.W[3], 0, 4096)]
        wk = [self.W[0], self.W[1], self.W[2], self.W[3]]
        ps = self.PS[0]
        self.dma("sync", self.rowbuf.ap, bmod, writes=[self.rowbuf])
        for n in range(12):
            a, b = (0, 1) if n % 2 == 0 else (2, 3)
            src = wmod[:, n * 512:(n + 1) * 512].rearrange("(k p) n -> p k n", p=128)
            self.dma("sync", wst[a].rearrange("p (k n) -> p k n", k=8), src[:, 0:8, :], writes=[wk[a]])
            self.dma("sync", wst[b].rearrange("p (k n) -> p k n", k=8), src[:, 8:16, :], writes=[wk[b]])
            for k in range(16):
                wb = a if k < 8 else b
                kk = k % 8
                P.op("tensor", lambda e, wb=wb, kk=kk, k=k: e.matmul(
                    out=ps[0:1, :], lhsT=self.cvec[:, k:k + 1], rhs=wst[wb][:, kk * 512:(kk + 1) * 512],
                    start=(k == 0), stop=(k == 15)), reads=[self.cvec, wk[wb]], writes=[ps])
            addc = 0.0 if n < 4 else 1.0
            P.op("vector", lambda e, n=n, addc=addc: e.scalar_tensor_tensor(
                out=self.rowbuf.ap[0:1, n * 512:(n + 1) * 512], in0=ps[0:1, :], scalar=addc,
                in1=self.rowbuf.ap[0:1, n * 512:(n + 1) * 512], op0=ALU.add, op1=ALU.add),
                reads=[ps, self.rowbuf], writes=[self.rowbuf])
        self.bcast_mod()

    def bcast_mod(self):
        P = self.P
        for n in range(12):
            dst = [self.BC[1], self.BC[0], self.BC[2]][n // 4]
            psb = self.PS[1 + n % 2]
            P.op("tensor", lambda e, n=n, psb=psb: e.matmul(
                out=psb[:], lhsT=self.ones[0:1, :], rhs=self.rowbuf.ap[0:1, n * 512:(n + 1) * 512],
                start=True, stop=True), reads=[self.ones, self.rowbuf], writes=[psb])
            P.op("scalar", lambda e, n=n, psb=psb, dst=dst: e.activation(
                out=dst[:, (n % 4) * 512:(n % 4 + 1) * 512], in_=psb[:], func=AF.Copy),
                reads=[psb], writes=[dst])

    def make_hT(self, x_ap, ntok, hT, hT_key, router=None):
        P = self.P
        xt = [self.f32view(self.W[0], 0, 2048), self.f32view(self.W[1], 0, 2048)]
        xk = [self.W[0], self.W[1]]
        htf = self.f32view(self.W[2], 0, 2048)
        for t in range(ntok // 128):
            xb, xkk = xt[t % 2], xk[t % 2]
            self.dma("sync", xb, (x_ap(t) if callable(x_ap) else x_ap[t * 128:(t + 1) * 128, :]), writes=[xkk])
            if router is not None:
                acc = router["acc"](t)
                P.op("scalar", lambda e, xb=xb, acc=acc: e.activation(out=acc, in_=xb, func=AF.Copy, scale=ALPHA),
                     reads=[xkk], writes=[(router["acckey"], t)])
            P.op("vector", lambda e, xb=xb: e.tensor_tensor(out=xb, in0=xb, in1=self.BC[0][:], op=ALU.mult),
                 reads=[xkk, self.BC[0]], writes=[xkk])
            P.op("vector", lambda e, xb=xb: e.tensor_tensor(out=xb, in0=xb, in1=self.BC[1][:], op=ALU.add),
                 reads=[xkk, self.BC[1]], writes=[xkk])
            for g in range(4):
                ps = self.PS[g % 4]
                for j in range(4):
                    k = g * 4 + j
                    P.op("tensor", lambda e, xb=xb, k=k, j=j, ps=ps: e.transpose(
                        out=ps[:, j * 128:(j + 1) * 128], in_=xb[:, k * 128:(k + 1) * 128], identity=self.ident[:]),
                        reads=[xkk, self.ident], writes=[ps])
                eng = "vector" if g % 2 == 0 else "scalar"
                dst = hT[:, g * 4:(g + 1) * 4, t * 128:(t + 1) * 128]
                src = ps[:].rearrange("p (a b) -> p a b", a=4)
                if eng == "vector":
                    P.op("vector", lambda e, dst=dst, src=src: e.tensor_copy(out=dst, in_=src),
                         reads=[ps], writes=[(hT_key, t)])
                else:
                    P.op("scalar", lambda e, dst=dst, src=src: e.activation(out=dst, in_=src, func=AF.Copy),
                         reads=[ps], writes=[(hT_key, t)])
                if router is not None:
                    P.op("gpsimd" if False else "vector", lambda e, src=src, g=g: e.tensor_copy(
                        out=htf[:, g * 512:(g + 1) * 512].rearrange("p (a b) -> p a b", a=4), in_=src),
                        reads=[ps], writes=[self.W[2]])
            if router is not None:
                self.route(t, htf, router)

    def route(self, t, htf, router):
        P = self.P
        ps = self.PS[4]
        for k in range(16):
            P.op("tensor", lambda e, k=k: e.matmul(out=ps[:, 0:36], lhsT=htf[:, k * 128:(k + 1) * 128],
                                                   rhs=self.wr[:, k, :], start=(k == 0), stop=(k == 15)),
                 reads=[self.W[2], self.wr], writes=[ps])
        rt = self.rt
        V = "vector"

        def v(fn, reads=(), writes=()):
            P.op(V, fn, reads=[rt] + list(reads), writes=[rt] + list(writes))
        v(lambda e: e.tensor_tensor(out=rt[:, 0:36], in0=ps[:, 0:36], in1=self.brt[:], op=ALU.add), reads=[ps, self.brt])
        v(lambda e: e.reduce_max(out=rt[:, 36:37], in_=rt[:, 0:4], axis=AX.X))
        v(lambda e: e.tensor_scalar(out=rt[:, 40:44], in0=rt[:, 0:4], scalar1=rt[:, 36:37], scalar2=None, op0=ALU.is_ge))
        v(lambda e: e.tensor_scalar(out=rt[:, 44:48], in0=rt[:, 0:4], scalar1=rt[:, 36:37], scalar2=None, op0=ALU.subtract))
        P.op("scalar", lambda e: e.activation(out=rt[:, 44:48], in_=rt[:, 44:48], func=AF.Exp, accum_out=rt[:, 37:38]),
             reads=[rt], writes=[rt])
        v(lambda e: e.reciprocal(out=rt[:, 38:39], in_=rt[:, 37:38]))
        v(lambda e: e.tensor_scalar(out=rt[:, 48:56], in0=rt[:, 4:12], scalar1=rt[:, 40:41], scalar2=None, op0=ALU.mult))
        for g in range(1, 4):
            v(lambda e, g=g: e.scalar_tensor_tensor(out=rt[:, 48:56], in0=rt[:, 4 + 8 * g:12 + 8 * g],
                                                    scalar=rt[:, 40 + g:41 + g], in1=rt[:, 48:56],
                                                    op0=ALU.mult, op1=ALU.add))
        v(lambda e: e.reduce_max(out=rt[:, 56:57], in_=rt[:, 48:56], axis=AX.X))
        v(lambda e: e.tensor_scalar(out=rt[:, 64:72], in0=rt[:, 48:56], scalar1=rt[:, 56:57], scalar2=None, op0=ALU.is_ge))
        v(lambda e: e.scalar_tensor_tensor(out=rt[:, 72:80], in0=rt[:, 64:72], scalar=NEG, in1=rt[:, 48:56],
                                           op0=ALU.mult, op1=ALU.add))
        v(lambda e: e.reduce_max(out=rt[:, 57:58], in_=rt[:, 72:80], axis=AX.X))
        v(lambda e: e.tensor_scalar(out=rt[:, 80:88], in0=rt[:, 72:80], scalar1=rt[:, 57:58], scalar2=None, op0=ALU.is_ge))
        v(lambda e: e.tensor_tensor(out=rt[:, 58:59], in0=rt[:, 57:58], in1=rt[:, 56:57], op=ALU.subtract))
        P.op("scalar", lambda e: e.activation(out=rt[:, 59:60], in_=rt[:, 58:59], func=AF.Exp), reads=[rt], writes=[rt])
        v(lambda e: e.tensor_scalar(out=rt[:, 60:61], in0=rt[:, 59:60], scalar1=1.0, scalar2=None, op0=ALU.add))
        v(lambda e: e.reciprocal(out=rt[:, 61:62], in_=rt[:, 60:61]))
        v(lambda e: e.tensor_tensor(out=rt[:, 62:63], in0=rt[:, 59:60], in1=rt[:, 61:62], op=ALU.mult))
        v(lambda e: e.tensor_scalar(out=rt[:, 64:72], in0=rt[:, 64:72], scalar1=rt[:, 61:62], scalar2=None, op0=ALU.mult))
        v(lambda e: e.scalar_tensor_tensor(out=rt[:, 64:72], in0=rt[:, 80:88], scalar=rt[:, 62:63], in1=rt[:, 64:72],
                                           op0=ALU.mult, op1=ALU.add))
        v(lambda e: e.tensor_scalar(out=rt[:, 64:72], in0=rt[:, 64:72], scalar1=rt[:, 38:39], scalar2=None, op0=ALU.mult))
        for g in range(4):
            P.op(V, lambda e, g=g: e.tensor_scalar(out=self.w32[:, t, g * 8:(g + 1) * 8], in0=rt[:, 64:72],
                                                   scalar1=rt[:, 40 + g:41 + g], scalar2=None, op0=ALU.mult),
                 reads=[rt], writes=[("w32", t)])

    def layer_norm(self, z, zkey, gt, bt, scr, scrkey):
        P = self.P
        st = self.small
        P.op("vector", lambda e: e.reduce_sum(out=st[:, 0:1], in_=z, axis=AX.X), reads=[zkey], writes=[st])
        P.op("vector", lambda e: e.tensor_scalar(out=st[:, 1:2], in0=st[:, 0:1], scalar1=-1.0 / D, scalar2=None, op0=ALU.mult),
             reads=[st], writes=[st])
        P.op("scalar", lambda e: e.activation(out=scr, in_=z, func=AF.Square, bias=st[:, 1:2], scale=1.0, accum_out=st[:, 2:3]),
             reads=[zkey, st], writes=[scrkey, st])
        P.op("vector", lambda e: e.tensor_scalar(out=st[:, 3:4], in0=st[:, 2:3], scalar1=1.0 / D, scalar2=LN_EPS, op0=ALU.mult, op1=ALU.add),
             reads=[st], writes=[st])
        P.op("scalar", lambda e: e.activation(out=st[:, 5:6], in_=st[:, 3:4], func=AF.Sqrt), reads=[st], writes=[st])
        P.op("vector", lambda e: e.reciprocal(out=st[:, 4:5], in_=st[:, 5:6]), reads=[st], writes=[st])
        P.op("vector", lambda e: e.tensor_scalar(out=z, in0=z, scalar1=st[:, 1:2], scalar2=st[:, 4:5], op0=ALU.add, op1=ALU.mult),
             reads=[zkey, st], writes=[zkey])
        P.op("vector", lambda e: e.tensor_tensor(out=z, in0=z, in1=gt[:], op=ALU.mult), reads=[zkey, gt], writes=[zkey])
        P.op("vector", lambda e: e.tensor_tensor(out=z, in0=z, in1=bt[:], op=ALU.add), reads=[zkey, bt], writes=[zkey])

    def mixer_params(self, kind, lam_init, lamv, subw, fbias, w_in):
        B = self
        P = self.P
        if kind == "A":
            lt = [B.f32view(B.W[4], i * 128, 128) for i in range(4)]
            for i in range(4):
                B.dma("sync", lt[i], lamv[i].partition_broadcast(128), writes=[B.W[4]])
            P.op("vector", lambda e: e.tensor_tensor(out=lt[0], in0=lt[0], in1=lt[1], op=ALU.mult), reads=[B.W[4]], writes=[B.W[4]])
            P.op("vector", lambda e: e.tensor_tensor(out=lt[2], in0=lt[2], in1=lt[3], op=ALU.mult), reads=[B.W[4]], writes=[B.W[4]])
            P.op("vector", lambda e: e.reduce_sum(out=B.lam[:, 2:3], in_=lt[0], axis=AX.X), reads=[B.W[4]], writes=[B.lam])
            P.op("vector", lambda e: e.reduce_sum(out=B.lam[:, 3:4], in_=lt[2], axis=AX.X), reads=[B.W[4]], writes=[B.lam])
            P.op("scalar", lambda e: e.activation(out=B.lam[:, 4:6], in_=B.lam[:, 2:4], func=AF.Exp), reads=[B.lam], writes=[B.lam])
            P.op("vector", lambda e: e.tensor_tensor(out=B.lam[:, 0:1], in0=B.lam[:, 4:5], in1=B.lam[:, 5:6], op=ALU.subtract), reads=[B.lam], writes=[B.lam])
            P.op("vector", lambda e: e.tensor_scalar(out=B.lam[:, 0:1], in0=B.lam[:, 0:1], scalar1=lam_init, scalar2=None, op0=ALU.add), reads=[B.lam], writes=[B.lam])
            P.op("vector", lambda e: e.tensor_scalar(out=B.lam[:, 1:2], in0=B.lam[:, 0:1], scalar1=-1.0, scalar2=None, op0=ALU.mult), reads=[B.lam], writes=[B.lam])
            B.dma("sync", B.subw[:], subw.partition_broadcast(128), writes=[B.subw])
            P.op("vector", lambda e: e.tensor_scalar(out=B.subw[:], in0=B.subw[:], scalar1=1.0 - lam_init, scalar2=None, op0=ALU.mult), reads=[B.subw], writes=[B.subw])
        else:
            B.dma("sync", B.fb[:], fbias.partition_broadcast(128), writes=[B.fb])
            B.dma("gpsimd", B.wf.ap, w_in[:, 6144:6160].rearrange("(k p) n -> p k n", p=128), writes=[B.wf])

    def qkv(self, l, kind, w_in, hT, hT_key, ntok, tok0, with_q, sc):
        P = self.P
        wb = [self.W[0], self.W[1]]
        nblk = 12
        first = 0 if with_q else 4
        bi = 0
        import os
        skip = os.environ.get("QKV_SKIP", "")
        for n in range(first, nblk):
            if (skip == "v" and n >= 8) or (skip == "qk" and n < 8):
                continue
            wt = wb[bi % 2]
            bi += 1
            wv = wt[:].rearrange("p (k n) -> p k n", k=16)
            self.dma("gpsimd", wv, w_in[:, n * 512:(n + 1) * 512].rearrange("(k p) n -> p k n", p=128), writes=[wt])
            if n < 8:
                isq = n < 4
                for tb in range(ntok // 512):
                    if kind == "A":
                        cs = [self.f32view(self.W[2], 0, 512), self.f32view(self.W[2], 512, 512)]
                        t0 = tok0 + tb * 512
                        self.dma("sync", cs[0], sc["cos"][:, t0:t0 + 512], writes=[(self.W[2].key, "c")])
                        self.dma("sync", cs[1], sc["sin"][:, t0:t0 + 512], writes=[(self.W[2].key, "s")])
                    for mi in range(4):
                        m = (n % 4) * 4 + mi
                        ps = self.PS[mi % 2]
                        for k in range(16):
                            P.op("tensor", lambda e, k=k, mi=mi, tb=tb, ps=ps, wv=wv: e.matmul(
                                out=ps[:], lhsT=wv[:, k, mi * 128:(mi + 1) * 128], rhs=hT[:, k, tb * 512:(tb + 1) * 512],
                                start=(k == 0), stop=(k == 15)), reads=[wt] + [(hT_key, tb * 4 + i) for i in range(4)], writes=[ps])
                        ob = self.W[3][:, (mi % 2) * 512:(mi % 2) * 512 + 512]
                        okey = (self.W[3].key, "o", mi % 2)
                        lvl = int(os.environ.get("QK_LEVEL", "9"))
                        if lvl == 0:
                            P.op("scalar", lambda e, ob=ob, ps=ps: e.activation(out=ob, in_=ps[:], func=AF.Copy),
                                 reads=[ps], writes=[okey])
                        elif kind == "A":
                            qraw = self.W[3][:, 1024 + (mi % 2) * 512:1024 + (mi % 2) * 512 + 512]
                            qkey = (self.W[3].key, "q", mi % 2)
                            ps2 = self.PS[2 + mi % 2]
                            t1 = self.f32view(self.W[3], 1024 + (mi % 2) * 1024, 512)
                            t2 = self.f32view(self.W[3], 1024 + (mi % 2) * 1024 + 512, 512)
                            tkey = (self.W[3].key, "t", mi % 2)
                            P.op("scalar", lambda e, qraw=qraw, ps=ps: e.activation(out=qraw, in_=ps[:], func=AF.Copy),
                                 reads=[ps], writes=[qkey])
                            P.op("tensor", lambda e, qraw=qraw, ps2=ps2: e.matmul(out=ps2[:], lhsT=self.rotT[:], rhs=qraw,
                                                                                   start=True, stop=True),
                                 reads=[qkey, self.rotT], writes=[ps2])
                            P.op("vector", lambda e, t1=t1, ps=ps, cs=cs: e.tensor_tensor(out=t1, in0=ps[:], in1=cs[0], op=ALU.mult),
                                 reads=[ps, (self.W[2].key, "c")], writes=[(tkey, 1)])
                            P.op("vector", lambda e, t2=t2, ps2=ps2, cs=cs: e.tensor_tensor(out=t2, in0=ps2[:], in1=cs[1], op=ALU.mult),
                                 reads=[ps2, (self.W[2].key, "s")], writes=[(tkey, 2)])
                            P.op("vector", lambda e, ob=ob, t1=t1, t2=t2: e.tensor_tensor(out=ob, in0=t1, in1=t2, op=ALU.add),
                                 reads=[(tkey, 1), (tkey, 2)], writes=[okey])
                        else:
                            P.op("scalar", lambda e, ob=ob, ps=ps: e.activation(out=ob, in_=ps[:], func=AF.Copy),
                                 reads=[ps], writes=[okey])
                        if isq:
                            dst = sc["qT"][m, :, tb * 512:(tb + 1) * 512]
                            dk = ("qT", m)
                        else:
                            dst = sc["kT"][m, :, tok0 + tb * 512:tok0 + (tb + 1) * 512]
                            dk = ("kT", m)
                        if os.environ.get("NO_STORE", "") != "1":
                            self.dma("sync", dst, ob, reads=[okey], writes=[dk])
            else:
                hv = 256 if kind == "A" else 128
                nh = 512 // hv
                for tt in range(ntok // 128):
                    ps = self.PS[4 + tt % 2]
                    for k in range(16):
                        P.op("tensor", lambda e, k=k, tt=tt, ps=ps, wv=wv: e.matmul(
                            out=ps[:], lhsT=hT[:, k, tt * 128:(tt + 1) * 128], rhs=wv[:, k, :],
                            start=(k == 0), stop=(k == 15)), reads=[wt, (hT_key, tt)], writes=[ps])
                    vb = self.W[2][:, 2048 + (tt % 2) * 1024:2048 + (tt % 2) * 1024 + nh * (hv + 1)].rearrange("p (h c) -> p h c", h=nh)
                    vkey = (self.W[2].key, "v", tt % 2)
                    P.op("scalar" if tt % 2 == 0 else "vector",
                         (lambda e, vb=vb, ps=ps: e.activation(out=vb[:, :, 0:hv], in_=ps[:].rearrange("p (h c) -> p h c", h=nh), func=AF.Copy))
                         if tt % 2 == 0 else
                         (lambda e, vb=vb, ps=ps: e.tensor_copy(out=vb[:, :, 0:hv], in_=ps[:].rearrange("p (h c) -> p h c", h=nh))),
                         reads=[ps], writes=[vkey])
                    P.op("gpsimd", lambda e, vb=vb: e.memset(vb[:, :, hv:hv + 1], 1.0), writes=[vkey])
                    h0 = (n - 8) * nh
                    ct = (tok0 // 128) + tt
                    self.dma("sync", sc["v"][h0:h0 + nh, ct, :, :].rearrange("h p c -> p h c"), vb, reads=[vkey], writes=[("v", h0)])
        if kind == "B":
            P.op("sync", None, reads=[self.wf])
            for tt in range(ntok // 128):
                ps = self.PS[6]
                ct = (tok0 // 128) + tt
                for k in range(16):
                    P.op("tensor", lambda e, k=k, tt=tt: e.matmul(out=ps[:, 0:16], lhsT=hT[:, k, tt * 128:(tt + 1) * 128],
                                                                  rhs=self.wf.ap[:, k, :], start=(k == 0), stop=(k == 15)),
                         reads=[self.wf, (hT_key, tt)], writes=[ps])
                st = self.small
                P.op("vector", lambda e: e.tensor_tensor(out=st[:, 16:32], in0=ps[:, 0:16], in1=self.fb[:], op=ALU.add),
                     reads=[ps, self.fb], writes=[st])
                P.op("scalar", lambda e: e.activation(out=st[:, 32:48], in_=st[:, 16:32], func=AF.Exp, scale=-1.0), reads=[st], writes=[st])
                P.op("scalar", lambda e: e.activation(out=st[:, 32:48], in_=st[:, 32:48], func=AF.Ln, bias=1.0, scale=1.0), reads=[st], writes=[st])
                P.op("vector", lambda e, ct=ct: e.tensor_scalar(out=self.lf[:, ct, :], in0=st[:, 32:48], scalar1=-1.0, scalar2=None, op0=ALU.mult),
                     reads=[st], writes=[("lf", ct)])

    def attention(self, l, kind, sc):
        P = self.P
        T, TP, TC = self.T, self.TP, self.TC
        nprev = TP // 128
        nq = T // 512
        scale = 128 ** -0.5
        if kind == "A":
            nheads, nmaps, hv = 8, 2, 256
        else:
            nheads, nmaps, hv = 16, 1, 128
        hv1 = hv + 1
        kTs = [self.BIGA[:, m * TC:(m + 1) * TC] for m in range(nmaps)]
        o0 = nmaps * TC
        qTs = [self.BIGA[:, o0 + m * T:o0 + (m + 1) * T] for m in range(nmaps)]
        o1 = o0 + nmaps * T
        v1 = self.BIGA[:, o1:o1 + (TC // 128) * hv1].rearrange("p (t c) -> p t c", c=hv1)
        assert o1 + (TC // 128) * hv1 <= 32768
        if kind == "B":
            ps = self.PS[7]
            for t in range(TC // 128):
                for t2 in range(t + 1):
                    P.op("tensor", lambda e, t=t, t2=t2: e.matmul(
                        out=ps[:, 0:16], lhsT=(self.tri[:] if t2 == t else self.ones[:]), rhs=self.lf[:, t2, :],
                        start=(t2 == 0), stop=(t2 == t)), reads=[("lf", t2), self.tri, self.ones], writes=[ps])
                P.op("vector", lambda e, t=t: e.tensor_copy(out=self.cum[:, t, :], in_=ps[:, 0:16]), reads=[ps], writes=[("cum", t)])
            for g in range(nq):
                ta = (TP + g * 512 + 256) // 128
                for t2 in range(ta):
                    P.op("tensor", lambda e, t2=t2, ta=ta: e.matmul(out=ps[:, 0:16], lhsT=self.ones[:], rhs=self.lf[:, t2, :],
                                                                    start=(t2 == 0), stop=(t2 == ta - 1)),
                         reads=[("lf", t2), self.ones], writes=[ps])
                P.op("vector", lambda e, g=g: e.tensor_copy(out=self.anc[:, g, :], in_=ps[:, 0:16]), reads=[ps], writes=[("anc", g)])
        ebuf = [self.W[0], self.W[1]]
        for h in range(nheads):
            for m in range(nmaps):
                mm = h * nmaps + m
                self.dma("sync", kTs[m], sc["kT"][mm, :, :], reads=[("kT", mm)], writes=[("kTs", m)])
                self.dma("sync", qTs[m], sc["qT"][mm, :, :], reads=[("qT", mm)], writes=[("qTs", m)])
            self.dma("sync", v1, sc["v"][h, :, :, :].rearrange("t p c -> p t c"), reads=[("v", (h // (512 // hv)) * (512 // hv))], writes=["v1"])
            for g in range(nq):
                nkb = nprev + 4 * g + 4
                o1n = self.f32view(self.W[2], 0, 1024).rearrange("p (i c) -> p i c", i=4)
                for m in range(nmaps):
                    for j in range(nkb):
                        pss = self.PS[4 + j % 2]
                        P.op("tensor", lambda e, m=m, j=j, g=g, pss=pss: e.matmul(
                            out=pss[:], lhsT=kTs[m][:, j * 128:(j + 1) * 128], rhs=qTs[m][:, g * 512:(g + 1) * 512],
                            start=True, stop=True), reads=[("kTs", m), ("qTs", m)], writes=[pss])
                        et = ebuf[j % 2]
                        ev = et[:, 0:512]
                        if kind == "A":
                            bias = self.pbias[:, 0:1] if j < nprev else self.zero[:, 0:1]
                            breads = [self.pbias, self.zero]
                        else:
                            bcol = self.small[:, 48 + (j % 2):49 + (j % 2)]
                            P.op("vector", lambda e, bcol=bcol, g=g, j=j, h=h: e.tensor_tensor(
                                out=bcol, in0=self.anc[:, g, h:h + 1], in1=self.cum[:, j, h:h + 1], op=ALU.subtract),
                                reads=[("anc", g), ("cum", j)], writes=[("bcol", j % 2)])
                            if j < nprev:
                                P.op("vector", lambda e, bcol=bcol: e.tensor_tensor(out=bcol, in0=bcol, in1=self.pbias[:, 0:1], op=ALU.add),
                                     reads=[("bcol", j % 2), self.pbias], writes=[("bcol", j % 2)])
                            bias = bcol
                            breads = [("bcol", j % 2)]
                        P.op("scalar", lambda e, ev=ev, pss=pss, bias=bias: e.activation(out=ev, in_=pss[:], func=AF.Exp, bias=bias, scale=scale),
                             reads=[pss] + breads, writes=[et])
                        jo = j - nprev
                        for i in range(4):
                            qi = 4 * g + i
                            if jo > qi:
                                continue
                            if jo == qi:
                                P.op("gpsimd", lambda e, ev=ev, i=i: e.tensor_tensor(
                                    out=ev[:, i * 128:(i + 1) * 128], in0=ev[:, i * 128:(i + 1) * 128], in1=self.trib[:], op=ALU.mult),
                                    reads=[et, self.trib], writes=[et])
                            pso = self.PS[i]
                            last = nprev + qi
                            P.op("tensor", lambda e, ev=ev, i=i, j=j, pso=pso, last=last: e.matmul(
                                out=pso[:, 0:hv1], lhsT=ev[:, i * 128:(i + 1) * 128], rhs=v1[:, j, :],
                                start=(j == 0), stop=(j == last)), reads=[et, "v1"], writes=[pso])
                    st = self.small
                    for i in range(4):
                        pso = self.PS[i]
                        qi = 4 * g + i
                        P.op("vector", lambda e, pso=pso, i=i: e.reciprocal(out=st[:, 8 + i:9 + i], in_=pso[:, hv:hv1]), reads=[pso], writes=[st])
                        if kind == "A" and m == 0:
                            P.op("vector", lambda e, pso=pso, i=i: e.tensor_scalar(
                                out=o1n[:, i, :], in0=pso[:, 0:hv], scalar1=st[:, 8 + i:9 + i], scalar2=None, op0=ALU.mult),
                                reads=[pso, st], writes=[(self.W[2].key, "o1n", i)])
                        elif kind == "A":
                            ot = self.f32view(self.W[3], i * 256, 256)
                            okey = (self.W[3].key, "ot", i)
                            P.op("vector", lambda e, i=i: e.tensor_tensor(out=st[:, 12 + i:13 + i], in0=st[:, 8 + i:9 + i], in1=self.lam[:, 1:2], op=ALU.mult),
                                 reads=[st, self.lam], writes=[st])
                            P.op("vector", lambda e, pso=pso, i=i, ot=ot: e.scalar_tensor_tensor(
                                out=ot, in0=pso[:, 0:hv], scalar=st[:, 12 + i:13 + i], in1=o1n[:, i, :], op0=ALU.mult, op1=ALU.add),
                                reads=[pso, st, (self.W[2].key, "o1n", i)], writes=[okey])
                            sq = self.f32view(self.W[3], 1024 + i * 256, 256)
                            P.op("scalar", lambda e, ot=ot, sq=sq, i=i: e.activation(out=sq, in_=ot, func=AF.Square, accum_out=st[:, 16 + i:17 + i]),
                                 reads=[okey], writes=[(self.W[3].key, "sq", i), st])
                            P.op("vector", lambda e, i=i: e.tensor_scalar(out=st[:, 20 + i:21 + i], in0=st[:, 16 + i:17 + i], scalar1=1.0 / 256,
                                                                         scalar2=RMS_EPS, op0=ALU.mult, op1=ALU.add), reads=[st], writes=[st])
                            P.op("scalar", lambda e, i=i: e.activation(out=st[:, 24 + i:25 + i], in_=st[:, 20 + i:21 + i], func=AF.Sqrt), reads=[st], writes=[st])
                            P.op("vector", lambda e, i=i: e.reciprocal(out=st[:, 28 + i:29 + i], in_=st[:, 24 + i:25 + i]), reads=[st], writes=[st])
                            ob = self.W[3][:, 4096 + i * 256:4096 + (i + 1) * 256]
                            obk = (self.W[3].key, "ob", i)
                            P.op("vector", lambda e, ot=ot, ob=ob, i=i: e.scalar_tensor_tensor(
                                out=ob, in0=ot, scalar=st[:, 28 + i:29 + i], in1=self.subw[:], op0=ALU.mult, op1=ALU.mult),
                                reads=[okey, st, self.subw], writes=[obk])
                            self.dma("sync", sc["o"][qi * 128:(qi + 1) * 128, h * 256:(h + 1) * 256], ob, reads=[obk], writes=[("o", qi)])
                        else:
                            ob = self.W[3][:, 4096 + i * 128:4096 + (i + 1) * 128]
                            obk = (self.W[3].key, "ob", i)
                            P.op("vector", lambda e, pso=pso, ob=ob, i=i: e.tensor_scalar(
                                out=ob, in0=pso[:, 0:hv], scalar1=st[:, 8 + i:9 + i], scalar2=None, op0=ALU.mult),
                                reads=[pso, st], writes=[obk])
                            self.dma("sync", sc["o"][qi * 128:(qi + 1) * 128, h * 128:(h + 1) * 128], ob, reads=[obk], writes=[("o", qi)])

    def out_proj(self, l, w_out, x_ap, lng, lnb, sc, x1_ap):
        P = self.P
        T = self.T
        wo = [self.W[i] for i in range(4)]
        for n in range(4):
            self.dma("gpsimd", wo[n][:].rearrange("p (k n) -> p k n", k=16),
                     w_out[:, n * 512:(n + 1) * 512].rearrange("(k p) n -> p k n", p=128), writes=[wo[n]])
        self.dma("sync", self.BC[0][:], lng.partition_broadcast(128), writes=[self.BC[0]])
        self.dma("sync", self.BC[1][:], lnb.partition_broadcast(128), writes=[self.BC[1]])
        for t in range(T // 128):
            ob = self.BIGB[:, (t % 2) * 2048:(t % 2 + 1) * 2048]
            obk = ("ob", t % 2)
            self.dma("sync", ob, sc["o"][t * 128:(t + 1) * 128, :], reads=[("o", t)], writes=[obk])
            xt = self.f32view(self.BIGA, (t % 2) * 2048, 2048)
            xk = ("xt", t % 2)
            self.dma("sync", xt, x_ap[t * 128:(t + 1) * 128, :], writes=[xk])
            oT = self.BIGB[:, 4096 + (t % 2) * 2048:4096 + (t % 2 + 1) * 2048].rearrange("p (k n) -> p k n", k=16)
            oTk = ("oT", t % 2)
            for g in range(4):
                ps = self.PS[4 + g % 2]
                psb = ps.h.bitcast(BF16)
                for j in range(4):
                    k = g * 4 + j
                    P.op("tensor", lambda e, ob=ob, k=k, j=j, psb=psb: e.transpose(
                        out=psb[:, j * 128:(j + 1) * 128], in_=ob[:, k * 128:(k + 1) * 128], identity=self.identb[:]),
                        reads=[obk, self.identb], writes=[ps])
                P.op("scalar" if g % 2 else "vector",
                     (lambda e, oT=oT, psb=psb, g=g: e.activation(out=oT[:, g * 4:(g + 1) * 4, :], in_=psb[:, 0:512].rearrange("p (a b) -> p a b", a=4), func=AF.Copy))
                     if g % 2 else
                     (lambda e, oT=oT, psb=psb, g=g: e.tensor_copy(out=oT[:, g * 4:(g + 1) * 4, :], in_=psb[:, 0:512].rearrange("p (a b) -> p a b", a=4))),
                     reads=[ps], writes=[oTk])
            y = self.f32view(self.BIGA, 4096 + (t % 2) * 2048, 2048)
            yk = ("y", t % 2)
            for n in range(4):
                ps = self.PS[n]
                for k in range(16):
                    P.op("tensor", lambda e, n=n, k=k, oT=oT, ps=ps: e.matmul(
                        out=ps[:], lhsT=oT[:, k, :], rhs=wo[n][:, k * 512:(k + 1) * 512], start=(k == 0), stop=(k == 15)),
                        reads=[oTk, wo[n]], writes=[ps])
                P.op("vector", lambda e, n=n, ps=ps, y=y: e.tensor_tensor(
                    out=y[:, n * 512:(n + 1) * 512], in0=ps[:], in1=self.BC[2][:, n * 512:(n + 1) * 512], op=ALU.mult),
                    reads=[ps, self.BC[2]], writes=[yk])
            P.op("vector", lambda e, xt=xt, y=y: e.scalar_tensor_tensor(out=xt, in0=xt, scalar=ALPHA, in1=y, op0=ALU.mult, op1=ALU.add),
                 reads=[xk, yk], writes=[xk])
            self.layer_norm(xt, xk, self.BC[0], self.BC[1], y, yk)
            self.dma("sync", x1_ap[t * 128:(t + 1) * 128, :], xt, reads=[xk], writes=[("x1", t)])

    def moe(self, l, W, x1_ap, out_ap, outkey):
        P = self.P
        T = self.T
        TPASS = 512
        self.dma("sync", self.wr[:, :, 0:4], W["w_group"].rearrange("(k p) g -> p k g", p=128), writes=[self.wr])
        for g in range(4):
            self.dma("sync", self.wr[:, :, 4 + 8 * g:12 + 8 * g], W["w_router"][g].rearrange("(k p) e -> p k e", p=128), writes=[self.wr])
        self.dma("sync", self.brt[:, 0:4], W["b_group"].partition_broadcast(128), writes=[self.brt])
        self.dma("sync", self.brt[:, 4:36], W["b_router"].partition_broadcast(128), writes=[self.brt])
        ntile = TPASS // 128
        hT2 = self.BIGB[:, 0:16 * TPASS].rearrange("p (k t) -> p k t", k=16)
        accv = self.BIGA.h.bitcast(F32)
        for ps_i in range(T // TPASS):
            P.fence()
            tok0 = ps_i * TPASS
            router = dict(acc=lambda t: accv[:, t * 2048:(t + 1) * 2048], acckey="acc")
            self.make_hT(x1_ap[tok0:tok0 + TPASS, :], TPASS, hT2, "hT2", router=router)
            P.fence()
            for ex in range(32):
                g, e = ex // 8, ex % 8
                wg = self.W[0 + ex % 2]
                wu = self.W[2 + ex % 2]
                wd = self.W[4]
                wgv = wg[:].rearrange("p (k n) -> p k n", k=16)
                wuv = wu[:].rearrange("p (k n) -> p k n", k=16)
                wdv = wd[:].rearrange("p (k n) -> p k n", k=4)
                self.dma("gpsimd", wgv, W["w_gate"][g, e].rearrange("(k p) n -> p k n", p=128), writes=[wg])
                self.dma("gpsimd", wuv, W["w_up"][g, e].rearrange("(k p) n -> p k n", p=128), writes=[wu])
                self.dma("gpsimd", wdv, W["w_down"][g, e].rearrange("(k p) n -> p k n", p=128), writes=[wd])
                for k in range(4):
                    P.op("gpsimd", lambda e_, k=k, wdv=wdv: e_.tensor_tensor(out=wdv[:, k, :], in0=wdv[:, k, :], in1=self.BC[2][:], op=ALU.mult),
                         reads=[wd, self.BC[2]], writes=[wd])
                for tb in range(TPASS // 512):
                    hidb = self.rowhid(ex)
                    for fc in range(4):
                        psg = self.PS[fc % 2]
                        psu = self.PS[2 + fc % 2]
                        for k in range(16):
                            P.op("tensor", lambda e_, k=k, fc=fc, tb=tb, psg=psg, wgv=wgv: e_.matmul(
                                out=psg[:], lhsT=wgv[:, k, fc * 128:(fc + 1) * 128], rhs=hT2[:, k, tb * 512:(tb + 1) * 512],
                                start=(k == 0), stop=(k == 15)), reads=[wg] + [("hT2", tb * 4 + i) for i in range(4)], writes=[psg])
                        for k in range(16):
                            P.op("tensor", lambda e_, k=k, fc=fc, tb=tb, psu=psu, wuv=wuv: e_.matmul(
                                out=psu[:], lhsT=wuv[:, k, fc * 128:(fc + 1) * 128], rhs=hT2[:, k, tb * 512:(tb + 1) * 512],
                                start=(k == 0), stop=(k == 15)), reads=[wu] + [("hT2", tb * 4 + i) for i in range(4)], writes=[psu])
                        sg = self.sgbuf(fc % 2)
                        sgk = ("sg", fc % 2)
                        P.op("scalar", lambda e_, sg=sg, psg=psg: e_.activation(out=sg, in_=psg[:], func=AF.Silu), reads=[psg], writes=[sgk])
                        P.op("vector", lambda e_, sg=sg, psu=psu, hidb=hidb, fc=fc: e_.tensor_tensor(
                            out=hidb[:, fc, :], in0=psu[:], in1=sg, op=ALU.mult), reads=[psu, sgk], writes=[("hid", ex % 2, fc)])
                    for tt in range(4):
                        t = tb * 4 + tt
                        for dmb in range(4):
                            psd = self.PS[4 + (tt * 4 + dmb) % 4]
                            for fc in range(4):
                                P.op("tensor", lambda e_, fc=fc, tt=tt, dmb=dmb, psd=psd, hidb=hidb, wdv=wdv: e_.matmul(
                                    out=psd[:], lhsT=hidb[:, fc, tt * 128:(tt + 1) * 128], rhs=wdv[:, fc, dmb * 512:(dmb + 1) * 512],
                                    start=(fc == 0), stop=(fc == 3)), reads=[("hid", ex % 2, fc), wd], writes=[psd])
                            av = accv[:, t * 2048 + dmb * 512:t * 2048 + (dmb + 1) * 512]
                            P.op("vector", lambda e_, psd=psd, av=av, t=t, ex=ex: e_.scalar_tensor_tensor(
                                out=av, in0=psd[:], scalar=self.w32[:, t, ex:ex + 1], in1=av, op0=ALU.mult, op1=ALU.add),
                                reads=[psd, ("w32", t), ("acc", t)], writes=[("acc", t)])
            P.fence()
            self.dma("sync", self.f32view(self.W[0], 0, 2048), W["ln_g"].partition_broadcast(128), writes=[self.W[0]])
            self.dma("sync", self.f32view(self.W[1], 0, 2048), W["ln_b"].partition_broadcast(128), writes=[self.W[1]])
            gt = _View(self.f32view(self.W[0], 0, 2048), self.W[0].key)
            bt = _View(self.f32view(self.W[1], 0, 2048), self.W[1].key)
            for t in range(ntile):
                z = accv[:, t * 2048:(t + 1) * 2048]
                scr = self.f32view(self.W[2], (t % 2) * 2048, 2048)
                self.layer_norm(z, ("acc", t), gt, bt, scr, (self.W[2].key, "scr", t % 2))
                self.dma("sync", out_ap[tok0 + t * 128:tok0 + (t + 1) * 128, :], z, reads=[("acc", t)], writes=[(outkey, tok0 // 128 + t)])

    def rowhid(self, tb):
        o = 8192 + (tb % 2) * 2048
        return self.BIGB[:, o:o + 2048].rearrange("p (f t) -> p f t", f=4)

    def sgbuf(self, i):
        return self.BIGB.h.bitcast(F32)[:, 6144 + i * 512:6144 + (i + 1) * 512]


Buf.register = None


def _is_buf(x):
    return isinstance(x, (Buf, _View))


def _k(x):
    return x.key if isinstance(x, (Buf, _View)) else x


def build_program(T, TP, layers, ncores=8):
    B = Builder(T, TP, layers)
    nc, P = B.nc, B.P
    TC = T + TP
    B.load_consts()
    xo = B.din("xo", [T, D])
    xp = B.din("xp", [TP, D])
    cosT = B.din("c_cos", [128, TC])
    sinT = B.din("c_sin", [128, TC])
    out = nc.dram_tensor("out", [T, D], F32, kind="ExternalOutput").ap()
    sc = dict(
        qT=B.dscr("s_qT", [16, 128, T], BF16), kT=B.dscr("s_kT", [16, 128, TC], BF16),
        o=B.dscr("s_o", [T, D], BF16), cos=cosT, sin=sinT)
    vA = B.dscr("s_vA", [8, TC // 128, 128, 257], BF16)
    vB = B.dscr("s_vB", [16, TC // 128, 128, 129], BF16)
    x1 = B.dscr("s_x1", [T, D], F32)
    CH = 256
    nch = T // CH
    if len(layers) > 1:
        x2 = B.dscr("s_x2", [T, D], F32)
        gath = B.dscr("s_gath", [nch, 2 * CH, D], F32)
    cur_o, cur_p = xo, xp
    for li, l in enumerate(layers):
        kind = "A" if l % 2 == 0 else "B"
        last = (li == len(layers) - 1)
        sc["v"] = vA if kind == "A" else vB
        wmod = B.din("mix_mod_w%d" % l, [D, 6144])
        bmod = B.din("mix_mod_b%d" % l, [1, 6144])
        lng = B.din("mix_ln_g%d" % l, [1, D])
        lnb = B.din("mix_ln_b%d" % l, [1, D])
        lam_init = lamv = subw = fbias = None
        if kind == "A":
            w_in = B.din("a_w_in", [D, 6144])
            w_out = B.din("a_w_out", [D, D])
            lamv = [B.din("a_lam_q1", [1, 128]), B.din("a_lam_k1", [1, 128]),
                    B.din("a_lam_q2", [1, 128]), B.din("a_lam_k2", [1, 128])]
            subw = B.din("a_subln_w", [1, 256])
            lam_init = 0.8 - 0.6 * math.exp(-0.3 * l)
        else:
            w_in = B.din("b_w_in", [D, 6160])
            w_out = B.din("b_w_out", [D, D])
            fbias = B.din("b_forget_bias", [1, 16])
        Wm = dict(
            w_group=B.din("moe_w_group%d" % l, [D, 4]), b_group=B.din("moe_b_group%d" % l, [1, 4]),
            w_router=B.din("moe_w_router%d" % l, [4, D, 8]), b_router=B.din("moe_b_router%d" % l, [1, 32]),
            w_gate=B.din("moe_w_gate%d" % l, [4, 8, D, 512]), w_up=B.din("moe_w_up%d" % l, [4, 8, D, 512]),
            w_down=B.din("moe_w_down%d" % l, [4, 8, 512, D]),
            ln_g=B.din("ffn_ln_g%d" % l, [1, D]), ln_b=B.din("ffn_ln_b%d" % l, [1, D]))
        fwmod = B.din("ffn_mod_w%d" % l, [D, 6144])
        fbmod = B.din("ffn_mod_b%d" % l, [1, 6144])

        P.fence()
        B.modulation(wmod, bmod)
        B.mixer_params(kind, lam_init, lamv, subw, fbias, w_in)
        hT = B.BIGA[:, 0:16 * max(T, TP)].rearrange("p (k t) -> p k t", k=16)
        for (x_ap, ntok, tok0, with_q) in ((cur_p, TP, 0, False), (cur_o, T, TP, True)):
            P.fence()
            B.make_hT(x_ap, ntok, hT[:, :, 0:ntok], "hT")
            P.fence()
            B.qkv(l, kind, w_in, hT[:, :, 0:ntok], "hT", ntok, tok0, with_q, sc)
        P.fence()
        B.attention(l, kind, sc)
        P.fence()
        B.out_proj(l, w_out, cur_o, lng, lnb, sc, x1)
        P.fence()
        B.modulation(fwmod, fbmod)
        B.moe(l, Wm, x1, out if last else x2, "out" if last else "x2")
        if not last:
            P.fence()
            groups = [[2 * i, 2 * i + 1] for i in range(ncores // 2)]
            for ch in range(nch):
                if os.environ.get("K_NOCC"):
                    B.dma("sync", gath[ch, 0:CH, :], x2[ch * CH:(ch + 1) * CH, :], writes=[("gath", ch)])
                    continue
                P.op("gpsimd", lambda e, ch=ch: e.collective_compute(
                    "AllGather", ALU.bypass, replica_groups=groups,
                    ins=[x2[ch * CH:(ch + 1) * CH, :]], outs=[gath[ch]]),
                    writes=[("gath", ch)], dma=True, cc=True)
            P.fence()
            cur_o = x2
            cur_p = (lambda t: gath[t // 2, (t % 2) * 128:(t % 2 + 1) * 128, :])
    P.fence()
    P.op("sync", None, reads=[])
    P.emit()
    return nc, B


def _consts(T, TP, p):
    TC = T + TP
    ident = np.eye(128, dtype=np.float32)
    tri = np.triu(np.ones((128, 128), np.float32))
    R = np.zeros((128, 128), np.float32)
    for i in range(64):
        R[i, i + 64] = -1.0
        R[i + 64, i] = 1.0
    rotT = np.ascontiguousarray(R.T)
    pos = np.concatenate([np.arange(TP), p * T + np.arange(T)]).astype(np.float32)
    inv = np.power(np.float32(10000.0), -np.arange(0, 128, 2, dtype=np.float32) / 128).astype(np.float32)
    ang = pos[None, :] * np.concatenate([inv, inv])[:, None]
    return dict(c_ident=ident, c_tri=tri, c_rotT=rotT,
                c_pbias=np.full((128, 1), 0.0 if p == 1 else NEG, np.float32),
                c_cos=np.cos(ang).astype(np.float32), c_sin=np.sin(ang).astype(np.float32))


_PROG_CACHE = {}
_RUN_KW = {}
_LAST = {}


def run_layers(layers, x_in, inp, T, TP, ncores):
    key = (tuple(layers), T, TP, ncores)
    if key not in _PROG_CACHE:
        _PROG_CACHE[key] = build_program(T, TP, list(layers), ncores)
    nc, B = _PROG_CACHE[key]
    f = np.float32
    shared = {}
    for l in layers:
        j = l // 2
        shared["mix_mod_w%d" % l] = inp["mix_mod_w"][l]
        shared["mix_mod_b%d" % l] = inp["mix_mod_b"][l].reshape(1, -1)
        shared["mix_ln_g%d" % l] = inp["mix_ln_g"][l].reshape(1, -1)
        shared["mix_ln_b%d" % l] = inp["mix_ln_b"][l].reshape(1, -1)
        if l % 2 == 0:
            shared["a_w_in"] = inp["a_w_in"][j]
            shared["a_w_out"] = inp["a_w_out"][j]
            shared["a_lam_q1"] = inp["a_lam_q1"][j].reshape(1, -1)
            shared["a_lam_k1"] = inp["a_lam_k1"][j].reshape(1, -1)
            shared["a_lam_q2"] = inp["a_lam_q2"][j].reshape(1, -1)
            shared["a_lam_k2"] = inp["a_lam_k2"][j].reshape(1, -1)
            shared["a_subln_w"] = inp["a_subln_w"][j].reshape(1, -1)
        else:
            shared["b_w_in"] = inp["b_w_in"][j]
            shared["b_w_out"] = inp["b_w_out"][j]
            shared["b_forget_bias"] = inp["b_forget_bias"][j].reshape(1, -1)
        shared["ffn_mod_w%d" % l] = inp["ffn_mod_w"][l]
        shared["ffn_mod_b%d" % l] = inp["ffn_mod_b"][l].reshape(1, -1)
        shared["ffn_ln_g%d" % l] = inp["ffn_ln_g"][l].reshape(1, -1)
        shared["ffn_ln_b%d" % l] = inp["ffn_ln_b"][l].reshape(1, -1)
        shared["moe_w_group%d" % l] = inp["moe_w_group"][l]
        shared["moe_b_group%d" % l] = inp["moe_b_group"][l].reshape(1, -1)
        shared["moe_w_router%d" % l] = inp["moe_w_router"][l]
        shared["moe_b_router%d" % l] = inp["moe_b_router"][l].reshape(1, -1)
        shared["moe_w_gate%d" % l] = inp["moe_w_gate"][l]
        shared["moe_w_up%d" % l] = inp["moe_w_up"][l]
        shared["moe_w_down%d" % l] = inp["moe_w_down"][l]
    shared = {k: np.ascontiguousarray(np.asarray(v, dtype=f)) for k, v in shared.items() if k in B.inputs}
    in_maps = []
    for c in range(ncores):
        b, p = c // 2, c % 2
        m = _consts(T, TP, p)
        m["xo"] = x_in[b, p * T:(p + 1) * T]
        m["xp"] = x_in[b, 0:TP]
        m["cvec_in"] = inp["c"][b].reshape(16, 128).T
        m = {k: np.ascontiguousarray(np.asarray(v, dtype=f)) for k, v in m.items() if k in B.inputs}
        m.update(shared)
        in_maps.append(m)
    res = run_bass_kernel_spmd(nc, in_maps, core_ids=list(range(ncores)), **_RUN_KW)
    _LAST['res'] = res
    out = np.empty_like(x_in)
    for c in range(ncores):
        b, p = c // 2, c % 2
        out[b, p * T:(p + 1) * T] = res.results[c]["out"]
    return out


def run_layer(l, x_in, inp, T, TP, ncores, stop=99):
    return run_layers([l], x_in, inp, T, TP, ncores)


def kernel(**inputs):
    inp = {k: np.asarray(v) for k, v in inputs.items()}
    x = np.asarray(inp["x"], dtype=np.float32)
    Bn, S, _ = x.shape
    T = S // 2
    ncores = Bn * 2
    return run_layers(list(range(DEPTH)), x, inp, T, T, ncores)
```

```python
import math
import os
import numpy as np
import concourse.bass as bass
import concourse.mybir as mybir
from concourse.bass_utils import run_bass_kernel_spmd

F32 = mybir.dt.float32
BF16 = mybir.dt.bfloat16
AF = mybir.ActivationFunctionType
ALU = mybir.AluOpType
AX = mybir.AxisListType

D = 2048
KC = 16
DEPTH = 2
ALPHA = (2.0 * DEPTH) ** 0.25
LN_EPS = 1e-5
RMS_EPS = 1e-5
NEG = -30000.0
COMPUTE = ("tensor", "vector", "scalar", "gpsimd")
NDMASEM = 8


class Buf:
    _n = 0

    def __init__(self, h, name, psum=False):
        self.h = h
        self.psum = psum
        Buf._n += 1
        self.key = name + "#" + str(Buf._n)

    def __getitem__(self, idx):
        return self.h[idx]


def _k(x):
    return x.key if isinstance(x, Buf) else x


class _View:
    def __init__(self, ap, key):
        self.ap = ap
        self.key = key

    def __getitem__(self, idx):
        return self.ap


class Prog:
    def __init__(self, nc):
        self.nc = nc
        self.ops = []
        self.last_w = {}
        self.readers = {}
        self.last_eng = {}
        self.last_dma = {}
        self.cc_ops = []

    def sbuf(self, name, shape, dtype):
        return Buf(self.nc.alloc_sbuf_tensor(name, list(shape), dtype), name)

    def psum(self, name, shape, dtype=F32):
        return Buf(self.nc.alloc_psum_tensor(name, list(shape), dtype), name, psum=True)

    def op(self, eng, fn, reads=(), writes=(), dma=False, cc=False):
        i = len(self.ops)
        deps = set()
        pr = [r for r in reads if isinstance(r, Buf) and r.psum]
        if pr:
            reads = [r for r in reads if not (isinstance(r, Buf) and r.psum)]
            writes = list(writes) + [r for r in pr if r not in writes]
        for r in reads:
            k = _k(r)
            if k in self.last_w:
                deps.add(self.last_w[k])
        for w in writes:
            k = _k(w)
            if k in self.last_w:
                deps.add(self.last_w[k])
            for rd in self.readers.get(k, ()):
                deps.add(rd)
        for r in reads:
            self.readers.setdefault(_k(r), []).append(i)
        for w in writes:
            k = _k(w)
            self.last_w[k] = i
            self.readers[k] = []
        deps.discard(i)
        self.ops.append(dict(eng=eng, fn=fn, deps=deps, dma=dma, signal=False, cc=cc))
        if cc:
            self.cc_ops.append(i)
        elif dma:
            self.last_dma.setdefault(eng, []).append(i)
            self.last_dma[eng] = self.last_dma[eng][-NDMASEM:]
        else:
            self.last_eng[eng] = i
        return i

    def fence(self):
        deps = set(self.last_eng.values())
        for v in self.last_dma.values():
            deps.update(v)
        deps.update(self.cc_ops[-1:])
        for e in COMPUTE + ("sync",):
            self.ops.append(dict(eng=e, fn=None, deps=set(deps), dma=False, signal=False, cc=False))
        self.last_w = {}
        self.readers = {}

    def emit(self):
        nc = self.nc
        ops = self.ops

        def dom(o):
            if o["cc"]:
                return "cc"
            return ("dma", o["eng"]) if o["dma"] else o["eng"]

        ccsem = nc.alloc_semaphore(name="s_cc") if self.cc_ops else None
        ncc = 0
        for o in ops:
            if o["cc"]:
                ncc += 1
            o["ncc_before"] = ncc
        for o in ops:
            for d in o["deps"]:
                od = ops[d]
                if od["cc"]:
                    continue
                if (not od["dma"]) and (not o["dma"]) and od["eng"] == o["eng"] == "tensor":
                    continue
                od["signal"] = True
        cnt = {}
        dma_idx = {}
        for o in ops:
            dm = dom(o)
            if o["cc"]:
                continue
            if o["dma"]:
                n = dma_idx.get(dm, 0)
                o["dslot"] = n % NDMASEM
                o["dround"] = n // NDMASEM
                dma_idx[dm] = n + 1
            elif o["signal"] and o["fn"] is not None:
                cnt[dm] = cnt.get(dm, 0) + 1
                o["cnt"] = cnt[dm]
        sems = {e: nc.alloc_semaphore(name="s_" + e) for e in COMPUTE}
        dsems = {dm: [nc.alloc_semaphore(name="d_%s_%d" % (dm[1], k)) for k in range(NDMASEM)]
                 for dm in dma_idx}
        streams = {}
        for i, o in enumerate(ops):
            streams.setdefault(o["eng"], []).append(i)

        def run_stream(eng_name, engine):
            waited = {}

            def wait(sem, key, val):
                if waited.get(key, -1) >= val:
                    return
                engine.wait_ge(sem, val)
                waited[key] = val

            for i in streams.get(eng_name, []):
                o = ops[i]
                for d in sorted(o["deps"]):
                    od = ops[d]
                    if od["cc"]:
                        wait(ccsem, "cc", o["ncc_before"] - (1 if o["cc"] else 0))
                        continue
                    if od["dma"]:
                        dm = dom(od)
                        wait(dsems[dm][od["dslot"]], (dm, od["dslot"]), 16 * (od["dround"] + 1))
                    else:
                        if "cnt" not in od:
                            continue
                        if od["eng"] == eng_name == "tensor" and not o["dma"]:
                            continue
                        wait(sems[od["eng"]], od["eng"], od["cnt"])
                if o["dma"] and not o["cc"] and o["dround"] > 0:
                    dm = dom(o)
                    wait(dsems[dm][o["dslot"]], (dm, o["dslot"]), 16 * o["dround"])
                if o["fn"] is None:
                    continue
                ins = o["fn"](engine)
                if o["cc"]:
                    ins.then_inc(ccsem, 1)
                elif o["dma"]:
                    ins.then_inc(dsems[dom(o)][o["dslot"]], 16)
                elif "cnt" in o:
                    ins.then_inc(sems[eng_name], 1)

        with nc.Block() as block:
            @block.tensor
            def _(e):
                run_stream("tensor", e)

            @block.vector
            def _(e):
                run_stream("vector", e)

            @block.scalar
            def _(e):
                run_stream("scalar", e)

            @block.gpsimd
            def _(e):
                run_stream("gpsimd", e)

            @block.sync
            def _(e):
                run_stream("sync", e)


class Builder:
    def __init__(self, T, TP, layers, n_exp_groups=4):
        self.T, self.TP, self.TC = T, TP, T + TP
        self.layers = layers
        nc = self.nc = bass.Bass("TRN2", target_bir_lowering=False)
        P = self.P = Prog(nc)
        self.inputs = {}
        self.BIGA = P.sbuf("BIGA", [128, 32768], BF16)
        self.BIGB = P.sbuf("BIGB", [128, 16384], BF16)
        self.W = [P.sbuf("W%d" % i, [128, 8192], BF16) for i in range(5)]
        self.BC = [P.sbuf("BC%d" % i, [128, 2048], F32) for i in range(3)]
        self.ident = P.sbuf("ident", [128, 128], F32)
        self.identb = P.sbuf("identb", [128, 128], BF16)
        self.tri = P.sbuf("tri", [128, 128], F32)
        self.trib = P.sbuf("trib", [128, 128], BF16)
        self.rotT = P.sbuf("rotT", [128, 128], BF16)
        self.ones = P.sbuf("ones", [128, 128], F32)
        self.pbias = P.sbuf("pbias", [128, 1], F32)
        self.zero = P.sbuf("zero", [128, 1], F32)
        self.cvec = P.sbuf("cvec", [128, 16], F32)
        self.rowbuf = _View(self.BIGA.h.bitcast(F32)[0:1, 0:6144], self.BIGA.key)
        self.small = P.sbuf("small", [128, 64], F32)
        self.lam = P.sbuf("lam", [128, 8], F32)
        self.subw = P.sbuf("subw", [128, 256], F32)
        self.w32 = P.sbuf("w32", [128, 8, 32], F32)
        self.rt = P.sbuf("rt", [128, 128], F32)
        self.wr = P.sbuf("wr", [128, 16, 36], F32)
        self.brt = P.sbuf("brt", [128, 36], F32)
        bf = self.BIGB.h.bitcast(F32)
        self.lf = bf[:, 0:512].rearrange("p (t h) -> p t h", h=16)
        self.cum = bf[:, 512:1024].rearrange("p (t h) -> p t h", h=16)
        self.anc = bf[:, 1024:1088].rearrange("p (t h) -> p t h", h=16)
        self.fb = P.sbuf("fb", [128, 16], F32)
        self.wf = _View(self.BIGB[:, 4096:4352].rearrange("p (k n) -> p k n", k=16), "wfkey")
        self.PS = [P.psum("ps%d" % i, [128, 512], F32) for i in range(8)]

    def din(self, name, shape, dt=F32):
        t = self.nc.dram_tensor(name, list(shape), dt, kind="ExternalInput").ap()
        self.inputs[name] = t
        return t

    def dscr(self, name, shape, dt):
        return self.nc.dram_tensor(name, list(shape), dt).ap()

    def dma(self, eng, out, in_, reads=(), writes=()):
        self.P.op(eng, lambda e: e.dma_start(out=out, in_=in_), reads=reads, writes=writes, dma=True)

    def f32view(self, buf, off_f32, n):
        return buf.h.bitcast(F32)[:, off_f32:off_f32 + n]

    def load_consts(self):
        P = self.P
        c_ident = self.din("c_ident", [128, 128])
        c_tri = self.din("c_tri", [128, 128])
        c_rotT = self.din("c_rotT", [128, 128])
        c_pbias = self.din("c_pbias", [128, 1])
        self.dma("sync", self.ident[:], c_ident, writes=[self.ident])
        self.dma("sync", self.tri[:], c_tri, writes=[self.tri])
        self.dma("sync", self.pbias[:], c_pbias, writes=[self.pbias])
        self.dma("gpsimd", self.identb[:], c_ident, writes=[self.identb])
        self.dma("gpsimd", self.trib[:], c_tri, writes=[self.trib])
        self.dma("gpsimd", self.rotT[:], c_rotT, writes=[self.rotT])
        P.op("vector", lambda e: e.memset(self.ones[:], 1.0), writes=[self.ones])
        P.op("vector", lambda e: e.memset(self.zero[:], 0.0), writes=[self.zero])
        cv = self.din("cvec_in", [128, 16])
        self.dma("sync", self.cvec[:], cv, writes=[self.cvec])
        P.op("scalar", lambda e: e.activation(out=self.cvec[:], in_=self.cvec[:], func=AF.Silu),
             reads=[self.cvec], writes=[self.cvec])

    def modulation(self, wmod, bmod):
        P = self.P
        wst = [self.f32view(self.W[0], 0, 4096), self.f32view(self.W[1], 0, 4096),
               self.f32view(self.W[2], 0, 4096), self.f32view(self.W[3], 0, 4096)]
        wk = [self.W[0], self.W[1], self.W[2], self.W[3]]
        ps = self.PS[0]
        self.dma("sync", self.rowbuf.ap, bmod, writes=[self.rowbuf])
        for n in range(12):
            a, b = (0, 1) if n % 2 == 0 else (2, 3)
            src = wmod[:, n * 512:(n + 1) * 512].rearrange("(k p) n -> p k n", p=128)
            self.dma("sync", wst[a].rearrange("p (k n) -> p k n", k=8), src[:, 0:8, :], writes=[wk[a]])
            self.dma("sync", wst[b].rearrange("p (k n) -> p k n", k=8), src[:, 8:16, :], writes=[wk[b]])
            for k in range(16):
                wb = a if k < 8 else b
                kk = k % 8
                P.op("tensor", lambda e, wb=wb, kk=kk, k=k: e.matmul(
                    out=ps[0:1, :], lhsT=self.cvec[:, k:k + 1], rhs=wst[wb][:, kk * 512:(kk + 1) * 512],
                    start=(k == 0), stop=(k == 15)), reads=[self.cvec, wk[wb]], writes=[ps])
            addc = 0.0 if n < 4 else 1.0
            P.op("vector", lambda e, n=n, addc=addc: e.scalar_tensor_tensor(
                out=self.rowbuf.ap[0:1, n * 512:(n + 1) * 512], in0=ps[0:1, :], scalar=addc,
                in1=self.rowbuf.ap[0:1, n * 512:(n + 1) * 512], op0=ALU.add, op1=ALU.add),
                reads=[ps, self.rowbuf], writes=[self.rowbuf])
        self.bcast_mod()

    def bcast_mod(self):
        P = self.P
        for n in range(12):
            dst = [self.BC[1], self.BC[0], self.BC[2]][n // 4]
            psb = self.PS[1 + n % 2]
            P.op("tensor", lambda e, n=n, psb=psb: e.matmul(
                out=psb[:], lhsT=self.ones[0:1, :], rhs=self.rowbuf.ap[0:1, n * 512:(n + 1) * 512],
                start=True, stop=True), reads=[self.ones, self.rowbuf], writes=[psb])
            P.op("scalar", lambda e, n=n, psb=psb, dst=dst: e.activation(
                out=dst[:, (n % 4) * 512:(n % 4 + 1) * 512], in_=psb[:], func=AF.Copy),
                reads=[psb], writes=[dst])

    def make_hT(self, x_ap, ntok, hT, hT_key, router=None):
        P = self.P
        xt = [self.f32view(self.W[0], 0, 2048), self.f32view(self.W[1], 0, 2048)]
        xk = [self.W[0], self.W[1]]
        htf = self.f32view(self.W[2], 0, 2048)
        for t in range(ntok // 128):
            xb, xkk = xt[t % 2], xk[t % 2]
            self.dma("sync", xb, (x_ap(t) if callable(x_ap) else x_ap[t * 128:(t + 1) * 128, :]), writes=[xkk])
            if router is not None and "acc" in router:
                acc = router["acc"](t)
                P.op("scalar", lambda e, xb=xb, acc=acc: e.activation(out=acc, in_=xb, func=AF.Copy, scale=ALPHA),
                     reads=[xkk], writes=[(router["acckey"], t)])
            P.op("vector", lambda e, xb=xb: e.tensor_tensor(out=xb, in0=xb, in1=self.BC[0][:], op=ALU.mult),
                 reads=[xkk, self.BC[0]], writes=[xkk])
            P.op("vector", lambda e, xb=xb: e.tensor_tensor(out=xb, in0=xb, in1=self.BC[1][:], op=ALU.add),
                 reads=[xkk, self.BC[1]], writes=[xkk])
            for g in range(4):
                ps = self.PS[g % 4]
                for j in range(4):
                    k = g * 4 + j
                    P.op("tensor", lambda e, xb=xb, k=k, j=j, ps=ps: e.transpose(
                        out=ps[:, j * 128:(j + 1) * 128], in_=xb[:, k * 128:(k + 1) * 128], identity=self.ident[:]),
                        reads=[xkk, self.ident], writes=[ps])
                eng = "vector" if g % 2 == 0 else "scalar"
                dst = hT[:, g * 4:(g + 1) * 4, t * 128:(t + 1) * 128]
                src = ps[:].rearrange("p (a b) -> p a b", a=4)
                if eng == "vector":
                    P.op("vector", lambda e, dst=dst, src=src: e.tensor_copy(out=dst, in_=src),
                         reads=[ps], writes=[(hT_key, t)])
                else:
                    P.op("scalar", lambda e, dst=dst, src=src: e.activation(out=dst, in_=src, func=AF.Copy),
                         reads=[ps], writes=[(hT_key, t)])
                if router is not None:
                    P.op("gpsimd" if False else "vector", lambda e, src=src, g=g: e.tensor_copy(
                        out=htf[:, g * 512:(g + 1) * 512].rearrange("p (a b) -> p a b", a=4), in_=src),
                        reads=[ps], writes=[self.W[2]])
            if router is not None:
                self.route(t, htf, router)

    def route(self, t, htf, router):
        P = self.P
        ps = self.PS[4]
        for k in range(16):
            P.op("tensor", lambda e, k=k: e.matmul(out=ps[:, 0:36], lhsT=htf[:, k * 128:(k + 1) * 128],
                                                   rhs=self.wr[:, k, :], start=(k == 0), stop=(k == 15)),
                 reads=[self.W[2], self.wr], writes=[ps])
        rt = self.rt
        V = "vector"

        def v(fn, reads=(), writes=()):
            P.op(V, fn, reads=[rt] + list(reads), writes=[rt] + list(writes))
        v(lambda e: e.tensor_tensor(out=rt[:, 0:36], in0=ps[:, 0:36], in1=self.brt[:], op=ALU.add), reads=[ps, self.brt])
        v(lambda e: e.reduce_max(out=rt[:, 36:37], in_=rt[:, 0:4], axis=AX.X))
        v(lambda e: e.tensor_scalar(out=rt[:, 40:44], in0=rt[:, 0:4], scalar1=rt[:, 36:37], scalar2=None, op0=ALU.is_ge))
        v(lambda e: e.tensor_scalar(out=rt[:, 44:48], in0=rt[:, 0:4], scalar1=rt[:, 36:37], scalar2=None, op0=ALU.subtract))
        P.op("scalar", lambda e: e.activation(out=rt[:, 44:48], in_=rt[:, 44:48], func=AF.Exp, accum_out=rt[:, 37:38]),
             reads=[rt], writes=[rt])
        v(lambda e: e.reciprocal(out=rt[:, 38:39], in_=rt[:, 37:38]))
        v(lambda e: e.tensor_scalar(out=rt[:, 48:56], in0=rt[:, 4:12], scalar1=rt[:, 40:41], scalar2=None, op0=ALU.mult))
        for g in range(1, 4):
            v(lambda e, g=g: e.scalar_tensor_tensor(out=rt[:, 48:56], in0=rt[:, 4 + 8 * g:12 + 8 * g],
                                                    scalar=rt[:, 40 + g:41 + g], in1=rt[:, 48:56],
                                                    op0=ALU.mult, op1=ALU.add))
        v(lambda e: e.reduce_max(out=rt[:, 56:57], in_=rt[:, 48:56], axis=AX.X))
        v(lambda e: e.tensor_scalar(out=rt[:, 64:72], in0=rt[:, 48:56], scalar1=rt[:, 56:57], scalar2=None, op0=ALU.is_ge))
        v(lambda e: e.scalar_tensor_tensor(out=rt[:, 72:80], in0=rt[:, 64:72], scalar=NEG, in1=rt[:, 48:56],
                                           op0=ALU.mult, op1=ALU.add))
        v(lambda e: e.reduce_max(out=rt[:, 57:58], in_=rt[:, 72:80], axis=AX.X))
        v(lambda e: e.tensor_scalar(out=rt[:, 80:88], in0=rt[:, 72:80], scalar1=rt[:, 57:58], scalar2=None, op0=ALU.is_ge))
        v(lambda e: e.tensor_tensor(out=rt[:, 58:59], in0=rt[:, 57:58], in1=rt[:, 56:57], op=ALU.subtract))
        P.op("scalar", lambda e: e.activation(out=rt[:, 59:60], in_=rt[:, 58:59], func=AF.Exp), reads=[rt], writes=[rt])
        v(lambda e: e.tensor_scalar(out=rt[:, 60:61], in0=rt[:, 59:60], scalar1=1.0, scalar2=None, op0=ALU.add))
        v(lambda e: e.reciprocal(out=rt[:, 61:62], in_=rt[:, 60:61]))
        v(lambda e: e.tensor_tensor(out=rt[:, 62:63], in0=rt[:, 59:60], in1=rt[:, 61:62], op=ALU.mult))
        v(lambda e: e.tensor_scalar(out=rt[:, 64:72], in0=rt[:, 64:72], scalar1=rt[:, 61:62], scalar2=None, op0=ALU.mult))
        v(lambda e: e.scalar_tensor_tensor(out=rt[:, 64:72], in0=rt[:, 80:88], scalar=rt[:, 62:63], in1=rt[:, 64:72],
                                           op0=ALU.mult, op1=ALU.add))
        v(lambda e: e.tensor_scalar(out=rt[:, 64:72], in0=rt[:, 64:72], scalar1=rt[:, 38:39], scalar2=None, op0=ALU.mult))
        for g in range(4):
            P.op(V, lambda e, g=g: e.tensor_scalar(out=self.w32[:, t, g * 8:(g + 1) * 8], in0=rt[:, 64:72],
                                                   scalar1=rt[:, 40 + g:41 + g], scalar2=None, op0=ALU.mult),
                 reads=[rt], writes=[("w32", t)])

    def layer_norm(self, z, zkey, gt, bt, scr, scrkey):
        P = self.P
        st = self.small
        P.op("vector", lambda e: e.reduce_sum(out=st[:, 0:1], in_=z, axis=AX.X), reads=[zkey], writes=[st])
        P.op("vector", lambda e: e.tensor_scalar(out=st[:, 1:2], in0=st[:, 0:1], scalar1=-1.0 / D, scalar2=None, op0=ALU.mult),
             reads=[st], writes=[st])
        P.op("scalar", lambda e: e.activation(out=scr, in_=z, func=AF.Square, bias=st[:, 1:2], scale=1.0, accum_out=st[:, 2:3]),
             reads=[zkey, st], writes=[scrkey, st])
        P.op("vector", lambda e: e.tensor_scalar(out=st[:, 3:4], in0=st[:, 2:3], scalar1=1.0 / D, scalar2=LN_EPS, op0=ALU.mult, op1=ALU.add),
             reads=[st], writes=[st])
        P.op("scalar", lambda e: e.activation(out=st[:, 5:6], in_=st[:, 3:4], func=AF.Sqrt), reads=[st], writes=[st])
        P.op("vector", lambda e: e.reciprocal(out=st[:, 4:5], in_=st[:, 5:6]), reads=[st], writes=[st])
        P.op("vector", lambda e: e.tensor_scalar(out=z, in0=z, scalar1=st[:, 1:2], scalar2=st[:, 4:5], op0=ALU.add, op1=ALU.mult),
             reads=[zkey, st], writes=[zkey])
        P.op("vector", lambda e: e.tensor_tensor(out=z, in0=z, in1=gt[:], op=ALU.mult), reads=[zkey, gt], writes=[zkey])
        P.op("vector", lambda e: e.tensor_tensor(out=z, in0=z, in1=bt[:], op=ALU.add), reads=[zkey, bt], writes=[zkey])

    def mixer_params(self, kind, lam_init, lamv, subw, fbias, w_in):
        B = self
        P = self.P
        if kind == "A":
            lt = [B.f32view(B.W[4], i * 128, 128) for i in range(4)]
            for i in range(4):
                B.dma("sync", lt[i], lamv[i].partition_broadcast(128), writes=[B.W[4]])
            P.op("vector", lambda e: e.tensor_tensor(out=lt[0], in0=lt[0], in1=lt[1], op=ALU.mult), reads=[B.W[4]], writes=[B.W[4]])
            P.op("vector", lambda e: e.tensor_tensor(out=lt[2], in0=lt[2], in1=lt[3], op=ALU.mult), reads=[B.W[4]], writes=[B.W[4]])
            P.op("vector", lambda e: e.reduce_sum(out=B.lam[:, 2:3], in_=lt[0], axis=AX.X), reads=[B.W[4]], writes=[B.lam])
            P.op("vector", lambda e: e.reduce_sum(out=B.lam[:, 3:4], in_=lt[2], axis=AX.X), reads=[B.W[4]], writes=[B.lam])
            P.op("scalar", lambda e: e.activation(out=B.lam[:, 4:6], in_=B.lam[:, 2:4], func=AF.Exp), reads=[B.lam], writes=[B.lam])
            P.op("vector", lambda e: e.tensor_tensor(out=B.lam[:, 0:1], in0=B.lam[:, 4:5], in1=B.lam[:, 5:6], op=ALU.subtract), reads=[B.lam], writes=[B.lam])
            P.op("vector", lambda e: e.tensor_scalar(out=B.lam[:, 0:1], in0=B.lam[:, 0:1], scalar1=lam_init, scalar2=None, op0=ALU.add), reads=[B.lam], writes=[B.lam])
            P.op("vector", lambda e: e.tensor_scalar(out=B.lam[:, 1:2], in0=B.lam[:, 0:1], scalar1=-1.0, scalar2=None, op0=ALU.mult), reads=[B.lam], writes=[B.lam])
            B.dma("sync", B.subw[:], subw.partition_broadcast(128), writes=[B.subw])
            P.op("vector", lambda e: e.tensor_scalar(out=B.subw[:], in0=B.subw[:], scalar1=1.0 - lam_init, scalar2=None, op0=ALU.mult), reads=[B.subw], writes=[B.subw])
        else:
            B.dma("sync", B.fb[:], fbias.partition_broadcast(128), writes=[B.fb])
            B.dma("gpsimd", B.wf.ap, w_in[:, 6144:6160].rearrange("(k p) n -> p k n", p=128), writes=[B.wf])

    def qkv(self, l, kind, w_in, hT, hT_key, ntok, tok0, with_q, sc):
        P = self.P
        wb = [self.W[0], self.W[1]]
        nblk = 12
        first = 0 if with_q else 4
        bi = 0
        import os
        skip = os.environ.get("QKV_SKIP", "")
        for n in range(first, nblk):
            if (skip == "v" and n >= 8) or (skip == "qk" and n < 8):
                continue
            wt = wb[bi % 2]
            bi += 1
            wv = wt[:].rearrange("p (k n) -> p k n", k=16)
            self.dma("gpsimd", wv, w_in[:, n * 512:(n + 1) * 512].rearrange("(k p) n -> p k n", p=128), writes=[wt])
            if n < 8:
                isq = n < 4
                for tb in range(ntok // 512):
                    if kind == "A":
                        cs = [self.f32view(self.W[2], 0, 512), self.f32view(self.W[2], 512, 512)]
                        t0 = tok0 + tb * 512
                        self.dma("sync", cs[0], sc["cos"][:, t0:t0 + 512], writes=[(self.W[2].key, "c")])
                        self.dma("sync", cs[1], sc["sin"][:, t0:t0 + 512], writes=[(self.W[2].key, "s")])
                    for mi in range(4):
                        m = (n % 4) * 4 + mi
                        ps = self.PS[mi % 2]
                        for k in range(16):
                            P.op("tensor", lambda e, k=k, mi=mi, tb=tb, ps=ps, wv=wv: e.matmul(
                                out=ps[:], lhsT=wv[:, k, mi * 128:(mi + 1) * 128], rhs=hT[:, k, tb * 512:(tb + 1) * 512],
                                start=(k == 0), stop=(k == 15)), reads=[wt] + [(hT_key, tb * 4 + i) for i in range(4)], writes=[ps])
                        ob = self.W[3][:, (mi % 2) * 512:(mi % 2) * 512 + 512]
                        okey = (self.W[3].key, "o", mi % 2)
                        lvl = int(os.environ.get("QK_LEVEL", "9"))
                        if lvl == 0:
                            P.op("scalar", lambda e, ob=ob, ps=ps: e.activation(out=ob, in_=ps[:], func=AF.Copy),
                                 reads=[ps], writes=[okey])
                        elif kind == "A":
                            qraw = self.W[3][:, 1024 + (mi % 2) * 512:1024 + (mi % 2) * 512 + 512]
                            qkey = (self.W[3].key, "q", mi % 2)
                            ps2 = self.PS[2 + mi % 2]
                            t1 = self.f32view(self.W[3], 1024 + (mi % 2) * 1024, 512)
                            t2 = self.f32view(self.W[3], 1024 + (mi % 2) * 1024 + 512, 512)
                            tkey = (self.W[3].key, "t", mi % 2)
                            P.op("scalar", lambda e, qraw=qraw, ps=ps: e.activation(out=qraw, in_=ps[:], func=AF.Copy),
                                 reads=[ps], writes=[qkey])
                            P.op("tensor", lambda e, qraw=qraw, ps2=ps2: e.matmul(out=ps2[:], lhsT=self.rotT[:], rhs=qraw,
                                                                                   start=True, stop=True),
                                 reads=[qkey, self.rotT], writes=[ps2])
                            P.op("vector", lambda e, t1=t1, ps=ps, cs=cs: e.tensor_tensor(out=t1, in0=ps[:], in1=cs[0], op=ALU.mult),
                                 reads=[ps, (self.W[2].key, "c")], writes=[(tkey, 1)])
                            P.op("vector", lambda e, t2=t2, ps2=ps2, cs=cs: e.tensor_tensor(out=t2, in0=ps2[:], in1=cs[1], op=ALU.mult),
                                 reads=[ps2, (self.W[2].key, "s")], writes=[(tkey, 2)])
                            P.op("vector", lambda e, ob=ob, t1=t1, t2=t2: e.tensor_tensor(out=ob, in0=t1, in1=t2, op=ALU.add),
                                 reads=[(tkey, 1), (tkey, 2)], writes=[okey])
                        else:
                            P.op("scalar", lambda e, ob=ob, ps=ps: e.activation(out=ob, in_=ps[:], func=AF.Copy),
                                 reads=[ps], writes=[okey])
                        if isq:
                            dst = sc["qT"][m, :, tb * 512:(tb + 1) * 512]
                            dk = ("qT", m)
                        else:
                            dst = sc["kT"][m, :, tok0 + tb * 512:tok0 + (tb + 1) * 512]
                            dk = ("kT", m)
                        if os.environ.get("NO_STORE", "") != "1":
                            self.dma("sync", dst, ob, reads=[okey], writes=[dk])
            else:
                hv = 256 if kind == "A" else 128
                nh = 512 // hv
                for tt in range(ntok // 128):
                    ps = self.PS[4 + tt % 2]
                    for k in range(16):
                        P.op("tensor", lambda e, k=k, tt=tt, ps=ps, wv=wv: e.matmul(
                            out=ps[:], lhsT=hT[:, k, tt * 128:(tt + 1) * 128], rhs=wv[:, k, :],
                            start=(k == 0), stop=(k == 15)), reads=[wt, (hT_key, tt)], writes=[ps])
                    vb = self.W[2][:, 2048 + (tt % 2) * 1024:2048 + (tt % 2) * 1024 + nh * (hv + 1)].rearrange("p (h c) -> p h c", h=nh)
                    vkey = (self.W[2].key, "v", tt % 2)
                    P.op("scalar" if tt % 2 == 0 else "vector",
                         (lambda e, vb=vb, ps=ps: e.activation(out=vb[:, :, 0:hv], in_=ps[:].rearrange("p (h c) -> p h c", h=nh), func=AF.Copy))
                         if tt % 2 == 0 else
                         (lambda e, vb=vb, ps=ps: e.tensor_copy(out=vb[:, :, 0:hv], in_=ps[:].rearrange("p (h c) -> p h c", h=nh))),
                         reads=[ps], writes=[vkey])
                    P.op("gpsimd", lambda e, vb=vb: e.memset(vb[:, :, hv:hv + 1], 1.0), writes=[vkey])
                    h0 = (n - 8) * nh
                    ct = (tok0 // 128) + tt
                    self.dma("sync", sc["v"][h0:h0 + nh, ct, :, :].rearrange("h p c -> p h c"), vb, reads=[vkey], writes=[("v", h0)])
        if kind == "B":
            P.op("sync", None, reads=[self.wf])
            for tt in range(ntok // 128):
                ps = self.PS[6]
                ct = (tok0 // 128) + tt
                for k in range(16):
                    P.op("tensor", lambda e, k=k, tt=tt: e.matmul(out=ps[:, 0:16], lhsT=hT[:, k, tt * 128:(tt + 1) * 128],
                                                                  rhs=self.wf.ap[:, k, :], start=(k == 0), stop=(k == 15)),
                         reads=[self.wf, (hT_key, tt)], writes=[ps])
                st = self.small
                P.op("vector", lambda e: e.tensor_tensor(out=st[:, 16:32], in0=ps[:, 0:16], in1=self.fb[:], op=ALU.add),
                     reads=[ps, self.fb], writes=[st])
                P.op("scalar", lambda e: e.activation(out=st[:, 32:48], in_=st[:, 16:32], func=AF.Exp, scale=-1.0), reads=[st], writes=[st])
                P.op("scalar", lambda e: e.activation(out=st[:, 32:48], in_=st[:, 32:48], func=AF.Ln, bias=1.0, scale=1.0), reads=[st], writes=[st])
                P.op("vector", lambda e, ct=ct: e.tensor_scalar(out=self.lf[:, ct, :], in0=st[:, 32:48], scalar1=-1.0, scalar2=None, op0=ALU.mult),
                     reads=[st], writes=[("lf", ct)])

    def attention(self, l, kind, sc):
        P = self.P
        T, TP, TC = self.T, self.TP, self.TC
        nprev = TP // 128
        nq = T // 512
        scale = 128 ** -0.5
        if kind == "A":
            nheads, nmaps, hv = 8, 2, 256
        else:
            nheads, nmaps, hv = 16, 1, 128
        hv1 = hv + 1
        kTs = [self.BIGA[:, m * TC:(m + 1) * TC] for m in range(nmaps)]
        o0 = nmaps * TC
        qTs = [self.BIGA[:, o0 + m * T:o0 + (m + 1) * T] for m in range(nmaps)]
        o1 = o0 + nmaps * T
        v1 = self.BIGA[:, o1:o1 + (TC // 128) * hv1].rearrange("p (t c) -> p t c", c=hv1)
        assert o1 + (TC // 128) * hv1 <= 32768
        if kind == "B":
            ps = self.PS[7]
            for t in range(TC // 128):
                for t2 in range(t + 1):
                    P.op("tensor", lambda e, t=t, t2=t2: e.matmul(
                        out=ps[:, 0:16], lhsT=(self.tri[:] if t2 == t else self.ones[:]), rhs=self.lf[:, t2, :],
                        start=(t2 == 0), stop=(t2 == t)), reads=[("lf", t2), self.tri, self.ones], writes=[ps])
                P.op("vector", lambda e, t=t: e.tensor_copy(out=self.cum[:, t, :], in_=ps[:, 0:16]), reads=[ps], writes=[("cum", t)])
            for g in range(nq):
                ta = (TP + g * 512 + 256) // 128
                for t2 in range(ta):
                    P.op("tensor", lambda e, t2=t2, ta=ta: e.matmul(out=ps[:, 0:16], lhsT=self.ones[:], rhs=self.lf[:, t2, :],
                                                                    start=(t2 == 0), stop=(t2 == ta - 1)),
                         reads=[("lf", t2), self.ones], writes=[ps])
                P.op("vector", lambda e, g=g: e.tensor_copy(out=self.anc[:, g, :], in_=ps[:, 0:16]), reads=[ps], writes=[("anc", g)])
        ebuf = [self.W[0], self.W[1]]
        for h in range(nheads):
            for m in range(nmaps):
                mm = h * nmaps + m
                self.dma("sync", kTs[m], sc["kT"][mm, :, :], reads=[("kT", mm)], writes=[("kTs", m)])
                self.dma("sync", qTs[m], sc["qT"][mm, :, :], reads=[("qT", mm)], writes=[("qTs", m)])
            self.dma("sync", v1, sc["v"][h, :, :, :].rearrange("t p c -> p t c"), reads=[("v", (h // (512 // hv)) * (512 // hv))], writes=["v1"])
            for g in range(nq):
                nkb = nprev + 4 * g + 4
                o1n = self.f32view(self.W[2], 0, 1024).rearrange("p (i c) -> p i c", i=4)
                for m in range(nmaps):
                    for j in range(nkb):
                        pss = self.PS[4 + j % 2]
                        P.op("tensor", lambda e, m=m, j=j, g=g, pss=pss: e.matmul(
                            out=pss[:], lhsT=kTs[m][:, j * 128:(j + 1) * 128], rhs=qTs[m][:, g * 512:(g + 1) * 512],
                            start=True, stop=True), reads=[("kTs", m), ("qTs", m)], writes=[pss])
                        et = ebuf[j % 2]
                        ev = et[:, 0:512]
                        if kind == "A":
                            bias = self.pbias[:, 0:1] if j < nprev else self.zero[:, 0:1]
                            breads = [self.pbias, self.zero]
                        else:
                            bcol = self.small[:, 48 + (j % 2):49 + (j % 2)]
                            P.op("vector", lambda e, bcol=bcol, g=g, j=j, h=h: e.tensor_tensor(
                                out=bcol, in0=self.anc[:, g, h:h + 1], in1=self.cum[:, j, h:h + 1], op=ALU.subtract),
                                reads=[("anc", g), ("cum", j)], writes=[("bcol", j % 2)])
                            if j < nprev:
                                P.op("vector", lambda e, bcol=bcol: e.tensor_tensor(out=bcol, in0=bcol, in1=self.pbias[:, 0:1], op=ALU.add),
                                     reads=[("bcol", j % 2), self.pbias], writes=[("bcol", j % 2)])
                            bias = bcol
                            breads = [("bcol", j % 2)]
                        P.op("scalar", lambda e, ev=ev, pss=pss, bias=bias: e.activation(out=ev, in_=pss[:], func=AF.Exp, bias=bias, scale=scale),
                             reads=[pss] + breads, writes=[et])
                        jo = j - nprev
                        for i in range(4):
                            qi = 4 * g + i
                            if jo > qi:
                                continue
                            if jo == qi:
                                P.op("gpsimd", lambda e, ev=ev, i=i: e.tensor_tensor(
                                    out=ev[:, i * 128:(i + 1) * 128], in0=ev[:, i * 128:(i + 1) * 128], in1=self.trib[:], op=ALU.mult),
                                    reads=[et, self.trib], writes=[et])
                            pso = self.PS[i]
                            last = nprev + qi
                            P.op("tensor", lambda e, ev=ev, i=i, j=j, pso=pso, last=last: e.matmul(
                                out=pso[:, 0:hv1], lhsT=ev[:, i * 128:(i + 1) * 128], rhs=v1[:, j, :],
                                start=(j == 0), stop=(j == last)), reads=[et, "v1"], writes=[pso])
                    st = self.small
                    for i in range(4):
                        pso = self.PS[i]
                        qi = 4 * g + i
                        P.op("vector", lambda e, pso=pso, i=i: e.reciprocal(out=st[:, 8 + i:9 + i], in_=pso[:, hv:hv1]), reads=[pso], writes=[st])
                        if kind == "A" and m == 0:
                            P.op("vector", lambda e, pso=pso, i=i: e.tensor_scalar(
                                out=o1n[:, i, :], in0=pso[:, 0:hv], scalar1=st[:, 8 + i:9 + i], scalar2=None, op0=ALU.mult),
                                reads=[pso, st], writes=[(self.W[2].key, "o1n", i)])
                        elif kind == "A":
                            ot = self.f32view(self.W[3], i * 256, 256)
                            okey = (self.W[3].key, "ot", i)
                            P.op("vector", lambda e, i=i: e.tensor_tensor(out=st[:, 12 + i:13 + i], in0=st[:, 8 + i:9 + i], in1=self.lam[:, 1:2], op=ALU.mult),
                                 reads=[st, self.lam], writes=[st])
                            P.op("vector", lambda e, pso=pso, i=i, ot=ot: e.scalar_tensor_tensor(
                                out=ot, in0=pso[:, 0:hv], scalar=st[:, 12 + i:13 + i], in1=o1n[:, i, :], op0=ALU.mult, op1=ALU.add),
                                reads=[pso, st, (self.W[2].key, "o1n", i)], writes=[okey])
                            sq = self.f32view(self.W[3], 1024 + i * 256, 256)
                            P.op("scalar", lambda e, ot=ot, sq=sq, i=i: e.activation(out=sq, in_=ot, func=AF.Square, accum_out=st[:, 16 + i:17 + i]),
                                 reads=[okey], writes=[(self.W[3].key, "sq", i), st])
                            P.op("vector", lambda e, i=i: e.tensor_scalar(out=st[:, 20 + i:21 + i], in0=st[:, 16 + i:17 + i], scalar1=1.0 / 256,
                                                                         scalar2=RMS_EPS, op0=ALU.mult, op1=ALU.add), reads=[st], writes=[st])
                            P.op("scalar", lambda e, i=i: e.activation(out=st[:, 24 + i:25 + i], in_=st[:, 20 + i:21 + i], func=AF.Sqrt), reads=[st], writes=[st])
                            P.op("vector", lambda e, i=i: e.reciprocal(out=st[:, 28 + i:29 + i], in_=st[:, 24 + i:25 + i]), reads=[st], writes=[st])
                            ob = self.W[3][:, 4096 + i * 256:4096 + (i + 1) * 256]
                            obk = (self.W[3].key, "ob", i)
                            P.op("vector", lambda e, ot=ot, ob=ob, i=i: e.scalar_tensor_tensor(
                                out=ob, in0=ot, scalar=st[:, 28 + i:29 + i], in1=self.subw[:], op0=ALU.mult, op1=ALU.mult),
                                reads=[okey, st, self.subw], writes=[obk])
                            self.dma("sync", sc["o"][qi * 128:(qi + 1) * 128, h * 256:(h + 1) * 256], ob, reads=[obk], writes=[("o", qi)])
                        else:
                            ob = self.W[3][:, 4096 + i * 128:4096 + (i + 1) * 128]
                            obk = (self.W[3].key, "ob", i)
                            P.op("vector", lambda e, pso=pso, ob=ob, i=i: e.tensor_scalar(
                                out=ob, in0=pso[:, 0:hv], scalar1=st[:, 8 + i:9 + i], scalar2=None, op0=ALU.mult),
                                reads=[pso, st], writes=[obk])
                            self.dma("sync", sc["o"][qi * 128:(qi + 1) * 128, h * 128:(h + 1) * 128], ob, reads=[obk], writes=[("o", qi)])

    def out_proj(self, l, w_out, x_ap, lng, lnb, sc, x1_ap):
        P = self.P
        T = self.T
        wo = [self.W[i] for i in range(4)]
        for n in range(4):
            self.dma("gpsimd", wo[n][:].rearrange("p (k n) -> p k n", k=16),
                     w_out[:, n * 512:(n + 1) * 512].rearrange("(k p) n -> p k n", p=128), writes=[wo[n]])
        self.dma("sync", self.BC[0][:], lng.partition_broadcast(128), writes=[self.BC[0]])
        self.dma("sync", self.BC[1][:], lnb.partition_broadcast(128), writes=[self.BC[1]])
        for t in range(T // 128):
            ob = self.BIGB[:, (t % 2) * 2048:(t % 2 + 1) * 2048]
            obk = ("ob", t % 2)
            self.dma("sync", ob, sc["o"][t * 128:(t + 1) * 128, :], reads=[("o", t)], writes=[obk])
            xt = self.f32view(self.BIGA, (t % 2) * 2048, 2048)
            xk = ("xt", t % 2)
            self.dma("sync", xt, x_ap[t * 128:(t + 1) * 128, :], writes=[xk])
            oT = self.BIGB[:, 4096 + (t % 2) * 2048:4096 + (t % 2 + 1) * 2048].rearrange("p (k n) -> p k n", k=16)
            oTk = ("oT", t % 2)
            for g in range(4):
                ps = self.PS[4 + g % 2]
                psb = ps.h.bitcast(BF16)
                for j in range(4):
                    k = g * 4 + j
                    P.op("tensor", lambda e, ob=ob, k=k, j=j, psb=psb: e.transpose(
                        out=psb[:, j * 128:(j + 1) * 128], in_=ob[:, k * 128:(k + 1) * 128], identity=self.identb[:]),
                        reads=[obk, self.identb], writes=[ps])
                P.op("scalar" if g % 2 else "vector",
                     (lambda e, oT=oT, psb=psb, g=g: e.activation(out=oT[:, g * 4:(g + 1) * 4, :], in_=psb[:, 0:512].rearrange("p (a b) -> p a b", a=4), func=AF.Copy))
                     if g % 2 else
                     (lambda e, oT=oT, psb=psb, g=g: e.tensor_copy(out=oT[:, g * 4:(g + 1) * 4, :], in_=psb[:, 0:512].rearrange("p (a b) -> p a b", a=4))),
                     reads=[ps], writes=[oTk])
            y = self.f32view(self.BIGA, 4096 + (t % 2) * 2048, 2048)
            yk = ("y", t % 2)
            for n in range(4):
                ps = self.PS[n]
                for k in range(16):
                    P.op("tensor", lambda e, n=n, k=k, oT=oT, ps=ps: e.matmul(
                        out=ps[:], lhsT=oT[:, k, :], rhs=wo[n][:, k * 512:(k + 1) * 512], start=(k == 0), stop=(k == 15)),
                        reads=[oTk, wo[n]], writes=[ps])
                P.op("vector", lambda e, n=n, ps=ps, y=y: e.tensor_tensor(
                    out=y[:, n * 512:(n + 1) * 512], in0=ps[:], in1=self.BC[2][:, n * 512:(n + 1) * 512], op=ALU.mult),
                    reads=[ps, self.BC[2]], writes=[yk])
            P.op("vector", lambda e, xt=xt, y=y: e.scalar_tensor_tensor(out=xt, in0=xt, scalar=ALPHA, in1=y, op0=ALU.mult, op1=ALU.add),
                 reads=[xk, yk], writes=[xk])
            self.layer_norm(xt, xk, self.BC[0], self.BC[1], y, yk)
            self.dma("sync", x1_ap[t * 128:(t + 1) * 128, :], xt, reads=[xk], writes=[("x1", t)])

    def moe(self, l, W, x1_ap, out_ap, outkey):
        P = self.P
        T = self.T
        TPASS = min(1024, T)
        npass = T // TPASS
        self.dma("sync", self.wr[:, :, 0:4], W["w_group"].rearrange("(k p) g -> p k g", p=128), writes=[self.wr])
        for g in range(4):
            self.dma("sync", self.wr[:, :, 4 + 8 * g:12 + 8 * g], W["w_router"][g].rearrange("(k p) e -> p k e", p=128), writes=[self.wr])
        self.dma("sync", self.brt[:, 0:4], W["b_group"].partition_broadcast(128), writes=[self.brt])
        self.dma("sync", self.brt[:, 4:36], W["b_router"].partition_broadcast(128), writes=[self.brt])
        if npass > 1:
            if not hasattr(self, "bcsave"):
                self.bcsave = self.dscr("s_bcsave", [2, 128, 2048], F32)
            self.dma("sync", self.bcsave[0], self.BC[0][:], reads=[self.BC[0]], writes=["bcsave0"])
            self.dma("sync", self.bcsave[1], self.BC[1][:], reads=[self.BC[1]], writes=["bcsave1"])
        ntile = TPASS // 128
        hT2 = self.BIGB[:, 0:16 * TPASS].rearrange("p (k t) -> p k t", k=16)
        accv = self.BIGA.h.bitcast(F32)
        hidbufs = [self.BC[0].h.bitcast(BF16)[:, i * 2048:(i + 1) * 2048].rearrange("p (f t) -> p f t", f=4) for i in range(2)]
        sgbufs = [self.BC[1][:, i * 512:(i + 1) * 512] for i in range(2)]
        for ps_i in range(npass):
            P.fence()
            tok0 = ps_i * TPASS
            if ps_i > 0:
                self.dma("sync", self.BC[0][:], self.bcsave[0], writes=[self.BC[0]])
                self.dma("sync", self.BC[1][:], self.bcsave[1], writes=[self.BC[1]])
            self.make_hT(x1_ap[tok0:tok0 + TPASS, :], TPASS, hT2, "hT2", router=dict())
            P.fence()
            hi = 0
            for ex in range(32):
                g, e = ex // 8, ex % 8
                wg = self.W[0 + ex % 2]
                wu = self.W[2 + ex % 2]
                wd = self.W[4]
                wgv = wg[:].rearrange("p (k n) -> p k n", k=16)
                wuv = wu[:].rearrange("p (k n) -> p k n", k=16)
                wdv = wd[:].rearrange("p (k n) -> p k n", k=4)
                self.dma("gpsimd", wgv, W["w_gate"][g, e].rearrange("(k p) n -> p k n", p=128), writes=[wg])
                self.dma("gpsimd", wuv, W["w_up"][g, e].rearrange("(k p) n -> p k n", p=128), writes=[wu])
                self.dma("gpsimd", wdv, W["w_down"][g, e].rearrange("(k p) n -> p k n", p=128), writes=[wd])
                for tb in range(TPASS // 512):
                    hb = hi % 2
                    hi += 1
                    hidb = hidbufs[hb]
                    for fc in range(4):
                        psg = self.PS[fc % 2]
                        psu = self.PS[2 + fc % 2]
                        for k in range(16):
                            P.op("tensor", lambda e_, k=k, fc=fc, tb=tb, psg=psg, wgv=wgv: e_.matmul(
                                out=psg[:], lhsT=wgv[:, k, fc * 128:(fc + 1) * 128], rhs=hT2[:, k, tb * 512:(tb + 1) * 512],
                                start=(k == 0), stop=(k == 15)), reads=[wg] + [("hT2", tb * 4 + i) for i in range(4)], writes=[psg])
                        for k in range(16):
                            P.op("tensor", lambda e_, k=k, fc=fc, tb=tb, psu=psu, wuv=wuv: e_.matmul(
                                out=psu[:], lhsT=wuv[:, k, fc * 128:(fc + 1) * 128], rhs=hT2[:, k, tb * 512:(tb + 1) * 512],
                                start=(k == 0), stop=(k == 15)), reads=[wu] + [("hT2", tb * 4 + i) for i in range(4)], writes=[psu])
                        sg = sgbufs[fc % 2]
                        sgk = ("sg", fc % 2)
                        P.op("scalar", lambda e_, sg=sg, psg=psg: e_.activation(out=sg, in_=psg[:], func=AF.Silu), reads=[psg], writes=[sgk])
                        P.op("vector", lambda e_, sg=sg, psu=psu, hidb=hidb, fc=fc: e_.tensor_tensor(
                            out=hidb[:, fc, :], in0=psu[:], in1=sg, op=ALU.mult), reads=[psu, sgk], writes=[("hid", hb, fc)])
                    for tt in range(4):
                        t = tb * 4 + tt
                        for dmb in range(4):
                            psd = self.PS[4 + (tt * 4 + dmb) % 4]
                            for fc in range(4):
                                P.op("tensor", lambda e_, fc=fc, tt=tt, dmb=dmb, psd=psd, hidb=hidb, wdv=wdv: e_.matmul(
                                    out=psd[:], lhsT=hidb[:, fc, tt * 128:(tt + 1) * 128], rhs=wdv[:, fc, dmb * 512:(dmb + 1) * 512],
                                    start=(fc == 0), stop=(fc == 3)), reads=[("hid", hb, fc), wd], writes=[psd])
                            av = accv[:, t * 2048 + dmb * 512:t * 2048 + (dmb + 1) * 512]
                            if ex == 0:
                                P.op("vector", lambda e_, psd=psd, av=av, t=t, ex=ex: e_.tensor_scalar(
                                    out=av, in0=psd[:], scalar1=self.w32[:, t, ex:ex + 1], scalar2=None, op0=ALU.mult),
                                    reads=[psd, ("w32", t)], writes=[("acc", t)])
                            else:
                                P.op("vector", lambda e_, psd=psd, av=av, t=t, ex=ex: e_.scalar_tensor_tensor(
                                    out=av, in0=psd[:], scalar=self.w32[:, t, ex:ex + 1], in1=av, op0=ALU.mult, op1=ALU.add),
                                    reads=[psd, ("w32", t), ("acc", t)], writes=[("acc", t)])
            P.fence()
            self.dma("sync", self.f32view(self.W[0], 0, 2048), W["ln_g"].partition_broadcast(128), writes=[self.W[0]])
            self.dma("sync", self.f32view(self.W[1], 0, 2048), W["ln_b"].partition_broadcast(128), writes=[self.W[1]])
            gt = _View(self.f32view(self.W[0], 0, 2048), self.W[0].key)
            bt = _View(self.f32view(self.W[1], 0, 2048), self.W[1].key)
            for t in range(ntile):
                z = accv[:, t * 2048:(t + 1) * 2048]
                xt = self.f32view(self.W[3], (t % 2) * 2048, 2048)
                xk = (self.W[3].key, "xt", t % 2)
                self.dma("sync", xt, x1_ap[tok0 + t * 128:tok0 + (t + 1) * 128, :], writes=[xk])
                P.op("vector", lambda e_, z=z: e_.tensor_tensor(out=z, in0=z, in1=self.BC[2][:], op=ALU.mult),
                     reads=[("acc", t), self.BC[2]], writes=[("acc", t)])
                P.op("vector", lambda e_, z=z, xt=xt: e_.scalar_tensor_tensor(out=z, in0=xt, scalar=ALPHA, in1=z, op0=ALU.mult, op1=ALU.add),
                     reads=[("acc", t), xk], writes=[("acc", t)])
                scr = self.f32view(self.W[2], (t % 2) * 2048, 2048)
                self.layer_norm(z, ("acc", t), gt, bt, scr, (self.W[2].key, "scr", t % 2))
                self.dma("sync", out_ap[tok0 + t * 128:tok0 + (t + 1) * 128, :], z, reads=[("acc", t)], writes=[(outkey, tok0 // 128 + t)])

    def rowhid(self, tb):
        o = 8192 + (tb % 2) * 2048
        return self.BIGB[:, o:o + 2048].rearrange("p (f t) -> p f t", f=4)

    def sgbuf(self, i):
        return self.BIGB.h.bitcast(F32)[:, 6144 + i * 512:6144 + (i + 1) * 512]


Buf.register = None


def _is_buf(x):
    return isinstance(x, (Buf, _View))


def _k(x):
    return x.key if isinstance(x, (Buf, _View)) else x


def build_program(T, TP, layers, ncores=8):
    B = Builder(T, TP, layers)
    nc, P = B.nc, B.P
    TC = T + TP
    B.load_consts()
    xo = B.din("xo", [T, D])
    xp = B.din("xp", [TP, D])
    cosT = B.din("c_cos", [128, TC])
    sinT = B.din("c_sin", [128, TC])
    out = nc.dram_tensor("out", [T, D], F32, kind="ExternalOutput").ap()
    sc = dict(
        qT=B.dscr("s_qT", [16, 128, T], BF16), kT=B.dscr("s_kT", [16, 128, TC], BF16),
        o=B.dscr("s_o", [T, D], BF16), cos=cosT, sin=sinT)
    vA = B.dscr("s_vA", [8, TC // 128, 128, 257], BF16)
    vB = B.dscr("s_vB", [16, TC // 128, 128, 129], BF16)
    x1 = B.dscr("s_x1", [T, D], F32)
    CH = 256
    nch = T // CH
    if len(layers) > 1:
        x2 = B.dscr("s_x2", [T, D], F32)
        gath = B.dscr("s_gath", [nch, 2 * CH, D], F32)
    cur_o, cur_p = xo, xp
    for li, l in enumerate(layers):
        kind = "A" if l % 2 == 0 else "B"
        last = (li == len(layers) - 1)
        sc["v"] = vA if kind == "A" else vB
        wmod = B.din("mix_mod_w%d" % l, [D, 6144])
        bmod = B.din("mix_mod_b%d" % l, [1, 6144])
        lng = B.din("mix_ln_g%d" % l, [1, D])
        lnb = B.din("mix_ln_b%d" % l, [1, D])
        lam_init = lamv = subw = fbias = None
        if kind == "A":
            w_in = B.din("a_w_in", [D, 6144])
            w_out = B.din("a_w_out", [D, D])
            lamv = [B.din("a_lam_q1", [1, 128]), B.din("a_lam_k1", [1, 128]),
                    B.din("a_lam_q2", [1, 128]), B.din("a_lam_k2", [1, 128])]
            subw = B.din("a_subln_w", [1, 256])
            lam_init = 0.8 - 0.6 * math.exp(-0.3 * l)
        else:
            w_in = B.din("b_w_in", [D, 6160])
            w_out = B.din("b_w_out", [D, D])
            fbias = B.din("b_forget_bias", [1, 16])
        Wm = dict(
            w_group=B.din("moe_w_group%d" % l, [D, 4]), b_group=B.din("moe_b_group%d" % l, [1, 4]),
            w_router=B.din("moe_w_router%d" % l, [4, D, 8]), b_router=B.din("moe_b_router%d" % l, [1, 32]),
            w_gate=B.din("moe_w_gate%d" % l, [4, 8, D, 512]), w_up=B.din("moe_w_up%d" % l, [4, 8, D, 512]),
            w_down=B.din("moe_w_down%d" % l, [4, 8, 512, D]),
            ln_g=B.din("ffn_ln_g%d" % l, [1, D]), ln_b=B.din("ffn_ln_b%d" % l, [1, D]))
        fwmod = B.din("ffn_mod_w%d" % l, [D, 6144])
        fbmod = B.din("ffn_mod_b%d" % l, [1, 6144])

        P.fence()
        B.modulation(wmod, bmod)
        B.mixer_params(kind, lam_init, lamv, subw, fbias, w_in)
        hT = B.BIGA[:, 0:16 * max(T, TP)].rearrange("p (k t) -> p k t", k=16)
        for (x_ap, ntok, tok0, with_q) in ((cur_p, TP, 0, False), (cur_o, T, TP, True)):
            P.fence()
            B.make_hT(x_ap, ntok, hT[:, :, 0:ntok], "hT")
            P.fence()
            B.qkv(l, kind, w_in, hT[:, :, 0:ntok], "hT", ntok, tok0, with_q, sc)
        P.fence()
        B.attention(l, kind, sc)
        P.fence()
        B.out_proj(l, w_out, cur_o, lng, lnb, sc, x1)
        P.fence()
        B.modulation(fwmod, fbmod)
        B.moe(l, Wm, x1, out if last else x2, "out" if last else "x2")
        if not last:
            P.fence()
            groups = [[2 * i, 2 * i + 1] for i in range(ncores // 2)]
            for ch in range(nch):
                if os.environ.get("K_NOCC"):
                    B.dma("sync", gath[ch, 0:CH, :], x2[ch * CH:(ch + 1) * CH, :], writes=[("gath", ch)])
                    continue
                P.op("gpsimd", lambda e, ch=ch: e.collective_compute(
                    "AllGather", ALU.bypass, replica_groups=groups,
                    ins=[x2[ch * CH:(ch + 1) * CH, :]], outs=[gath[ch]]),
                    writes=[("gath", ch)], dma=True, cc=True)
            P.fence()
            cur_o = x2
            cur_p = (lambda t: gath[t // 2, (t % 2) * 128:(t % 2 + 1) * 128, :])
    P.fence()
    P.op("sync", None, reads=[])
    P.emit()
    return nc, B


def _consts(T, TP, p):
    TC = T + TP
    ident = np.eye(128, dtype=np.float32)
    tri = np.triu(np.ones((128, 128), np.float32))
    R = np.zeros((128, 128), np.float32)
    for i in range(64):
        R[i, i + 64] = -1.0
        R[i + 64, i] = 1.0
    rotT = np.ascontiguousarray(R.T)
    pos = np.concatenate([np.arange(TP), p * T + np.arange(T)]).astype(np.float32)
    inv = np.power(np.float32(10000.0), -np.arange(0, 128, 2, dtype=np.float32) / 128).astype(np.float32)
    ang = pos[None, :] * np.concatenate([inv, inv])[:, None]
    return dict(c_ident=ident, c_tri=tri, c_rotT=rotT,
                c_pbias=np.full((128, 1), 0.0 if p == 1 else NEG, np.float32),
                c_cos=np.cos(ang).astype(np.float32), c_sin=np.sin(ang).astype(np.float32))


_PROG_CACHE = {}
_RUN_KW = {}
_LAST = {}


def run_layers(layers, x_in, inp, T, TP, ncores):
    key = (tuple(layers), T, TP, ncores)
    if key not in _PROG_CACHE:
        _PROG_CACHE[key] = build_program(T, TP, list(layers), ncores)
    nc, B = _PROG_CACHE[key]
    f = np.float32
    shared = {}
    for l in layers:
        j = l // 2
        shared["mix_mod_w%d" % l] = inp["mix_mod_w"][l]
        shared["mix_mod_b%d" % l] = inp["mix_mod_b"][l].reshape(1, -1)
        shared["mix_ln_g%d" % l] = inp["mix_ln_g"][l].reshape(1, -1)
        shared["mix_ln_b%d" % l] = inp["mix_ln_b"][l].reshape(1, -1)
        if l % 2 == 0:
            shared["a_w_in"] = inp["a_w_in"][j]
            shared["a_w_out"] = inp["a_w_out"][j]
            shared["a_lam_q1"] = inp["a_lam_q1"][j].reshape(1, -1)
            shared["a_lam_k1"] = inp["a_lam_k1"][j].reshape(1, -1)
            shared["a_lam_q2"] = inp["a_lam_q2"][j].reshape(1, -1)
            shared["a_lam_k2"] = inp["a_lam_k2"][j].reshape(1, -1)
            shared["a_subln_w"] = inp["a_subln_w"][j].reshape(1, -1)
        else:
            shared["b_w_in"] = inp["b_w_in"][j]
            shared["b_w_out"] = inp["b_w_out"][j]
            shared["b_forget_bias"] = inp["b_forget_bias"][j].reshape(1, -1)
        shared["ffn_mod_w%d" % l] = inp["ffn_mod_w"][l]
        shared["ffn_mod_b%d" % l] = inp["ffn_mod_b"][l].reshape(1, -1)
        shared["ffn_ln_g%d" % l] = inp["ffn_ln_g"][l].reshape(1, -1)
        shared["ffn_ln_b%d" % l] = inp["ffn_ln_b"][l].reshape(1, -1)
        shared["moe_w_group%d" % l] = inp["moe_w_group"][l]
        shared["moe_b_group%d" % l] = inp["moe_b_group"][l].reshape(1, -1)
        shared["moe_w_router%d" % l] = inp["moe_w_router"][l]
        shared["moe_b_router%d" % l] = inp["moe_b_router"][l].reshape(1, -1)
        shared["moe_w_gate%d" % l] = inp["moe_w_gate"][l]
        shared["moe_w_up%d" % l] = inp["moe_w_up"][l]
        shared["moe_w_down%d" % l] = inp["moe_w_down"][l]
    shared = {k: np.ascontiguousarray(np.asarray(v, dtype=f)) for k, v in shared.items() if k in B.inputs}
    in_maps = []
    for c in range(ncores):
        b, p = c // 2, c % 2
        m = _consts(T, TP, p)
        m["xo"] = x_in[b, p * T:(p + 1) * T]
        m["xp"] = x_in[b, 0:TP]
        m["cvec_in"] = inp["c"][b].reshape(16, 128).T
        m = {k: np.ascontiguousarray(np.asarray(v, dtype=f)) for k, v in m.items() if k in B.inputs}
        m.update(shared)
        in_maps.append(m)
    res = run_bass_kernel_spmd(nc, in_maps, core_ids=list(range(ncores)), **_RUN_KW)
    _LAST['res'] = res
    out = np.empty_like(x_in)
    for c in range(ncores):
        b, p = c // 2, c % 2
        out[b, p * T:(p + 1) * T] = res.results[c]["out"]
    return out


def run_layer(l, x_in, inp, T, TP, ncores, stop=99):
    return run_layers([l], x_in, inp, T, TP, ncores)


def kernel(**inputs):
    inp = {k: np.asarray(v) for k, v in inputs.items()}
    x = np.asarray(inp["x"], dtype=np.float32)
    Bn, S, _ = x.shape
    T = S // 2
    ncores = Bn * 2
    return run_layers(list(range(DEPTH)), x, inp, T, T, ncores)
```

```python
import math
import os
import numpy as np
import concourse.bass as bass
import concourse.mybir as mybir
from concourse.bass_utils import run_bass_kernel_spmd

F32 = mybir.dt.float32
BF16 = mybir.dt.bfloat16
AF = mybir.ActivationFunctionType
ALU = mybir.AluOpType
AX = mybir.AxisListType

D = 2048
KC = 16
DEPTH = 2
ALPHA = (2.0 * DEPTH) ** 0.25
LN_EPS = 1e-5
RMS_EPS = 1e-5
NEG = -30000.0
COMPUTE = ("tensor", "vector", "scalar", "gpsimd")
NDMASEM = 8


class Buf:
    _n = 0

    def __init__(self, h, name, psum=False):
        self.h = h
        self.psum = psum
        Buf._n += 1
        self.key = name + "#" + str(Buf._n)

    def __getitem__(self, idx):
        return self.h[idx]


def _k(x):
    return x.key if isinstance(x, Buf) else x


class _View:
    def __init__(self, ap, key):
        self.ap = ap
        self.key = key

    def __getitem__(self, idx):
        return self.ap


class Prog:
    def __init__(self, nc):
        self.nc = nc
        self.ops = []
        self.last_w = {}
        self.readers = {}
        self.last_eng = {}
        self.last_dma = {}
        self.cc_ops = []

    def sbuf(self, name, shape, dtype):
        return Buf(self.nc.alloc_sbuf_tensor(name, list(shape), dtype), name)

    def psum(self, name, shape, dtype=F32):
        return Buf(self.nc.alloc_psum_tensor(name, list(shape), dtype), name, psum=True)

    def op(self, eng, fn, reads=(), writes=(), dma=False, cc=False):
        i = len(self.ops)
        deps = set()
        pr = [r for r in reads if isinstance(r, Buf) and r.psum]
        if pr:
            reads = [r for r in reads if not (isinstance(r, Buf) and r.psum)]
            writes = list(writes) + [r for r in pr if r not in writes]
        for r in reads:
            k = _k(r)
            if k in self.last_w:
                deps.add(self.last_w[k])
        for w in writes:
            k = _k(w)
            if k in self.last_w:
                deps.add(self.last_w[k])
            for rd in self.readers.get(k, ()):
                deps.add(rd)
        for r in reads:
            self.readers.setdefault(_k(r), []).append(i)
        for w in writes:
            k = _k(w)
            self.last_w[k] = i
            self.readers[k] = []
        deps.discard(i)
        self.ops.append(dict(eng=eng, fn=fn, deps=deps, dma=dma, signal=False, cc=cc))
        if cc:
            self.cc_ops.append(i)
        elif dma:
            self.last_dma.setdefault(eng, []).append(i)
            self.last_dma[eng] = self.last_dma[eng][-NDMASEM:]
        else:
            self.last_eng[eng] = i
        return i

    def fence(self):
        deps = set(self.last_eng.values())
        for v in self.last_dma.values():
            deps.update(v)
        deps.update(self.cc_ops[-1:])
        for e in COMPUTE + ("sync",):
            self.ops.append(dict(eng=e, fn=None, deps=set(deps), dma=False, signal=False, cc=False))
        self.last_w = {}
        self.readers = {}

    def emit(self):
        nc = self.nc
        ops = self.ops

        def dom(o):
            if o["cc"]:
                return "cc"
            return ("dma", o["eng"]) if o["dma"] else o["eng"]

        ccsem = nc.alloc_semaphore(name="s_cc") if self.cc_ops else None
        ncc = 0
        for o in ops:
            if o["cc"]:
                ncc += 1
            o["ncc_before"] = ncc
        for o in ops:
            for d in o["deps"]:
                od = ops[d]
                if od["cc"]:
                    continue
                if (not od["dma"]) and (not o["dma"]) and od["eng"] == o["eng"] == "tensor":
                    continue
                od["signal"] = True
        cnt = {}
        dma_idx = {}
        for o in ops:
            dm = dom(o)
            if o["cc"]:
                continue
            if o["dma"]:
                n = dma_idx.get(dm, 0)
                o["dslot"] = n % NDMASEM
                o["dround"] = n // NDMASEM
                dma_idx[dm] = n + 1
            elif o["signal"] and o["fn"] is not None:
                cnt[dm] = cnt.get(dm, 0) + 1
                o["cnt"] = cnt[dm]
        sems = {e: nc.alloc_semaphore(name="s_" + e) for e in COMPUTE}
        dsems = {dm: [nc.alloc_semaphore(name="d_%s_%d" % (dm[1], k)) for k in range(NDMASEM)]
                 for dm in dma_idx}
        streams = {}
        for i, o in enumerate(ops):
            streams.setdefault(o["eng"], []).append(i)

        def run_stream(eng_name, engine):
            waited = {}

            def wait(sem, key, val):
                if waited.get(key, -1) >= val:
                    return
                engine.wait_ge(sem, val)
                waited[key] = val

            for i in streams.get(eng_name, []):
                o = ops[i]
                for d in sorted(o["deps"]):
                    od = ops[d]
                    if od["cc"]:
                        wait(ccsem, "cc", o["ncc_before"] - (1 if o["cc"] else 0))
                        continue
                    if od["dma"]:
                        dm = dom(od)
                        wait(dsems[dm][od["dslot"]], (dm, od["dslot"]), 16 * (od["dround"] + 1))
                    else:
                        if "cnt" not in od:
                            continue
                        if od["eng"] == eng_name == "tensor" and not o["dma"]:
                            continue
                        wait(sems[od["eng"]], od["eng"], od["cnt"])
                if o["dma"] and not o["cc"] and o["dround"] > 0:
                    dm = dom(o)
                    wait(dsems[dm][o["dslot"]], (dm, o["dslot"]), 16 * o["dround"])
                if o["fn"] is None:
                    continue
                ins = o["fn"](engine)
                if o["cc"]:
                    ins.then_inc(ccsem, 1)
                elif o["dma"]:
                    ins.then_inc(dsems[dom(o)][o["dslot"]], 16)
                elif "cnt" in o:
                    ins.then_inc(sems[eng_name], 1)

        with nc.Block() as block:
            @block.tensor
            def _(e):
                run_stream("tensor", e)

            @block.vector
            def _(e):
                run_stream("vector", e)

            @block.scalar
            def _(e):
                run_stream("scalar", e)

            @block.gpsimd
            def _(e):
                run_stream("gpsimd", e)

            @block.sync
            def _(e):
                run_stream("sync", e)


class Builder:
    def __init__(self, T, TP, layers, n_exp_groups=4):
        self.T, self.TP, self.TC = T, TP, T + TP
        self.layers = layers
        nc = self.nc = bass.Bass("TRN2", target_bir_lowering=False)
        P = self.P = Prog(nc)
        self.inputs = {}
        self.BIGA = P.sbuf("BIGA", [128, 32768], BF16)
        self.BIGB = P.sbuf("BIGB", [128, 16384], BF16)
        self.W = [P.sbuf("W%d" % i, [128, 8192], BF16) for i in range(5)]
        self.BC = [P.sbuf("BC%d" % i, [128, 2048], F32) for i in range(3)]
        self.ident = P.sbuf("ident", [128, 128], F32)
        self.identb = P.sbuf("identb", [128, 128], BF16)
        self.tri = P.sbuf("tri", [128, 128], F32)
        self.trib = P.sbuf("trib", [128, 128], BF16)
        self.rotT = P.sbuf("rotT", [128, 128], BF16)
        self.ones = P.sbuf("ones", [128, 128], F32)
        self.pbias = P.sbuf("pbias", [128, 1], F32)
        self.zero = P.sbuf("zero", [128, 1], F32)
        self.cvec = P.sbuf("cvec", [128, 16], F32)
        self.rowbuf = _View(self.BIGA.h.bitcast(F32)[0:1, 0:6144], self.BIGA.key)
        self.small = P.sbuf("small", [128, 64], F32)
        self.lam = P.sbuf("lam", [128, 8], F32)
        self.subw = P.sbuf("subw", [128, 256], F32)
        self.w32 = P.sbuf("w32", [128, 8, 32], F32)
        self.rt = P.sbuf("rt", [128, 128], F32)
        self.wr = P.sbuf("wr", [128, 16, 36], F32)
        self.brt = P.sbuf("brt", [128, 36], F32)
        bf = self.BIGB.h.bitcast(F32)
        self.lf = bf[:, 0:512].rearrange("p (t h) -> p t h", h=16)
        self.cum = bf[:, 512:1024].rearrange("p (t h) -> p t h", h=16)
        self.anc = bf[:, 1024:1088].rearrange("p (t h) -> p t h", h=16)
        self.fb = P.sbuf("fb", [128, 16], F32)
        self.wf = _View(self.BIGB[:, 4096:4352].rearrange("p (k n) -> p k n", k=16), "wfkey")
        self.PS = [P.psum("ps%d" % i, [128, 512], F32) for i in range(8)]

    def din(self, name, shape, dt=F32):
        t = self.nc.dram_tensor(name, list(shape), dt, kind="ExternalInput").ap()
        self.inputs[name] = t
        return t

    def dscr(self, name, shape, dt):
        return self.nc.dram_tensor(name, list(shape), dt).ap()

    def dma(self, eng, out, in_, reads=(), writes=()):
        self.P.op(eng, lambda e: e.dma_start(out=out, in_=in_), reads=reads, writes=writes, dma=True)

    def f32view(self, buf, off_f32, n):
        return buf.h.bitcast(F32)[:, off_f32:off_f32 + n]

    def load_consts(self):
        P = self.P
        c_ident = self.din("c_ident", [128, 128])
        c_tri = self.din("c_tri", [128, 128])
        c_rotT = self.din("c_rotT", [128, 128])
        c_pbias = self.din("c_pbias", [128, 1])
        self.dma("sync", self.ident[:], c_ident, writes=[self.ident])
        self.dma("sync", self.tri[:], c_tri, writes=[self.tri])
        self.dma("sync", self.pbias[:], c_pbias, writes=[self.pbias])
        self.dma("gpsimd", self.identb[:], c_ident, writes=[self.identb])
        self.dma("gpsimd", self.trib[:], c_tri, writes=[self.trib])
        self.dma("gpsimd", self.rotT[:], c_rotT, writes=[self.rotT])
        P.op("vector", lambda e: e.memset(self.ones[:], 1.0), writes=[self.ones])
        P.op("vector", lambda e: e.memset(self.zero[:], 0.0), writes=[self.zero])
        cv = self.din("cvec_in", [128, 16])
        self.dma("sync", self.cvec[:], cv, writes=[self.cvec])
        P.op("scalar", lambda e: e.activation(out=self.cvec[:], in_=self.cvec[:], func=AF.Silu),
             reads=[self.cvec], writes=[self.cvec])

    def modulation(self, wmod, bmod):
        P = self.P
        wst = [self.f32view(self.W[0], 0, 4096), self.f32view(self.W[1], 0, 4096),
               self.f32view(self.W[2], 0, 4096), self.f32view(self.W[3], 0, 4096)]
        wk = [self.W[0], self.W[1], self.W[2], self.W[3]]
        ps = self.PS[0]
        self.dma("sync", self.rowbuf.ap, bmod, writes=[self.rowbuf])
        for n in range(12):
            a, b = (0, 1) if n % 2 == 0 else (2, 3)
            src = wmod[:, n * 512:(n + 1) * 512].rearrange("(k p) n -> p k n", p=128)
            self.dma("sync", wst[a].rearrange("p (k n) -> p k n", k=8), src[:, 0:8, :], writes=[wk[a]])
            self.dma("sync", wst[b].rearrange("p (k n) -> p k n", k=8), src[:, 8:16, :], writes=[wk[b]])
            for k in range(16):
                wb = a if k < 8 else b
                kk = k % 8
                P.op("tensor", lambda e, wb=wb, kk=kk, k=k: e.matmul(
                    out=ps[0:1, :], lhsT=self.cvec[:, k:k + 1], rhs=wst[wb][:, kk * 512:(kk + 1) * 512],
                    start=(k == 0), stop=(k == 15)), reads=[self.cvec, wk[wb]], writes=[ps])
            addc = 0.0 if n < 4 else 1.0
            P.op("vector", lambda e, n=n, addc=addc: e.scalar_tensor_tensor(
                out=self.rowbuf.ap[0:1, n * 512:(n + 1) * 512], in0=ps[0:1, :], scalar=addc,
                in1=self.rowbuf.ap[0:1, n * 512:(n + 1) * 512], op0=ALU.add, op1=ALU.add),
                reads=[ps, self.rowbuf], writes=[self.rowbuf])
        self.bcast_mod()

    def bcast_mod(self):
        P = self.P
        for n in range(12):
            dst = [self.BC[1], self.BC[0], self.BC[2]][n // 4]
            psb = self.PS[1 + n % 2]
            P.op("tensor", lambda e, n=n, psb=psb: e.matmul(
                out=psb[:], lhsT=self.ones[0:1, :], rhs=self.rowbuf.ap[0:1, n * 512:(n + 1) * 512],
                start=True, stop=True), reads=[self.ones, self.rowbuf], writes=[psb])
            P.op("scalar", lambda e, n=n, psb=psb, dst=dst: e.activation(
                out=dst[:, (n % 4) * 512:(n % 4 + 1) * 512], in_=psb[:], func=AF.Copy),
                reads=[psb], writes=[dst])

    def make_hT(self, x_ap, ntok, hT, hT_key, router=None):
        P = self.P
        xt = [self.f32view(self.W[0], 0, 2048), self.f32view(self.W[1], 0, 2048)]
        xk = [self.W[0], self.W[1]]
        htf = self.f32view(self.W[2], 0, 2048)
        for t in range(ntok // 128):
            xb, xkk = xt[t % 2], xk[t % 2]
            self.dma("sync", xb, (x_ap(t) if callable(x_ap) else x_ap[t * 128:(t + 1) * 128, :]), writes=[xkk])
            if router is not None and "acc" in router:
                acc = router["acc"](t)
                P.op("scalar", lambda e, xb=xb, acc=acc: e.activation(out=acc, in_=xb, func=AF.Copy, scale=ALPHA),
                     reads=[xkk], writes=[(router["acckey"], t)])
            P.op("vector", lambda e, xb=xb: e.tensor_tensor(out=xb, in0=xb, in1=self.BC[0][:], op=ALU.mult),
                 reads=[xkk, self.BC[0]], writes=[xkk])
            P.op("vector", lambda e, xb=xb: e.tensor_tensor(out=xb, in0=xb, in1=self.BC[1][:], op=ALU.add),
                 reads=[xkk, self.BC[1]], writes=[xkk])
            for g in range(4):
                ps = self.PS[g % 4]
                for j in range(4):
                    k = g * 4 + j
                    P.op("tensor", lambda e, xb=xb, k=k, j=j, ps=ps: e.transpose(
                        out=ps[:, j * 128:(j + 1) * 128], in_=xb[:, k * 128:(k + 1) * 128], identity=self.ident[:]),
                        reads=[xkk, self.ident], writes=[ps])
                eng = "vector" if g % 2 == 0 else "scalar"
                dst = hT[:, g * 4:(g + 1) * 4, t * 128:(t + 1) * 128]
                src = ps[:].rearrange("p (a b) -> p a b", a=4)
                if eng == "vector":
                    P.op("vector", lambda e, dst=dst, src=src: e.tensor_copy(out=dst, in_=src),
                         reads=[ps], writes=[(hT_key, t)])
                else:
                    P.op("scalar", lambda e, dst=dst, src=src: e.activation(out=dst, in_=src, func=AF.Copy),
                         reads=[ps], writes=[(hT_key, t)])
                if router is not None:
                    P.op("gpsimd" if False else "vector", lambda e, src=src, g=g: e.tensor_copy(
                        out=htf[:, g * 512:(g + 1) * 512].rearrange("p (a b) -> p a b", a=4), in_=src),
                        reads=[ps], writes=[self.W[2]])
            if router is not None:
                self.route(t, htf, router)

    def route(self, t, htf, router):
        P = self.P
        ps = self.PS[4]
        for k in range(16):
            P.op("tensor", lambda e, k=k: e.matmul(out=ps[:, 0:36], lhsT=htf[:, k * 128:(k + 1) * 128],
                                                   rhs=self.wr[:, k, :], start=(k == 0), stop=(k == 15)),
                 reads=[self.W[2], self.wr], writes=[ps])
        rt = self.rt
        V = "vector"

        def v(fn, reads=(), writes=()):
            P.op(V, fn, reads=[rt] + list(reads), writes=[rt] + list(writes))
        v(lambda e: e.tensor_tensor(out=rt[:, 0:36], in0=ps[:, 0:36], in1=self.brt[:], op=ALU.add), reads=[ps, self.brt])
        v(lambda e: e.reduce_max(out=rt[:, 36:37], in_=rt[:, 0:4], axis=AX.X))
        v(lambda e: e.tensor_scalar(out=rt[:, 40:44], in0=rt[:, 0:4], scalar1=rt[:, 36:37], scalar2=None, op0=ALU.is_ge))
        v(lambda e: e.tensor_scalar(out=rt[:, 44:48], in0=rt[:, 0:4], scalar1=rt[:, 36:37], scalar2=None, op0=ALU.subtract))
        P.op("scalar", lambda e: e.activation(out=rt[:, 44:48], in_=rt[:, 44:48], func=AF.Exp, accum_out=rt[:, 37:38]),
             reads=[rt], writes=[rt])
        v(lambda e: e.reciprocal(out=rt[:, 38:39], in_=rt[:, 37:38]))
        v(lambda e: e.tensor_scalar(out=rt[:, 48:56], in0=rt[:, 4:12], scalar1=rt[:, 40:41], scalar2=None, op0=ALU.mult))
        for g in range(1, 4):
            v(lambda e, g=g: e.scalar_tensor_tensor(out=rt[:, 48:56], in0=rt[:, 4 + 8 * g:12 + 8 * g],
                                                    scalar=rt[:, 40 + g:41 + g], in1=rt[:, 48:56],
                                                    op0=ALU.mult, op1=ALU.add))
        v(lambda e: e.reduce_max(out=rt[:, 56:57], in_=rt[:, 48:56], axis=AX.X))
        v(lambda e: e.tensor_scalar(out=rt[:, 64:72], in0=rt[:, 48:56], scalar1=rt[:, 56:57], scalar2=None, op0=ALU.is_ge))
        v(lambda e: e.scalar_tensor_tensor(out=rt[:, 72:80], in0=rt[:, 64:72], scalar=NEG, in1=rt[:, 48:56],
                                           op0=ALU.mult, op1=ALU.add))
        v(lambda e: e.reduce_max(out=rt[:, 57:58], in_=rt[:, 72:80], axis=AX.X))
        v(lambda e: e.tensor_scalar(out=rt[:, 80:88], in0=rt[:, 72:80], scalar1=rt[:, 57:58], scalar2=None, op0=ALU.is_ge))
        v(lambda e: e.tensor_tensor(out=rt[:, 58:59], in0=rt[:, 57:58], in1=rt[:, 56:57], op=ALU.subtract))
        P.op("scalar", lambda e: e.activation(out=rt[:, 59:60], in_=rt[:, 58:59], func=AF.Exp), reads=[rt], writes=[rt])
        v(lambda e: e.tensor_scalar(out=rt[:, 60:61], in0=rt[:, 59:60], scalar1=1.0, scalar2=None, op0=ALU.add))
        v(lambda e: e.reciprocal(out=rt[:, 61:62], in_=rt[:, 60:61]))
        v(lambda e: e.tensor_tensor(out=rt[:, 62:63], in0=rt[:, 59:60], in1=rt[:, 61:62], op=ALU.mult))
        v(lambda e: e.tensor_scalar(out=rt[:, 64:72], in0=rt[:, 64:72], scalar1=rt[:, 61:62], scalar2=None, op0=ALU.mult))
        v(lambda e: e.scalar_tensor_tensor(out=rt[:, 64:72], in0=rt[:, 80:88], scalar=rt[:, 62:63], in1=rt[:, 64:72],
                                           op0=ALU.mult, op1=ALU.add))
        v(lambda e: e.tensor_scalar(out=rt[:, 64:72], in0=rt[:, 64:72], scalar1=rt[:, 38:39], scalar2=None, op0=ALU.mult))
        for g in range(4):
            P.op(V, lambda e, g=g: e.tensor_scalar(out=self.w32[:, t, g * 8:(g + 1) * 8], in0=rt[:, 64:72],
                                                   scalar1=rt[:, 40 + g:41 + g], scalar2=None, op0=ALU.mult),
                 reads=[rt], writes=[("w32", t)])

    def layer_norm(self, z, zkey, gt, bt, scr, scrkey):
        P = self.P
        st = self.small
        P.op("vector", lambda e: e.reduce_sum(out=st[:, 0:1], in_=z, axis=AX.X), reads=[zkey], writes=[st])
        P.op("vector", lambda e: e.tensor_scalar(out=st[:, 1:2], in0=st[:, 0:1], scalar1=-1.0 / D, scalar2=None, op0=ALU.mult),
             reads=[st], writes=[st])
        P.op("scalar", lambda e: e.activation(out=scr, in_=z, func=AF.Square, bias=st[:, 1:2], scale=1.0, accum_out=st[:, 2:3]),
             reads=[zkey, st], writes=[scrkey, st])
        P.op("vector", lambda e: e.tensor_scalar(out=st[:, 3:4], in0=st[:, 2:3], scalar1=1.0 / D, scalar2=LN_EPS, op0=ALU.mult, op1=ALU.add),
             reads=[st], writes=[st])
        P.op("scalar", lambda e: e.activation(out=st[:, 5:6], in_=st[:, 3:4], func=AF.Sqrt), reads=[st], writes=[st])
        P.op("vector", lambda e: e.reciprocal(out=st[:, 4:5], in_=st[:, 5:6]), reads=[st], writes=[st])
        P.op("vector", lambda e: e.tensor_scalar(out=z, in0=z, scalar1=st[:, 1:2], scalar2=st[:, 4:5], op0=ALU.add, op1=ALU.mult),
             reads=[zkey, st], writes=[zkey])
        P.op("vector", lambda e: e.tensor_tensor(out=z, in0=z, in1=gt[:], op=ALU.mult), reads=[zkey, gt], writes=[zkey])
        P.op("vector", lambda e: e.tensor_tensor(out=z, in0=z, in1=bt[:], op=ALU.add), reads=[zkey, bt], writes=[zkey])

    def mixer_params(self, kind, lam_init, lamv, subw, fbias, w_in):
        B = self
        P = self.P
        if kind == "A":
            lt = [B.f32view(B.W[4], i * 128, 128) for i in range(4)]
            for i in range(4):
                B.dma("sync", lt[i], lamv[i].partition_broadcast(128), writes=[B.W[4]])
            P.op("vector", lambda e: e.tensor_tensor(out=lt[0], in0=lt[0], in1=lt[1], op=ALU.mult), reads=[B.W[4]], writes=[B.W[4]])
            P.op("vector", lambda e: e.tensor_tensor(out=lt[2], in0=lt[2], in1=lt[3], op=ALU.mult), reads=[B.W[4]], writes=[B.W[4]])
            P.op("vector", lambda e: e.reduce_sum(out=B.lam[:, 2:3], in_=lt[0], axis=AX.X), reads=[B.W[4]], writes=[B.lam])
            P.op("vector", lambda e: e.reduce_sum(out=B.lam[:, 3:4], in_=lt[2], axis=AX.X), reads=[B.W[4]], writes=[B.lam])
            P.op("scalar", lambda e: e.activation(out=B.lam[:, 4:6], in_=B.lam[:, 2:4], func=AF.Exp), reads=[B.lam], writes=[B.lam])
            P.op("vector", lambda e: e.tensor_tensor(out=B.lam[:, 0:1], in0=B.lam[:, 4:5], in1=B.lam[:, 5:6], op=ALU.subtract), reads=[B.lam], writes=[B.lam])
            P.op("vector", lambda e: e.tensor_scalar(out=B.lam[:, 0:1], in0=B.lam[:, 0:1], scalar1=lam_init, scalar2=None, op0=ALU.add), reads=[B.lam], writes=[B.lam])
            P.op("vector", lambda e: e.tensor_scalar(out=B.lam[:, 1:2], in0=B.lam[:, 0:1], scalar1=-1.0, scalar2=None, op0=ALU.mult), reads=[B.lam], writes=[B.lam])
            B.dma("sync", B.subw[:], subw.partition_broadcast(128), writes=[B.subw])
            P.op("vector", lambda e: e.tensor_scalar(out=B.subw[:], in0=B.subw[:], scalar1=1.0 - lam_init, scalar2=None, op0=ALU.mult), reads=[B.subw], writes=[B.subw])
        else:
            B.dma("sync", B.fb[:], fbias.partition_broadcast(128), writes=[B.fb])
            B.dma("gpsimd", B.wf.ap, w_in[:, 6144:6160].rearrange("(k p) n -> p k n", p=128), writes=[B.wf])

    def qkv(self, l, kind, w_in, hT, hT_key, ntok, tok0, with_q, sc):
        P = self.P
        wb = [self.W[0], self.W[1]]
        nblk = 12
        first = 0 if with_q else 4
        bi = 0
        import os
        skip = os.environ.get("QKV_SKIP", "")
        for n in range(first, nblk):
            if (skip == "v" and n >= 8) or (skip == "qk" and n < 8):
                continue
            wt = wb[bi % 2]
            bi += 1
            wv = wt[:].rearrange("p (k n) -> p k n", k=16)
            self.dma("gpsimd", wv, w_in[:, n * 512:(n + 1) * 512].rearrange("(k p) n -> p k n", p=128), writes=[wt])
            if n < 8:
                isq = n < 4
                for tb in range(ntok // 512):
                    if kind == "A":
                        cs = [self.f32view(self.W[2], 0, 512), self.f32view(self.W[2], 512, 512)]
                        t0 = tok0 + tb * 512
                        self.dma("sync", cs[0], sc["cos"][:, t0:t0 + 512], writes=[(self.W[2].key, "c")])
                        self.dma("sync", cs[1], sc["sin"][:, t0:t0 + 512], writes=[(self.W[2].key, "s")])
                    for mi in range(4):
                        m = (n % 4) * 4 + mi
                        ps = self.PS[mi % 2]
                        for k in range(16):
                            P.op("tensor", lambda e, k=k, mi=mi, tb=tb, ps=ps, wv=wv: e.matmul(
                                out=ps[:], lhsT=wv[:, k, mi * 128:(mi + 1) * 128], rhs=hT[:, k, tb * 512:(tb + 1) * 512],
                                start=(k == 0), stop=(k == 15)), reads=[wt] + [(hT_key, tb * 4 + i) for i in range(4)], writes=[ps])
                        ob = self.W[3][:, (mi % 2) * 512:(mi % 2) * 512 + 512]
                        okey = (self.W[3].key, "o", mi % 2)
                        lvl = int(os.environ.get("QK_LEVEL", "9"))
                        if lvl == 0:
                            P.op("scalar", lambda e, ob=ob, ps=ps: e.activation(out=ob, in_=ps[:], func=AF.Copy),
                                 reads=[ps], writes=[okey])
                        elif kind == "A":
                            qraw = self.W[3][:, 1024 + (mi % 2) * 512:1024 + (mi % 2) * 512 + 512]
                            qkey = (self.W[3].key, "q", mi % 2)
                            ps2 = self.PS[2 + mi % 2]
                            t1 = self.f32view(self.W[3], 1024 + (mi % 2) * 1024, 512)
                            t2 = self.f32view(self.W[3], 1024 + (mi % 2) * 1024 + 512, 512)
                            tkey = (self.W[3].key, "t", mi % 2)
                            P.op("scalar", lambda e, qraw=qraw, ps=ps: e.activation(out=qraw, in_=ps[:], func=AF.Copy),
                                 reads=[ps], writes=[qkey])
                            P.op("tensor", lambda e, qraw=qraw, ps2=ps2: e.matmul(out=ps2[:], lhsT=self.rotT[:], rhs=qraw,
                                                                                   start=True, stop=True),
                                 reads=[qkey, self.rotT], writes=[ps2])
                            P.op("vector", lambda e, t1=t1, ps=ps, cs=cs: e.tensor_tensor(out=t1, in0=ps[:], in1=cs[0], op=ALU.mult),
                                 reads=[ps, (self.W[2].key, "c")], writes=[(tkey, 1)])
                            P.op("vector", lambda e, t2=t2, ps2=ps2, cs=cs: e.tensor_tensor(out=t2, in0=ps2[:], in1=cs[1], op=ALU.mult),
                                 reads=[ps2, (self.W[2].key, "s")], writes=[(tkey, 2)])
                            P.op("vector", lambda e, ob=ob, t1=t1, t2=t2: e.tensor_tensor(out=ob, in0=t1, in1=t2, op=ALU.add),
                                 reads=[(tkey, 1), (tkey, 2)], writes=[okey])
                        else:
                            P.op("scalar", lambda e, ob=ob, ps=ps: e.activation(out=ob, in_=ps[:], func=AF.Copy),
                                 reads=[ps], writes=[okey])
                        if isq:
                            dst = sc["qT"][m, :, tb * 512:(tb + 1) * 512]
                            dk = ("qT", m)
                        else:
                            dst = sc["kT"][m, :, tok0 + tb * 512:tok0 + (tb + 1) * 512]
                            dk = ("kT", m)
                        if os.environ.get("NO_STORE", "") != "1":
                            self.dma("sync", dst, ob, reads=[okey], writes=[dk])
            else:
                hv = 256 if kind == "A" else 128
                nh = 512 // hv
                for tt in range(ntok // 128):
                    ps = self.PS[4 + tt % 2]
                    for k in range(16):
                        P.op("tensor", lambda e, k=k, tt=tt, ps=ps, wv=wv: e.matmul(
                            out=ps[:], lhsT=hT[:, k, tt * 128:(tt + 1) * 128], rhs=wv[:, k, :],
                            start=(k == 0), stop=(k == 15)), reads=[wt, (hT_key, tt)], writes=[ps])
                    vb = self.W[2][:, 2048 + (tt % 2) * 1024:2048 + (tt % 2) * 1024 + nh * (hv + 1)].rearrange("p (h c) -> p h c", h=nh)
                    vkey = (self.W[2].key, "v", tt % 2)
                    P.op("scalar" if tt % 2 == 0 else "vector",
                         (lambda e, vb=vb, ps=ps: e.activation(out=vb[:, :, 0:hv], in_=ps[:].rearrange("p (h c) -> p h c", h=nh), func=AF.Copy))
                         if tt % 2 == 0 else
                         (lambda e, vb=vb, ps=ps: e.tensor_copy(out=vb[:, :, 0:hv], in_=ps[:].rearrange("p (h c) -> p h c", h=nh))),
                         reads=[ps], writes=[vkey])
                    P.op("gpsimd", lambda e, vb=vb: e.memset(vb[:, :, hv:hv + 1], 1.0), writes=[vkey])
                    h0 = (n - 8) * nh
                    ct = (tok0 // 128) + tt
                    self.dma("sync", sc["v"][h0:h0 + nh, ct, :, :].rearrange("h p c -> p h c"), vb, reads=[vkey], writes=[("v", h0)])
        if kind == "B":
            P.op("sync", None, reads=[self.wf])
            for tt in range(ntok // 128):
                ps = self.PS[6]
                ct = (tok0 // 128) + tt
                for k in range(16):
                    P.op("tensor", lambda e, k=k, tt=tt: e.matmul(out=ps[:, 0:16], lhsT=hT[:, k, tt * 128:(tt + 1) * 128],
                                                                  rhs=self.wf.ap[:, k, :], start=(k == 0), stop=(k == 15)),
                         reads=[self.wf, (hT_key, tt)], writes=[ps])
                st = self.small
                P.op("vector", lambda e: e.tensor_tensor(out=st[:, 16:32], in0=ps[:, 0:16], in1=self.fb[:], op=ALU.add),
                     reads=[ps, self.fb], writes=[st])
                P.op("scalar", lambda e: e.activation(out=st[:, 32:48], in_=st[:, 16:32], func=AF.Exp, scale=-1.0), reads=[st], writes=[st])
                P.op("scalar", lambda e: e.activation(out=st[:, 32:48], in_=st[:, 32:48], func=AF.Ln, bias=1.0, scale=1.0), reads=[st], writes=[st])
                P.op("vector", lambda e, ct=ct: e.tensor_scalar(out=self.lf[:, ct, :], in0=st[:, 32:48], scalar1=-1.0, scalar2=None, op0=ALU.mult),
                     reads=[st], writes=[("lf", ct)])

    def attention(self, l, kind, sc):
        P = self.P
        T, TP, TC = self.T, self.TP, self.TC
        nprev = TP // 128
        nq = T // 512
        scale = 128 ** -0.5
        if kind == "A":
            nheads, nmaps, hv = 8, 2, 256
        else:
            nheads, nmaps, hv = 16, 1, 128
        hv1 = hv + 1
        kTs = [self.BIGA[:, m * TC:(m + 1) * TC] for m in range(nmaps)]
        o0 = nmaps * TC
        qTs = [self.BIGA[:, o0 + m * T:o0 + (m + 1) * T] for m in range(nmaps)]
        o1 = o0 + nmaps * T
        v1 = self.BIGA[:, o1:o1 + (TC // 128) * hv1].rearrange("p (t c) -> p t c", c=hv1)
        assert o1 + (TC // 128) * hv1 <= 32768
        if kind == "B":
            ps = self.PS[7]
            for t in range(TC // 128):
                for t2 in range(t + 1):
                    P.op("tensor", lambda e, t=t, t2=t2: e.matmul(
                        out=ps[:, 0:16], lhsT=(self.tri[:] if t2 == t else self.ones[:]), rhs=self.lf[:, t2, :],
                        start=(t2 == 0), stop=(t2 == t)), reads=[("lf", t2), self.tri, self.ones], writes=[ps])
                P.op("vector", lambda e, t=t: e.tensor_copy(out=self.cum[:, t, :], in_=ps[:, 0:16]), reads=[ps], writes=[("cum", t)])
            for g in range(nq):
                ta = (TP + g * 512 + 256) // 128
                for t2 in range(ta):
                    P.op("tensor", lambda e, t2=t2, ta=ta: e.matmul(out=ps[:, 0:16], lhsT=self.ones[:], rhs=self.lf[:, t2, :],
                                                                    start=(t2 == 0), stop=(t2 == ta - 1)),
                         reads=[("lf", t2), self.ones], writes=[ps])
                P.op("vector", lambda e, g=g: e.tensor_copy(out=self.anc[:, g, :], in_=ps[:, 0:16]), reads=[ps], writes=[("anc", g)])
        ebuf = [self.W[0], self.W[1]]
        for h in range(nheads):
            for m in range(nmaps):
                mm = h * nmaps + m
                self.dma("sync", kTs[m], sc["kT"][mm, :, :], reads=[("kT", mm)], writes=[("kTs", m)])
                self.dma("sync", qTs[m], sc["qT"][mm, :, :], reads=[("qT", mm)], writes=[("qTs", m)])
            self.dma("sync", v1, sc["v"][h, :, :, :].rearrange("t p c -> p t c"), reads=[("v", (h // (512 // hv)) * (512 // hv))], writes=["v1"])
            for g in range(nq):
                nkb = nprev + 4 * g + 4
                o1n = self.f32view(self.W[2], 0, 1024).rearrange("p (i c) -> p i c", i=4)
                for m in range(nmaps):
                    def emit_S(j, m=m, g=g):
                        pss = self.PS[4 + j % 2]
                        P.op("tensor", lambda e, m=m, j=j, g=g, pss=pss: e.matmul(
                            out=pss[:], lhsT=kTs[m][:, j * 128:(j + 1) * 128], rhs=qTs[m][:, g * 512:(g + 1) * 512],
                            start=True, stop=True), reads=[("kTs", m), ("qTs", m)], writes=[pss])

                    emit_S(0)
                    for j in range(nkb):
                        if j + 1 < nkb:
                            emit_S(j + 1)
                        pss = self.PS[4 + j % 2]
                        et = ebuf[j % 2]
                        ev = et[:, 0:512]
                        if kind == "A":
                            bias = self.pbias[:, 0:1] if j < nprev else self.zero[:, 0:1]
                            breads = [self.pbias, self.zero]
                        else:
                            bcol = self.small[:, 48 + (j % 2):49 + (j % 2)]
                            P.op("vector", lambda e, bcol=bcol, g=g, j=j, h=h: e.tensor_tensor(
                                out=bcol, in0=self.anc[:, g, h:h + 1], in1=self.cum[:, j, h:h + 1], op=ALU.subtract),
                                reads=[("anc", g), ("cum", j)], writes=[("bcol", j % 2)])
                            if j < nprev:
                                P.op("vector", lambda e, bcol=bcol: e.tensor_tensor(out=bcol, in0=bcol, in1=self.pbias[:, 0:1], op=ALU.add),
                                     reads=[("bcol", j % 2), self.pbias], writes=[("bcol", j % 2)])
                            bias = bcol
                            breads = [("bcol", j % 2)]
                        P.op("scalar", lambda e, ev=ev, pss=pss, bias=bias: e.activation(out=ev, in_=pss[:], func=AF.Exp, bias=bias, scale=scale),
                             reads=[pss] + breads, writes=[et])
                        jo = j - nprev
                        for i in range(4):
                            qi = 4 * g + i
                            if jo > qi:
                                continue
                            if jo == qi:
                                P.op("gpsimd", lambda e, ev=ev, i=i: e.tensor_tensor(
                                    out=ev[:, i * 128:(i + 1) * 128], in0=ev[:, i * 128:(i + 1) * 128], in1=self.trib[:], op=ALU.mult),
                                    reads=[et, self.trib], writes=[et])
                            pso = self.PS[i]
                            last = nprev + qi
                            P.op("tensor", lambda e, ev=ev, i=i, j=j, pso=pso, last=last: e.matmul(
                                out=pso[:, 0:hv1], lhsT=ev[:, i * 128:(i + 1) * 128], rhs=v1[:, j, :],
                                start=(j == 0), stop=(j == last)), reads=[et, "v1"], writes=[pso])
                    st = self.small
                    for i in range(4):
                        pso = self.PS[i]
                        qi = 4 * g + i
                        P.op("vector", lambda e, pso=pso, i=i: e.reciprocal(out=st[:, 8 + i:9 + i], in_=pso[:, hv:hv1]), reads=[pso], writes=[st])
                        if kind == "A" and m == 0:
                            P.op("vector", lambda e, pso=pso, i=i: e.tensor_scalar(
                                out=o1n[:, i, :], in0=pso[:, 0:hv], scalar1=st[:, 8 + i:9 + i], scalar2=None, op0=ALU.mult),
                                reads=[pso, st], writes=[(self.W[2].key, "o1n", i)])
                        elif kind == "A":
                            ot = self.f32view(self.W[3], i * 256, 256)
                            okey = (self.W[3].key, "ot", i)
                            P.op("vector", lambda e, i=i: e.tensor_tensor(out=st[:, 12 + i:13 + i], in0=st[:, 8 + i:9 + i], in1=self.lam[:, 1:2], op=ALU.mult),
                                 reads=[st, self.lam], writes=[st])
                            P.op("vector", lambda e, pso=pso, i=i, ot=ot: e.scalar_tensor_tensor(
                                out=ot, in0=pso[:, 0:hv], scalar=st[:, 12 + i:13 + i], in1=o1n[:, i, :], op0=ALU.mult, op1=ALU.add),
                                reads=[pso, st, (self.W[2].key, "o1n", i)], writes=[okey])
                            sq = self.f32view(self.W[3], 1024 + i * 256, 256)
                            P.op("scalar", lambda e, ot=ot, sq=sq, i=i: e.activation(out=sq, in_=ot, func=AF.Square, accum_out=st[:, 16 + i:17 + i]),
                                 reads=[okey], writes=[(self.W[3].key, "sq", i), st])
                            P.op("vector", lambda e, i=i: e.tensor_scalar(out=st[:, 20 + i:21 + i], in0=st[:, 16 + i:17 + i], scalar1=1.0 / 256,
                                                                         scalar2=RMS_EPS, op0=ALU.mult, op1=ALU.add), reads=[st], writes=[st])
                            P.op("scalar", lambda e, i=i: e.activation(out=st[:, 24 + i:25 + i], in_=st[:, 20 + i:21 + i], func=AF.Sqrt), reads=[st], writes=[st])
                            P.op("vector", lambda e, i=i: e.reciprocal(out=st[:, 28 + i:29 + i], in_=st[:, 24 + i:25 + i]), reads=[st], writes=[st])
                            ob = self.W[3][:, 4096 + i * 256:4096 + (i + 1) * 256]
                            obk = (self.W[3].key, "ob", i)
                            P.op("vector", lambda e, ot=ot, ob=ob, i=i: e.scalar_tensor_tensor(
                                out=ob, in0=ot, scalar=st[:, 28 + i:29 + i], in1=self.subw[:], op0=ALU.mult, op1=ALU.mult),
                                reads=[okey, st, self.subw], writes=[obk])
                            self.dma("sync", sc["o"][qi * 128:(qi + 1) * 128, h * 256:(h + 1) * 256], ob, reads=[obk], writes=[("o", qi)])
                        else:
                            ob = self.W[3][:, 4096 + i * 128:4096 + (i + 1) * 128]
                            obk = (self.W[3].key, "ob", i)
                            P.op("vector", lambda e, pso=pso, ob=ob, i=i: e.tensor_scalar(
                                out=ob, in0=pso[:, 0:hv], scalar1=st[:, 8 + i:9 + i], scalar2=None, op0=ALU.mult),
                                reads=[pso, st], writes=[obk])
                            self.dma("sync", sc["o"][qi * 128:(qi + 1) * 128, h * 128:(h + 1) * 128], ob, reads=[obk], writes=[("o", qi)])

    def out_proj(self, l, w_out, x_ap, lng, lnb, sc, x1_ap):
        P = self.P
        T = self.T
        wo = [self.W[i] for i in range(4)]
        for n in range(4):
            self.dma("gpsimd", wo[n][:].rearrange("p (k n) -> p k n", k=16),
                     w_out[:, n * 512:(n + 1) * 512].rearrange("(k p) n -> p k n", p=128), writes=[wo[n]])
        self.dma("sync", self.BC[0][:], lng.partition_broadcast(128), writes=[self.BC[0]])
        self.dma("sync", self.BC[1][:], lnb.partition_broadcast(128), writes=[self.BC[1]])
        for t in range(T // 128):
            ob = self.BIGB[:, (t % 2) * 2048:(t % 2 + 1) * 2048]
            obk = ("ob", t % 2)
            self.dma("sync", ob, sc["o"][t * 128:(t + 1) * 128, :], reads=[("o", t)], writes=[obk])
            xt = self.f32view(self.BIGA, (t % 2) * 2048, 2048)
            xk = ("xt", t % 2)
            self.dma("sync", xt, x_ap[t * 128:(t + 1) * 128, :], writes=[xk])
            oT = self.BIGB[:, 4096 + (t % 2) * 2048:4096 + (t % 2 + 1) * 2048].rearrange("p (k n) -> p k n", k=16)
            oTk = ("oT", t % 2)
            for g in range(4):
                ps = self.PS[4 + g % 2]
                psb = ps.h.bitcast(BF16)
                for j in range(4):
                    k = g * 4 + j
                    P.op("tensor", lambda e, ob=ob, k=k, j=j, psb=psb: e.transpose(
                        out=psb[:, j * 128:(j + 1) * 128], in_=ob[:, k * 128:(k + 1) * 128], identity=self.identb[:]),
                        reads=[obk, self.identb], writes=[ps])
                P.op("scalar" if g % 2 else "vector",
                     (lambda e, oT=oT, psb=psb, g=g: e.activation(out=oT[:, g * 4:(g + 1) * 4, :], in_=psb[:, 0:512].rearrange("p (a b) -> p a b", a=4), func=AF.Copy))
                     if g % 2 else
                     (lambda e, oT=oT, psb=psb, g=g: e.tensor_copy(out=oT[:, g * 4:(g + 1) * 4, :], in_=psb[:, 0:512].rearrange("p (a b) -> p a b", a=4))),
                     reads=[ps], writes=[oTk])
            y = self.f32view(self.BIGA, 4096 + (t % 2) * 2048, 2048)
            yk = ("y", t % 2)
            for n in range(4):
                ps = self.PS[n]
                for k in range(16):
                    P.op("tensor", lambda e, n=n, k=k, oT=oT, ps=ps: e.matmul(
                        out=ps[:], lhsT=oT[:, k, :], rhs=wo[n][:, k * 512:(k + 1) * 512], start=(k == 0), stop=(k == 15)),
                        reads=[oTk, wo[n]], writes=[ps])
                P.op("vector", lambda e, n=n, ps=ps, y=y: e.tensor_tensor(
                    out=y[:, n * 512:(n + 1) * 512], in0=ps[:], in1=self.BC[2][:, n * 512:(n + 1) * 512], op=ALU.mult),
                    reads=[ps, self.BC[2]], writes=[yk])
            P.op("vector", lambda e, xt=xt, y=y: e.scalar_tensor_tensor(out=xt, in0=xt, scalar=ALPHA, in1=y, op0=ALU.mult, op1=ALU.add),
                 reads=[xk, yk], writes=[xk])
            self.layer_norm(xt, xk, self.BC[0], self.BC[1], y, yk)
            self.dma("sync", x1_ap[t * 128:(t + 1) * 128, :], xt, reads=[xk], writes=[("x1", t)])

    def moe(self, l, W, x1_ap, out_ap, outkey):
        P = self.P
        T = self.T
        TPASS = min(1024, T)
        npass = T // TPASS
        self.dma("sync", self.wr[:, :, 0:4], W["w_group"].rearrange("(k p) g -> p k g", p=128), writes=[self.wr])
        for g in range(4):
            self.dma("sync", self.wr[:, :, 4 + 8 * g:12 + 8 * g], W["w_router"][g].rearrange("(k p) e -> p k e", p=128), writes=[self.wr])
        self.dma("sync", self.brt[:, 0:4], W["b_group"].partition_broadcast(128), writes=[self.brt])
        self.dma("sync", self.brt[:, 4:36], W["b_router"].partition_broadcast(128), writes=[self.brt])
        if npass > 1:
            if not hasattr(self, "bcsave"):
                self.bcsave = self.dscr("s_bcsave", [2, 128, 2048], F32)
            self.dma("sync", self.bcsave[0], self.BC[0][:], reads=[self.BC[0]], writes=["bcsave0"])
            self.dma("sync", self.bcsave[1], self.BC[1][:], reads=[self.BC[1]], writes=["bcsave1"])
        ntile = TPASS // 128
        hT2 = self.BIGB[:, 0:16 * TPASS].rearrange("p (k t) -> p k t", k=16)
        accv = self.BIGA.h.bitcast(F32)
        hidbufs = [self.BC[0].h.bitcast(BF16)[:, i * 2048:(i + 1) * 2048].rearrange("p (f t) -> p f t", f=4) for i in range(2)]
        sgbufs = [self.BC[1][:, i * 512:(i + 1) * 512] for i in range(2)]
        for ps_i in range(npass):
            P.fence()
            tok0 = ps_i * TPASS
            if ps_i > 0:
                self.dma("sync", self.BC[0][:], self.bcsave[0], writes=[self.BC[0]])
                self.dma("sync", self.BC[1][:], self.bcsave[1], writes=[self.BC[1]])
            self.make_hT(x1_ap[tok0:tok0 + TPASS, :], TPASS, hT2, "hT2", router=dict())
            P.fence()
            hi = 0
            for ex in range(32):
                g, e = ex // 8, ex % 8
                wg = self.W[0 + ex % 2]
                wu = self.W[2 + ex % 2]
                wd = self.W[4]
                wgv = wg[:].rearrange("p (k n) -> p k n", k=16)
                wuv = wu[:].rearrange("p (k n) -> p k n", k=16)
                wdv = wd[:].rearrange("p (k n) -> p k n", k=4)
                self.dma("gpsimd", wgv, W["w_gate"][g, e].rearrange("(k p) n -> p k n", p=128), writes=[wg])
                self.dma("gpsimd", wuv, W["w_up"][g, e].rearrange("(k p) n -> p k n", p=128), writes=[wu])
                self.dma("gpsimd", wdv, W["w_down"][g, e].rearrange("(k p) n -> p k n", p=128), writes=[wd])
                for tb in range(TPASS // 512):
                    hb = hi % 2
                    hi += 1
                    hidb = hidbufs[hb]
                    for fc in range(4):
                        psg = self.PS[fc % 2]
                        psu = self.PS[2 + fc % 2]
                        for k in range(16):
                            P.op("tensor", lambda e_, k=k, fc=fc, tb=tb, psg=psg, wgv=wgv: e_.matmul(
                                out=psg[:], lhsT=wgv[:, k, fc * 128:(fc + 1) * 128], rhs=hT2[:, k, tb * 512:(tb + 1) * 512],
                                start=(k == 0), stop=(k == 15)), reads=[wg] + [("hT2", tb * 4 + i) for i in range(4)], writes=[psg])
                        for k in range(16):
                            P.op("tensor", lambda e_, k=k, fc=fc, tb=tb, psu=psu, wuv=wuv: e_.matmul(
                                out=psu[:], lhsT=wuv[:, k, fc * 128:(fc + 1) * 128], rhs=hT2[:, k, tb * 512:(tb + 1) * 512],
                                start=(k == 0), stop=(k == 15)), reads=[wu] + [("hT2", tb * 4 + i) for i in range(4)], writes=[psu])
                        sg = sgbufs[fc % 2]
                        sgk = ("sg", fc % 2)
                        P.op("scalar", lambda e_, sg=sg, psg=psg: e_.activation(out=sg, in_=psg[:], func=AF.Silu), reads=[psg], writes=[sgk])
                        P.op("vector", lambda e_, sg=sg, psu=psu, hidb=hidb, fc=fc: e_.tensor_tensor(
                            out=hidb[:, fc, :], in0=psu[:], in1=sg, op=ALU.mult), reads=[psu, sgk], writes=[("hid", hb, fc)])
                    for tt in range(4):
                        t = tb * 4 + tt
                        for dmb in range(4):
                            psd = self.PS[4 + (tt * 4 + dmb) % 4]
                            for fc in range(4):
                                P.op("tensor", lambda e_, fc=fc, tt=tt, dmb=dmb, psd=psd, hidb=hidb, wdv=wdv: e_.matmul(
                                    out=psd[:], lhsT=hidb[:, fc, tt * 128:(tt + 1) * 128], rhs=wdv[:, fc, dmb * 512:(dmb + 1) * 512],
                                    start=(fc == 0), stop=(fc == 3)), reads=[("hid", hb, fc), wd], writes=[psd])
                            av = accv[:, t * 2048 + dmb * 512:t * 2048 + (dmb + 1) * 512]
                            if ex == 0:
                                P.op("vector", lambda e_, psd=psd, av=av, t=t, ex=ex: e_.tensor_scalar(
                                    out=av, in0=psd[:], scalar1=self.w32[:, t, ex:ex + 1], scalar2=None, op0=ALU.mult),
                                    reads=[psd, ("w32", t)], writes=[("acc", t)])
                            else:
                                P.op("vector", lambda e_, psd=psd, av=av, t=t, ex=ex: e_.scalar_tensor_tensor(
                                    out=av, in0=psd[:], scalar=self.w32[:, t, ex:ex + 1], in1=av, op0=ALU.mult, op1=ALU.add),
                                    reads=[psd, ("w32", t), ("acc", t)], writes=[("acc", t)])
            P.fence()
            self.dma("sync", self.f32view(self.W[0], 0, 2048), W["ln_g"].partition_broadcast(128), writes=[self.W[0]])
            self.dma("sync", self.f32view(self.W[1], 0, 2048), W["ln_b"].partition_broadcast(128), writes=[self.W[1]])
            gt = _View(self.f32view(self.W[0], 0, 2048), self.W[0].key)
            bt = _View(self.f32view(self.W[1], 0, 2048), self.W[1].key)
            for t in range(ntile):
                z = accv[:, t * 2048:(t + 1) * 2048]
                xt = self.f32view(self.W[3], (t % 2) * 2048, 2048)
                xk = (self.W[3].key, "xt", t % 2)
                self.dma("sync", xt, x1_ap[tok0 + t * 128:tok0 + (t + 1) * 128, :], writes=[xk])
                P.op("vector", lambda e_, z=z: e_.tensor_tensor(out=z, in0=z, in1=self.BC[2][:], op=ALU.mult),
                     reads=[("acc", t), self.BC[2]], writes=[("acc", t)])
                P.op("vector", lambda e_, z=z, xt=xt: e_.scalar_tensor_tensor(out=z, in0=xt, scalar=ALPHA, in1=z, op0=ALU.mult, op1=ALU.add),
                     reads=[("acc", t), xk], writes=[("acc", t)])
                scr = self.f32view(self.W[2], (t % 2) * 2048, 2048)
                self.layer_norm(z, ("acc", t), gt, bt, scr, (self.W[2].key, "scr", t % 2))
                self.dma("sync", out_ap[tok0 + t * 128:tok0 + (t + 1) * 128, :], z, reads=[("acc", t)], writes=[(outkey, tok0 // 128 + t)])

    def rowhid(self, tb):
        o = 8192 + (tb % 2) * 2048
        return self.BIGB[:, o:o + 2048].rearrange("p (f t) -> p f t", f=4)

    def sgbuf(self, i):
        return self.BIGB.h.bitcast(F32)[:, 6144 + i * 512:6144 + (i + 1) * 512]


Buf.register = None


def _is_buf(x):
    return isinstance(x, (Buf, _View))


def _k(x):
    return x.key if isinstance(x, (Buf, _View)) else x


def build_program(T, TP, layers, ncores=8):
    B = Builder(T, TP, layers)
    nc, P = B.nc, B.P
    TC = T + TP
    B.load_consts()
    xo = B.din("xo", [T, D])
    xp = B.din("xp", [TP, D])
    cosT = B.din("c_cos", [128, TC])
    sinT = B.din("c_sin", [128, TC])
    out = nc.dram_tensor("out", [T, D], F32, kind="ExternalOutput").ap()
    sc = dict(
        qT=B.dscr("s_qT", [16, 128, T], BF16), kT=B.dscr("s_kT", [16, 128, TC], BF16),
        o=B.dscr("s_o", [T, D], BF16), cos=cosT, sin=sinT)
    vA = B.dscr("s_vA", [8, TC // 128, 128, 257], BF16)
    vB = B.dscr("s_vB", [16, TC // 128, 128, 129], BF16)
    x1 = B.dscr("s_x1", [T, D], F32)
    CH = 256
    nch = T // CH
    if len(layers) > 1:
        x2 = B.dscr("s_x2", [T, D], F32)
        gath = B.dscr("s_gath", [nch, 2 * CH, D], F32)
    cur_o, cur_p = xo, xp
    for li, l in enumerate(layers):
        kind = "A" if l % 2 == 0 else "B"
        last = (li == len(layers) - 1)
        sc["v"] = vA if kind == "A" else vB
        wmod = B.din("mix_mod_w%d" % l, [D, 6144])
        bmod = B.din("mix_mod_b%d" % l, [1, 6144])
        lng = B.din("mix_ln_g%d" % l, [1, D])
        lnb = B.din("mix_ln_b%d" % l, [1, D])
        lam_init = lamv = subw = fbias = None
        if kind == "A":
            w_in = B.din("a_w_in", [D, 6144])
            w_out = B.din("a_w_out", [D, D])
            lamv = [B.din("a_lam_q1", [1, 128]), B.din("a_lam_k1", [1, 128]),
                    B.din("a_lam_q2", [1, 128]), B.din("a_lam_k2", [1, 128])]
            subw = B.din("a_subln_w", [1, 256])
            lam_init = 0.8 - 0.6 * math.exp(-0.3 * l)
        else:
            w_in = B.din("b_w_in", [D, 6160])
            w_out = B.din("b_w_out", [D, D])
            fbias = B.din("b_forget_bias", [1, 16])
        Wm = dict(
            w_group=B.din("moe_w_group%d" % l, [D, 4]), b_group=B.din("moe_b_group%d" % l, [1, 4]),
            w_router=B.din("moe_w_router%d" % l, [4, D, 8]), b_router=B.din("moe_b_router%d" % l, [1, 32]),
            w_gate=B.din("moe_w_gate%d" % l, [4, 8, D, 512]), w_up=B.din("moe_w_up%d" % l, [4, 8, D, 512]),
            w_down=B.din("moe_w_down%d" % l, [4, 8, 512, D]),
            ln_g=B.din("ffn_ln_g%d" % l, [1, D]), ln_b=B.din("ffn_ln_b%d" % l, [1, D]))
        fwmod = B.din("ffn_mod_w%d" % l, [D, 6144])
        fbmod = B.din("ffn_mod_b%d" % l, [1, 6144])

        P.fence()
        B.modulation(wmod, bmod)
        B.mixer_params(kind, lam_init, lamv, subw, fbias, w_in)
        hT = B.BIGA[:, 0:16 * max(T, TP)].rearrange("p (k t) -> p k t", k=16)
        for (x_ap, ntok, tok0, with_q) in ((cur_p, TP, 0, False), (cur_o, T, TP, True)):
            P.fence()
            B.make_hT(x_ap, ntok, hT[:, :, 0:ntok], "hT")
            P.fence()
            B.qkv(l, kind, w_in, hT[:, :, 0:ntok], "hT", ntok, tok0, with_q, sc)
        P.fence()
        B.attention(l, kind, sc)
        P.fence()
        B.out_proj(l, w_out, cur_o, lng, lnb, sc, x1)
        P.fence()
        B.modulation(fwmod, fbmod)
        B.moe(l, Wm, x1, out if last else x2, "out" if last else "x2")
        if not last:
            P.fence()
            groups = [[2 * i, 2 * i + 1] for i in range(ncores // 2)]
            for ch in range(nch):
                if os.environ.get("K_NOCC"):
                    B.dma("sync", gath[ch, 0:CH, :], x2[ch * CH:(ch + 1) * CH, :], writes=[("gath", ch)])
                    continue
                P.op("gpsimd", lambda e, ch=ch: e.collective_compute(
                    "AllGather", ALU.bypass, replica_groups=groups,
                    ins=[x2[ch * CH:(ch + 1) * CH, :]], outs=[gath[ch]]),
                    writes=[("gath", ch)], dma=True, cc=True)
            P.fence()
            cur_o = x2
            cur_p = (lambda t: gath[t // 2, (t % 2) * 128:(t % 2 + 1) * 128, :])
    P.fence()
    P.op("sync", None, reads=[])
    P.emit()
    return nc, B


def _consts(T, TP, p):
    TC = T + TP
    ident = np.eye(128, dtype=np.float32)
    tri = np.triu(np.ones((128, 128), np.float32))
    R = np.zeros((128, 128), np.float32)
    for i in range(64):
        R[i, i + 64] = -1.0
        R[i + 64, i] = 1.0
    rotT = np.ascontiguousarray(R.T)
    pos = np.concatenate([np.arange(TP), p * T + np.arange(T)]).astype(np.float32)
    inv = np.power(np.float32(10000.0), -np.arange(0, 128, 2, dtype=np.float32) / 128).astype(np.float32)
    ang = pos[None, :] * np.concatenate([inv, inv])[:, None]
    return dict(c_ident=ident, c_tri=tri, c_rotT=rotT,
                c_pbias=np.full((128, 1), 0.0 if p == 1 else NEG, np.float32),
                c_cos=np.cos(ang).astype(np.float32), c_sin=np.sin(ang).astype(np.float32))


_PROG_CACHE = {}
_RUN_KW = {}
_LAST = {}


def run_layers(layers, x_in, inp, T, TP, ncores):
    key = (tuple(layers), T, TP, ncores)
    if key not in _PROG_CACHE:
        _PROG_CACHE[key] = build_program(T, TP, list(layers), ncores)
    nc, B = _PROG_CACHE[key]
    f = np.float32
    shared = {}
    for l in layers:
        j = l // 2
        shared["mix_mod_w%d" % l] = inp["mix_mod_w"][l]
        shared["mix_mod_b%d" % l] = inp["mix_mod_b"][l].reshape(1, -1)
        shared["mix_ln_g%d" % l] = inp["mix_ln_g"][l].reshape(1, -1)
        shared["mix_ln_b%d" % l] = inp["mix_ln_b"][l].reshape(1, -1)
        if l % 2 == 0:
            shared["a_w_in"] = inp["a_w_in"][j]
            shared["a_w_out"] = inp["a_w_out"][j]
            shared["a_lam_q1"] = inp["a_lam_q1"][j].reshape(1, -1)
            shared["a_lam_k1"] = inp["a_lam_k1"][j].reshape(1, -1)
            shared["a_lam_q2"] = inp["a_lam_q2"][j].reshape(1, -1)
            shared["a_lam_k2"] = inp["a_lam_k2"][j].reshape(1, -1)
            shared["a_subln_w"] = inp["a_subln_w"][j].reshape(1, -1)
        else:
            shared["b_w_in"] = inp["b_w_in"][j]
            shared["b_w_out"] = inp["b_w_out"][j]
            shared["b_forget_bias"] = inp["b_forget_bias"][j].reshape(1, -1)
        shared["ffn_mod_w%d" % l] = inp["ffn_mod_w"][l]
        shared["ffn_mod_b%d" % l] = inp["ffn_mod_b"][l].reshape(1, -1)
        shared["ffn_ln_g%d" % l] = inp["ffn_ln_g"][l].reshape(1, -1)
        shared["ffn_ln_b%d" % l] = inp["ffn_ln_b"][l].reshape(1, -1)
        shared["moe_w_group%d" % l] = inp["moe_w_group"][l]
        shared["moe_b_group%d" % l] = inp["moe_b_group"][l].reshape(1, -1)
        shared["moe_w_router%d" % l] = inp["moe_w_router"][l]
        shared["moe_b_router%d" % l] = inp["moe_b_router"][l].reshape(1, -1)
        shared["moe_w_gate%d" % l] = inp["moe_w_gate"][l]
        shared["moe_w_up%d" % l] = inp["moe_w_up"][l]
        shared["moe_w_down%d" % l] = inp["moe_w_down"][l]
    shared = {k: np.ascontiguousarray(np.asarray(v, dtype=f)) for k, v in shared.items() if k in B.inputs}
    in_maps = []
    for c in range(ncores):
        b, p = c // 2, c % 2
        m = _consts(T, TP, p)
        m["xo"] = x_in[b, p * T:(p + 1) * T]
        m["xp"] = x_in[b, 0:TP]
        m["cvec_in"] = inp["c"][b].reshape(16, 128).T
        m = {k: np.ascontiguousarray(np.asarray(v, dtype=f)) for k, v in m.items() if k in B.inputs}
        m.update(shared)
        in_maps.append(m)
    res = run_bass_kernel_spmd(nc, in_maps, core_ids=list(range(ncores)), **_RUN_KW)
    _LAST['res'] = res
    out = np.empty_like(x_in)
    for c in range(ncores):
        b, p = c // 2, c % 2
        out[b, p * T:(p + 1) * T] = res.results[c]["out"]
    return out


def run_layer(l, x_in, inp, T, TP, ncores, stop=99):
    return run_layers([l], x_in, inp, T, TP, ncores)


def kernel(**inputs):
    inp = {k: np.asarray(v) for k, v in inputs.items()}
    x = np.asarray(inp["x"], dtype=np.float32)
    Bn, S, _ = x.shape
    T = S // 2
    ncores = Bn * 2
    return run_layers(list(range(DEPTH)), x, inp, T, T, ncores)
```

```python
import math
import os
import numpy as np
import concourse.bass as bass
import concourse.mybir as mybir
from concourse.bass_utils import run_bass_kernel_spmd

F32 = mybir.dt.float32
BF16 = mybir.dt.bfloat16
AF = mybir.ActivationFunctionType
ALU = mybir.AluOpType
AX = mybir.AxisListType

D = 2048
KC = 16
DEPTH = 2
ALPHA = (2.0 * DEPTH) ** 0.25
LN_EPS = 1e-5
RMS_EPS = 1e-5
NEG = -30000.0
COMPUTE = ("tensor", "vector", "scalar", "gpsimd")
NDMASEM = 8


class Buf:
    _n = 0

    def __init__(self, h, name, psum=False):
        self.h = h
        self.psum = psum
        Buf._n += 1
        self.key = name + "#" + str(Buf._n)

    def __getitem__(self, idx):
        return self.h[idx]


def _k(x):
    return x.key if isinstance(x, Buf) else x


class _View:
    def __init__(self, ap, key):
        self.ap = ap
        self.key = key

    def __getitem__(self, idx):
        return self.ap


class Prog:
    def __init__(self, nc):
        self.nc = nc
        self.ops = []
        self.last_w = {}
        self.readers = {}
        self.last_eng = {}
        self.last_dma = {}
        self.cc_ops = []

    def sbuf(self, name, shape, dtype):
        return Buf(self.nc.alloc_sbuf_tensor(name, list(shape), dtype), name)

    def psum(self, name, shape, dtype=F32):
        return Buf(self.nc.alloc_psum_tensor(name, list(shape), dtype), name, psum=True)

    def op(self, eng, fn, reads=(), writes=(), dma=False, cc=False):
        i = len(self.ops)
        deps = set()
        pr = [r for r in reads if isinstance(r, Buf) and r.psum]
        if pr:
            reads = [r for r in reads if not (isinstance(r, Buf) and r.psum)]
            writes = list(writes) + [r for r in pr if r not in writes]
        for r in reads:
            k = _k(r)
            if k in self.last_w:
                deps.add(self.last_w[k])
        for w in writes:
            k = _k(w)
            if k in self.last_w:
                deps.add(self.last_w[k])
            for rd in self.readers.get(k, ()):
                deps.add(rd)
        for r in reads:
            self.readers.setdefault(_k(r), []).append(i)
        for w in writes:
            k = _k(w)
            self.last_w[k] = i
            self.readers[k] = []
        deps.discard(i)
        self.ops.append(dict(eng=eng, fn=fn, deps=deps, dma=dma, signal=False, cc=cc))
        if cc:
            self.cc_ops.append(i)
        elif dma:
            self.last_dma.setdefault(eng, []).append(i)
            self.last_dma[eng] = self.last_dma[eng][-NDMASEM:]
        else:
            self.last_eng[eng] = i
        return i

    def fence(self):
        deps = set(self.last_eng.values())
        for v in self.last_dma.values():
            deps.update(v)
        deps.update(self.cc_ops[-1:])
        for e in COMPUTE + ("sync",):
            self.ops.append(dict(eng=e, fn=None, deps=set(deps), dma=False, signal=False, cc=False))
        self.last_w = {}
        self.readers = {}

    def emit(self):
        nc = self.nc
        ops = self.ops

        def dom(o):
            if o["cc"]:
                return "cc"
            return ("dma", o["eng"]) if o["dma"] else o["eng"]

        ccsem = nc.alloc_semaphore(name="s_cc") if self.cc_ops else None
        ncc = 0
        for o in ops:
            if o["cc"]:
                ncc += 1
            o["ncc_before"] = ncc
        for o in ops:
            for d in o["deps"]:
                od = ops[d]
                if od["cc"]:
                    continue
                if (not od["dma"]) and (not o["dma"]) and od["eng"] == o["eng"] == "tensor":
                    continue
                od["signal"] = True
        cnt = {}
        dma_idx = {}
        for o in ops:
            dm = dom(o)
            if o["cc"]:
                continue
            if o["dma"]:
                n = dma_idx.get(dm, 0)
                o["dslot"] = n % NDMASEM
                o["dround"] = n // NDMASEM
                dma_idx[dm] = n + 1
            elif o["signal"] and o["fn"] is not None:
                cnt[dm] = cnt.get(dm, 0) + 1
                o["cnt"] = cnt[dm]
        sems = {e: nc.alloc_semaphore(name="s_" + e) for e in COMPUTE}
        dsems = {dm: [nc.alloc_semaphore(name="d_%s_%d" % (dm[1], k)) for k in range(NDMASEM)]
                 for dm in dma_idx}
        streams = {}
        for i, o in enumerate(ops):
            streams.setdefault(o["eng"], []).append(i)

        def run_stream(eng_name, engine):
            waited = {}

            def wait(sem, key, val):
                if waited.get(key, -1) >= val:
                    return
                engine.wait_ge(sem, val)
                waited[key] = val

            for i in streams.get(eng_name, []):
                o = ops[i]
                for d in sorted(o["deps"]):
                    od = ops[d]
                    if od["cc"]:
                        wait(ccsem, "cc", o["ncc_before"] - (1 if o["cc"] else 0))
                        continue
                    if od["dma"]:
                        dm = dom(od)
                        wait(dsems[dm][od["dslot"]], (dm, od["dslot"]), 16 * (od["dround"] + 1))
                    else:
                        if "cnt" not in od:
                            continue
                        if od["eng"] == eng_name == "tensor" and not o["dma"]:
                            continue
                        wait(sems[od["eng"]], od["eng"], od["cnt"])
                if o["dma"] and not o["cc"] and o["dround"] > 0:
                    dm = dom(o)
                    wait(dsems[dm][o["dslot"]], (dm, o["dslot"]), 16 * o["dround"])
                if o["fn"] is None:
                    continue
                ins = o["fn"](engine)
                if o["cc"]:
                    ins.then_inc(ccsem, 1)
                elif o["dma"]:
                    ins.then_inc(dsems[dom(o)][o["dslot"]], 16)
                elif "cnt" in o:
                    ins.then_inc(sems[eng_name], 1)

        with nc.Block() as block:
            @block.tensor
            def _(e):
                run_stream("tensor", e)

            @block.vector
            def _(e):
                run_stream("vector", e)

            @block.scalar
            def _(e):
                run_stream("scalar", e)

            @block.gpsimd
            def _(e):
                run_stream("gpsimd", e)

            @block.sync
            def _(e):
                run_stream("sync", e)


class Builder:
    def __init__(self, T, TP, layers, n_exp_groups=4):
        self.T, self.TP, self.TC = T, TP, T + TP
        self.layers = layers
        nc = self.nc = bass.Bass("TRN2", target_bir_lowering=False)
        P = self.P = Prog(nc)
        self.inputs = {}
        self.BIGA = P.sbuf("BIGA", [128, 32768], BF16)
        self.BIGB = P.sbuf("BIGB", [128, 16384], BF16)
        self.W = [P.sbuf("W%d" % i, [128, 8192], BF16) for i in range(5)]
        self.BC = [P.sbuf("BC%d" % i, [128, 2048], F32) for i in range(3)]
        self.ident = P.sbuf("ident", [128, 128], F32)
        self.identb = P.sbuf("identb", [128, 128], BF16)
        self.tri = P.sbuf("tri", [128, 128], F32)
        self.trib = P.sbuf("trib", [128, 128], BF16)
        self.rotT = P.sbuf("rotT", [128, 128], BF16)
        self.ones = P.sbuf("ones", [128, 128], F32)
        self.pbias = P.sbuf("pbias", [128, 1], F32)
        self.zero = P.sbuf("zero", [128, 1], F32)
        self.cvec = P.sbuf("cvec", [128, 16], F32)
        self.rowbuf = _View(self.BIGA.h.bitcast(F32)[0:1, 0:6144], self.BIGA.key)
        self.small = P.sbuf("small", [128, 64], F32)
        self.lam = P.sbuf("lam", [128, 8], F32)
        self.subw = P.sbuf("subw", [128, 256], F32)
        self.w32 = P.sbuf("w32", [128, 8, 32], F32)
        self.rt = P.sbuf("rt", [128, 128], F32)
        self.wr = P.sbuf("wr", [128, 16, 36], F32)
        self.brt = P.sbuf("brt", [128, 36], F32)
        bf = self.BIGB.h.bitcast(F32)
        self.lf = bf[:, 0:512].rearrange("p (t h) -> p t h", h=16)
        self.cum = bf[:, 512:1024].rearrange("p (t h) -> p t h", h=16)
        self.anc = bf[:, 1024:1088].rearrange("p (t h) -> p t h", h=16)
        self.fb = P.sbuf("fb", [128, 16], F32)
        self.wf = _View(self.BIGB[:, 4096:4352].rearrange("p (k n) -> p k n", k=16), "wfkey")
        self.PS = [P.psum("ps%d" % i, [128, 512], F32) for i in range(8)]

    def din(self, name, shape, dt=F32):
        t = self.nc.dram_tensor(name, list(shape), dt, kind="ExternalInput").ap()
        self.inputs[name] = t
        return t

    def dscr(self, name, shape, dt):
        return self.nc.dram_tensor(name, list(shape), dt).ap()

    def dma(self, eng, out, in_, reads=(), writes=()):
        self.P.op(eng, lambda e: e.dma_start(out=out, in_=in_), reads=reads, writes=writes, dma=True)

    def f32view(self, buf, off_f32, n):
        return buf.h.bitcast(F32)[:, off_f32:off_f32 + n]

    def load_consts(self):
        P = self.P
        c_ident = self.din("c_ident", [128, 128])
        c_tri = self.din("c_tri", [128, 128])
        c_rotT = self.din("c_rotT", [128, 128])
        c_pbias = self.din("c_pbias", [128, 1])
        self.dma("sync", self.ident[:], c_ident, writes=[self.ident])
        self.dma("sync", self.tri[:], c_tri, writes=[self.tri])
        self.dma("sync", self.pbias[:], c_pbias, writes=[self.pbias])
        self.dma("gpsimd", self.identb[:], c_ident, writes=[self.identb])
        self.dma("gpsimd", self.trib[:], c_tri, writes=[self.trib])
        self.dma("gpsimd", self.rotT[:], c_rotT, writes=[self.rotT])
        P.op("vector", lambda e: e.memset(self.ones[:], 1.0), writes=[self.ones])
        P.op("vector", lambda e: e.memset(self.zero[:], 0.0), writes=[self.zero])
        cv = self.din("cvec_in", [128, 16])
        self.dma("sync", self.cvec[:], cv, writes=[self.cvec])
        P.op("scalar", lambda e: e.activation(out=self.cvec[:], in_=self.cvec[:], func=AF.Silu),
             reads=[self.cvec], writes=[self.cvec])

    def modulation(self, wmod, bmod):
        P = self.P
        wst = [self.f32view(self.W[0], 0, 4096), self.f32view(self.W[1], 0, 4096),
               self.f32view(self.W[2], 0, 4096), self.f32view(self.W[3], 0, 4096)]
        wk = [self.W[0], self.W[1], self.W[2], self.W[3]]
        ps = self.PS[0]
        self.dma("sync", self.rowbuf.ap, bmod, writes=[self.rowbuf])
        for n in range(12):
            a, b = (0, 1) if n % 2 == 0 else (2, 3)
            src = wmod[:, n * 512:(n + 1) * 512].rearrange("(k p) n -> p k n", p=128)
            self.dma("sync", wst[a].rearrange("p (k n) -> p k n", k=8), src[:, 0:8, :], writes=[wk[a]])
            self.dma("sync", wst[b].rearrange("p (k n) -> p k n", k=8), src[:, 8:16, :], writes=[wk[b]])
            for k in range(16):
                wb = a if k < 8 else b
                kk = k % 8
                P.op("tensor", lambda e, wb=wb, kk=kk, k=k: e.matmul(
                    out=ps[0:1, :], lhsT=self.cvec[:, k:k + 1], rhs=wst[wb][:, kk * 512:(kk + 1) * 512],
                    start=(k == 0), stop=(k == 15)), reads=[self.cvec, wk[wb]], writes=[ps])
            addc = 0.0 if n < 4 else 1.0
            P.op("vector", lambda e, n=n, addc=addc: e.scalar_tensor_tensor(
                out=self.rowbuf.ap[0:1, n * 512:(n + 1) * 512], in0=ps[0:1, :], scalar=addc,
                in1=self.rowbuf.ap[0:1, n * 512:(n + 1) * 512], op0=ALU.add, op1=ALU.add),
                reads=[ps, self.rowbuf], writes=[self.rowbuf])
        self.bcast_mod()

    def bcast_mod(self):
        P = self.P
        for n in range(12):
            dst = [self.BC[1], self.BC[0], self.BC[2]][n // 4]
            psb = self.PS[1 + n % 2]
            P.op("tensor", lambda e, n=n, psb=psb: e.matmul(
                out=psb[:], lhsT=self.ones[0:1, :], rhs=self.rowbuf.ap[0:1, n * 512:(n + 1) * 512],
                start=True, stop=True), reads=[self.ones, self.rowbuf], writes=[psb])
            P.op("scalar", lambda e, n=n, psb=psb, dst=dst: e.activation(
                out=dst[:, (n % 4) * 512:(n % 4 + 1) * 512], in_=psb[:], func=AF.Copy),
                reads=[psb], writes=[dst])

    def make_hT(self, x_ap, ntok, hT, hT_key, router=None):
        P = self.P
        xt = [self.f32view(self.W[0], 0, 2048), self.f32view(self.W[1], 0, 2048)]
        xk = [self.W[0], self.W[1]]
        htf = self.f32view(self.W[2], 0, 2048)
        for t in range(ntok // 128):
            xb, xkk = xt[t % 2], xk[t % 2]
            self.dma("sync", xb, (x_ap(t) if callable(x_ap) else x_ap[t * 128:(t + 1) * 128, :]), writes=[xkk])
            if router is not None and "acc" in router:
                acc = router["acc"](t)
                P.op("scalar", lambda e, xb=xb, acc=acc: e.activation(out=acc, in_=xb, func=AF.Copy, scale=ALPHA),
                     reads=[xkk], writes=[(router["acckey"], t)])
            P.op("vector", lambda e, xb=xb: e.tensor_tensor(out=xb, in0=xb, in1=self.BC[0][:], op=ALU.mult),
                 reads=[xkk, self.BC[0]], writes=[xkk])
            P.op("vector", lambda e, xb=xb: e.tensor_tensor(out=xb, in0=xb, in1=self.BC[1][:], op=ALU.add),
                 reads=[xkk, self.BC[1]], writes=[xkk])
            for g in range(4):
                ps = self.PS[g % 4]
                for j in range(4):
                    k = g * 4 + j
                    P.op("tensor", lambda e, xb=xb, k=k, j=j, ps=ps: e.transpose(
                        out=ps[:, j * 128:(j + 1) * 128], in_=xb[:, k * 128:(k + 1) * 128], identity=self.ident[:]),
                        reads=[xkk, self.ident], writes=[ps])
                eng = "vector" if g % 2 == 0 else "scalar"
                dst = hT[:, g * 4:(g + 1) * 4, t * 128:(t + 1) * 128]
                src = ps[:].rearrange("p (a b) -> p a b", a=4)
                if eng == "vector":
                    P.op("vector", lambda e, dst=dst, src=src: e.tensor_copy(out=dst, in_=src),
                         reads=[ps], writes=[(hT_key, t)])
                else:
                    P.op("scalar", lambda e, dst=dst, src=src: e.activation(out=dst, in_=src, func=AF.Copy),
                         reads=[ps], writes=[(hT_key, t)])
                if router is not None:
                    P.op("gpsimd" if False else "vector", lambda e, src=src, g=g: e.tensor_copy(
                        out=htf[:, g * 512:(g + 1) * 512].rearrange("p (a b) -> p a b", a=4), in_=src),
                        reads=[ps], writes=[self.W[2]])
            if router is not None:
                self.route(t, htf, router)

    def route(self, t, htf, router):
        P = self.P
        ps = self.PS[4]
        for k in range(16):
            P.op("tensor", lambda e, k=k: e.matmul(out=ps[:, 0:36], lhsT=htf[:, k * 128:(k + 1) * 128],
                                                   rhs=self.wr[:, k, :], start=(k == 0), stop=(k == 15)),
                 reads=[self.W[2], self.wr], writes=[ps])
        rt = self.rt
        V = "vector"

        def v(fn, reads=(), writes=()):
            P.op(V, fn, reads=[rt] + list(reads), writes=[rt] + list(writes))
        v(lambda e: e.tensor_tensor(out=rt[:, 0:36], in0=ps[:, 0:36], in1=self.brt[:], op=ALU.add), reads=[ps, self.brt])
        v(lambda e: e.reduce_max(out=rt[:, 36:37], in_=rt[:, 0:4], axis=AX.X))
        v(lambda e: e.tensor_scalar(out=rt[:, 40:44], in0=rt[:, 0:4], scalar1=rt[:, 36:37], scalar2=None, op0=ALU.is_ge))
        v(lambda e: e.tensor_scalar(out=rt[:, 44:48], in0=rt[:, 0:4], scalar1=rt[:, 36:37], scalar2=None, op0=ALU.subtract))
        P.op("scalar", lambda e: e.activation(out=rt[:, 44:48], in_=rt[:, 44:48], func=AF.Exp, accum_out=rt[:, 37:38]),
             reads=[rt], writes=[rt])
        v(lambda e: e.reciprocal(out=rt[:, 38:39], in_=rt[:, 37:38]))
        v(lambda e: e.tensor_scalar(out=rt[:, 48:56], in0=rt[:, 4:12], scalar1=rt[:, 40:41], scalar2=None, op0=ALU.mult))
        for g in range(1, 4):
            v(lambda e, g=g: e.scalar_tensor_tensor(out=rt[:, 48:56], in0=rt[:, 4 + 8 * g:12 + 8 * g],
                                                    scalar=rt[:, 40 + g:41 + g], in1=rt[:, 48:56],
                                                    op0=ALU.mult, op1=ALU.add))
        v(lambda e: e.reduce_max(out=rt[:, 56:57], in_=rt[:, 48:56], axis=AX.X))
        v(lambda e: e.tensor_scalar(out=rt[:, 64:72], in0=rt[:, 48:56], scalar1=rt[:, 56:57], scalar2=None, op0=ALU.is_ge))
        v(lambda e: e.scalar_tensor_tensor(out=rt[:, 72:80], in0=rt[:, 64:72], scalar=NEG, in1=rt[:, 48:56],
                                           op0=ALU.mult, op1=ALU.add))
        v(lambda e: e.reduce_max(out=rt[:, 57:58], in_=rt[:, 72:80], axis=AX.X))
        v(lambda e: e.tensor_scalar(out=rt[:, 80:88], in0=rt[:, 72:80], scalar1=rt[:, 57:58], scalar2=None, op0=ALU.is_ge))
        v(lambda e: e.tensor_tensor(out=rt[:, 58:59], in0=rt[:, 57:58], in1=rt[:, 56:57], op=ALU.subtract))
        P.op("scalar", lambda e: e.activation(out=rt[:, 59:60], in_=rt[:, 58:59], func=AF.Exp), reads=[rt], writes=[rt])
        v(lambda e: e.tensor_scalar(out=rt[:, 60:61], in0=rt[:, 59:60], scalar1=1.0, scalar2=None, op0=ALU.add))
        v(lambda e: e.reciprocal(out=rt[:, 61:62], in_=rt[:, 60:61]))
        v(lambda e: e.tensor_tensor(out=rt[:, 62:63], in0=rt[:, 59:60], in1=rt[:, 61:62], op=ALU.mult))
        v(lambda e: e.tensor_scalar(out=rt[:, 64:72], in0=rt[:, 64:72], scalar1=rt[:, 61:62], scalar2=None, op0=ALU.mult))
        v(lambda e: e.scalar_tensor_tensor(out=rt[:, 64:72], in0=rt[:, 80:88], scalar=rt[:, 62:63], in1=rt[:, 64:72],
                                           op0=ALU.mult, op1=ALU.add))
        v(lambda e: e.tensor_scalar(out=rt[:, 64:72], in0=rt[:, 64:72], scalar1=rt[:, 38:39], scalar2=None, op0=ALU.mult))
        for g in range(4):
            P.op(V, lambda e, g=g: e.tensor_scalar(out=self.w32[:, t, g * 8:(g + 1) * 8], in0=rt[:, 64:72],
                                                   scalar1=rt[:, 40 + g:41 + g], scalar2=None, op0=ALU.mult),
                 reads=[rt], writes=[("w32", t)])

    def layer_norm(self, z, zkey, gt, bt, scr, scrkey):
        P = self.P
        st = self.small
        P.op("vector", lambda e: e.reduce_sum(out=st[:, 0:1], in_=z, axis=AX.X), reads=[zkey], writes=[st])
        P.op("vector", lambda e: e.tensor_scalar(out=st[:, 1:2], in0=st[:, 0:1], scalar1=-1.0 / D, scalar2=None, op0=ALU.mult),
             reads=[st], writes=[st])
        P.op("scalar", lambda e: e.activation(out=scr, in_=z, func=AF.Square, bias=st[:, 1:2], scale=1.0, accum_out=st[:, 2:3]),
             reads=[zkey, st], writes=[scrkey, st])
        P.op("vector", lambda e: e.tensor_scalar(out=st[:, 3:4], in0=st[:, 2:3], scalar1=1.0 / D, scalar2=LN_EPS, op0=ALU.mult, op1=ALU.add),
             reads=[st], writes=[st])
        P.op("scalar", lambda e: e.activation(out=st[:, 5:6], in_=st[:, 3:4], func=AF.Sqrt), reads=[st], writes=[st])
        P.op("vector", lambda e: e.reciprocal(out=st[:, 4:5], in_=st[:, 5:6]), reads=[st], writes=[st])
        P.op("vector", lambda e: e.tensor_scalar(out=z, in0=z, scalar1=st[:, 1:2], scalar2=st[:, 4:5], op0=ALU.add, op1=ALU.mult),
             reads=[zkey, st], writes=[zkey])
        P.op("vector", lambda e: e.tensor_tensor(out=z, in0=z, in1=gt[:], op=ALU.mult), reads=[zkey, gt], writes=[zkey])
        P.op("vector", lambda e: e.tensor_tensor(out=z, in0=z, in1=bt[:], op=ALU.add), reads=[zkey, bt], writes=[zkey])

    def mixer_params(self, kind, lam_init, lamv, subw, fbias, w_in):
        B = self
        P = self.P
        if kind == "A":
            lt = [B.f32view(B.W[4], i * 128, 128) for i in range(4)]
            for i in range(4):
                B.dma("sync", lt[i], lamv[i].partition_broadcast(128), writes=[B.W[4]])
            P.op("vector", lambda e: e.tensor_tensor(out=lt[0], in0=lt[0], in1=lt[1], op=ALU.mult), reads=[B.W[4]], writes=[B.W[4]])
            P.op("vector", lambda e: e.tensor_tensor(out=lt[2], in0=lt[2], in1=lt[3], op=ALU.mult), reads=[B.W[4]], writes=[B.W[4]])
            P.op("vector", lambda e: e.reduce_sum(out=B.lam[:, 2:3], in_=lt[0], axis=AX.X), reads=[B.W[4]], writes=[B.lam])
            P.op("vector", lambda e: e.reduce_sum(out=B.lam[:, 3:4], in_=lt[2], axis=AX.X), reads=[B.W[4]], writes=[B.lam])
            P.op("scalar", lambda e: e.activation(out=B.lam[:, 4:6], in_=B.lam[:, 2:4], func=AF.Exp), reads=[B.lam], writes=[B.lam])
            P.op("vector", lambda e: e.tensor_tensor(out=B.lam[:, 0:1], in0=B.lam[:, 4:5], in1=B.lam[:, 5:6], op=ALU.subtract), reads=[B.lam], writes=[B.lam])
            P.op("vector", lambda e: e.tensor_scalar(out=B.lam[:, 0:1], in0=B.lam[:, 0:1], scalar1=lam_init, scalar2=None, op0=ALU.add), reads=[B.lam], writes=[B.lam])
            P.op("vector", lambda e: e.tensor_scalar(out=B.lam[:, 1:2], in0=B.lam[:, 0:1], scalar1=-1.0, scalar2=None, op0=ALU.mult), reads=[B.lam], writes=[B.lam])
            B.dma("sync", B.subw[:], subw.partition_broadcast(128), writes=[B.subw])
            P.op("vector", lambda e: e.tensor_scalar(out=B.subw[:], in0=B.subw[:], scalar1=1.0 - lam_init, scalar2=None, op0=ALU.mult), reads=[B.subw], writes=[B.subw])
        else:
            B.dma("sync", B.fb[:], fbias.partition_broadcast(128), writes=[B.fb])
            B.dma("gpsimd", B.wf.ap, w_in[:, 6144:6160].rearrange("(k p) n -> p k n", p=128), writes=[B.wf])

    def qkv(self, l, kind, w_in, hT, hT_key, ntok, tok0, with_q, sc):
        P = self.P
        wb = [self.W[0], self.W[1]]
        nblk = 12
        first = 0 if with_q else 4
        bi = 0
        import os
        skip = os.environ.get("QKV_SKIP", "")
        for n in range(first, nblk):
            if (skip == "v" and n >= 8) or (skip == "qk" and n < 8):
                continue
            wt = wb[bi % 2]
            bi += 1
            wv = wt[:].rearrange("p (k n) -> p k n", k=16)
            self.dma("gpsimd", wv, w_in[:, n * 512:(n + 1) * 512].rearrange("(k p) n -> p k n", p=128), writes=[wt])
            if n < 8:
                isq = n < 4
                for tb in range(ntok // 512):
                    if kind == "A":
                        cs = [self.f32view(self.W[2], 0, 512), self.f32view(self.W[2], 512, 512)]
                        t0 = tok0 + tb * 512
                        self.dma("sync", cs[0], sc["cos"][:, t0:t0 + 512], writes=[(self.W[2].key, "c")])
                        self.dma("sync", cs[1], sc["sin"][:, t0:t0 + 512], writes=[(self.W[2].key, "s")])
                    for mi in range(4):
                        m = (n % 4) * 4 + mi
                        ps = self.PS[mi % 2]
                        for k in range(16):
                            P.op("tensor", lambda e, k=k, mi=mi, tb=tb, ps=ps, wv=wv: e.matmul(
                                out=ps[:], lhsT=wv[:, k, mi * 128:(mi + 1) * 128], rhs=hT[:, k, tb * 512:(tb + 1) * 512],
                                start=(k == 0), stop=(k == 15)), reads=[wt] + [(hT_key, tb * 4 + i) for i in range(4)], writes=[ps])
                        ob = self.W[3][:, (mi % 2) * 512:(mi % 2) * 512 + 512]
                        okey = (self.W[3].key, "o", mi % 2)
                        lvl = int(os.environ.get("QK_LEVEL", "9"))
                        if lvl == 0:
                            P.op("scalar", lambda e, ob=ob, ps=ps: e.activation(out=ob, in_=ps[:], func=AF.Copy),
                                 reads=[ps], writes=[okey])
                        elif kind == "A":
                            qraw = self.W[3][:, 1024 + (mi % 2) * 512:1024 + (mi % 2) * 512 + 512]
                            qkey = (self.W[3].key, "q", mi % 2)
                            ps2 = self.PS[2 + mi % 2]
                            t1 = self.f32view(self.W[3], 1024 + (mi % 2) * 1024, 512)
                            t2 = self.f32view(self.W[3], 1024 + (mi % 2) * 1024 + 512, 512)
                            tkey = (self.W[3].key, "t", mi % 2)
                            P.op("scalar", lambda e, qraw=qraw, ps=ps: e.activation(out=qraw, in_=ps[:], func=AF.Copy),
                                 reads=[ps], writes=[qkey])
                            P.op("tensor", lambda e, qraw=qraw, ps2=ps2: e.matmul(out=ps2[:], lhsT=self.rotT[:], rhs=qraw,
                                                                                   start=True, stop=True),
                                 reads=[qkey, self.rotT], writes=[ps2])
                            P.op("vector", lambda e, t1=t1, ps=ps, cs=cs: e.tensor_tensor(out=t1, in0=ps[:], in1=cs[0], op=ALU.mult),
                                 reads=[ps, (self.W[2].key, "c")], writes=[(tkey, 1)])
                            P.op("vector", lambda e, t2=t2, ps2=ps2, cs=cs: e.tensor_tensor(out=t2, in0=ps2[:], in1=cs[1], op=ALU.mult),
                                 reads=[ps2, (self.W[2].key, "s")], writes=[(tkey, 2)])
                            P.op("vector", lambda e, ob=ob, t1=t1, t2=t2: e.tensor_tensor(out=ob, in0=t1, in1=t2, op=ALU.add),
                                 reads=[(tkey, 1), (tkey, 2)], writes=[okey])
                        else:
                            P.op("scalar", lambda e, ob=ob, ps=ps: e.activation(out=ob, in_=ps[:], func=AF.Copy),
                                 reads=[ps], writes=[okey])
                        if isq:
                            dst = sc["qT"][m, :, tb * 512:(tb + 1) * 512]
                            dk = ("qT", m)
                        else:
                            dst = sc["kT"][m, :, tok0 + tb * 512:tok0 + (tb + 1) * 512]
                            dk = ("kT", m)
                        if os.environ.get("NO_STORE", "") != "1":
                            self.dma("sync", dst, ob, reads=[okey], writes=[dk])
            else:
                hv = 256 if kind == "A" else 128
                nh = 512 // hv
                for tt in range(ntok // 128):
                    ps = self.PS[4 + tt % 2]
                    for k in range(16):
                        P.op("tensor", lambda e, k=k, tt=tt, ps=ps, wv=wv: e.matmul(
                            out=ps[:], lhsT=hT[:, k, tt * 128:(tt + 1) * 128], rhs=wv[:, k, :],
                            start=(k == 0), stop=(k == 15)), reads=[wt, (hT_key, tt)], writes=[ps])
                    vb = self.W[2][:, 2048 + (tt % 2) * 1024:2048 + (tt % 2) * 1024 + nh * (hv + 1)].rearrange("p (h c) -> p h c", h=nh)
                    vkey = (self.W[2].key, "v", tt % 2)
                    P.op("scalar" if tt % 2 == 0 else "vector",
                         (lambda e, vb=vb, ps=ps: e.activation(out=vb[:, :, 0:hv], in_=ps[:].rearrange("p (h c) -> p h c", h=nh), func=AF.Copy))
                         if tt % 2 == 0 else
                         (lambda e, vb=vb, ps=ps: e.tensor_copy(out=vb[:, :, 0:hv], in_=ps[:].rearrange("p (h c) -> p h c", h=nh))),
                         reads=[ps], writes=[vkey])
                    P.op("gpsimd", lambda e, vb=vb: e.memset(vb[:, :, hv:hv + 1], 1.0), writes=[vkey])
                    h0 = (n - 8) * nh
                    ct = (tok0 // 128) + tt
                    self.dma("sync", sc["v"][h0:h0 + nh, ct, :, :].rearrange("h p c -> p h c"), vb, reads=[vkey], writes=[("v", h0)])
        if kind == "B":
            P.op("sync", None, reads=[self.wf])
            for tt in range(ntok // 128):
                ps = self.PS[6]
                ct = (tok0 // 128) + tt
                for k in range(16):
                    P.op("tensor", lambda e, k=k, tt=tt: e.matmul(out=ps[:, 0:16], lhsT=hT[:, k, tt * 128:(tt + 1) * 128],
                                                                  rhs=self.wf.ap[:, k, :], start=(k == 0), stop=(k == 15)),
                         reads=[self.wf, (hT_key, tt)], writes=[ps])
                st = self.small
                P.op("vector", lambda e: e.tensor_tensor(out=st[:, 16:32], in0=ps[:, 0:16], in1=self.fb[:], op=ALU.add),
                     reads=[ps, self.fb], writes=[st])
                P.op("scalar", lambda e: e.activation(out=st[:, 32:48], in_=st[:, 16:32], func=AF.Exp, scale=-1.0), reads=[st], writes=[st])
                P.op("scalar", lambda e: e.activation(out=st[:, 32:48], in_=st[:, 32:48], func=AF.Ln, bias=1.0, scale=1.0), reads=[st], writes=[st])
                P.op("vector", lambda e, ct=ct: e.tensor_scalar(out=self.lf[:, ct, :], in0=st[:, 32:48], scalar1=-1.0, scalar2=None, op0=ALU.mult),
                     reads=[st], writes=[("lf", ct)])

    def attention(self, l, kind, sc):
        P = self.P
        T, TP, TC = self.T, self.TP, self.TC
        nprev = TP // 128
        nq = T // 512
        scale = 128 ** -0.5
        if kind == "A":
            nheads, nmaps, hv = 8, 2, 256
        else:
            nheads, nmaps, hv = 16, 1, 128
        hv1 = hv + 1
        kTs = [self.BIGA[:, m * TC:(m + 1) * TC] for m in range(nmaps)]
        o0 = nmaps * TC
        qTs = [self.BIGA[:, o0 + m * T:o0 + (m + 1) * T] for m in range(nmaps)]
        o1 = o0 + nmaps * T
        v1 = self.BIGA[:, o1:o1 + (TC // 128) * hv1].rearrange("p (t c) -> p t c", c=hv1)
        assert o1 + (TC // 128) * hv1 <= 32768
        if kind == "B":
            ps = self.PS[7]
            for t in range(TC // 128):
                for t2 in range(t + 1):
                    P.op("tensor", lambda e, t=t, t2=t2: e.matmul(
                        out=ps[:, 0:16], lhsT=(self.tri[:] if t2 == t else self.ones[:]), rhs=self.lf[:, t2, :],
                        start=(t2 == 0), stop=(t2 == t)), reads=[("lf", t2), self.tri, self.ones], writes=[ps])
                P.op("vector", lambda e, t=t: e.tensor_copy(out=self.cum[:, t, :], in_=ps[:, 0:16]), reads=[ps], writes=[("cum", t)])
            for g in range(nq):
                ta = (TP + g * 512 + 256) // 128
                for t2 in range(ta):
                    P.op("tensor", lambda e, t2=t2, ta=ta: e.matmul(out=ps[:, 0:16], lhsT=self.ones[:], rhs=self.lf[:, t2, :],
                                                                    start=(t2 == 0), stop=(t2 == ta - 1)),
                         reads=[("lf", t2), self.ones], writes=[ps])
                P.op("vector", lambda e, g=g: e.tensor_copy(out=self.anc[:, g, :], in_=ps[:, 0:16]), reads=[ps], writes=[("anc", g)])
        ebuf = [self.W[0], self.W[1], self.W[4]]
        for h in range(nheads):
            for m in range(nmaps):
                mm = h * nmaps + m
                self.dma("sync", kTs[m], sc["kT"][mm, :, :], reads=[("kT", mm)], writes=[("kTs", m)])
                self.dma("sync", qTs[m], sc["qT"][mm, :, :], reads=[("qT", mm)], writes=[("qTs", m)])
            self.dma("sync", v1, sc["v"][h, :, :, :].rearrange("t p c -> p t c"), reads=[("v", (h // (512 // hv)) * (512 // hv))], writes=["v1"])
            for g in range(nq):
                nkb = nprev + 4 * g + 4
                o1n = self.f32view(self.W[2], 0, 1024).rearrange("p (i c) -> p i c", i=4)
                for m in range(nmaps):
                    def emit_S(j, m=m, g=g):
                        pss = self.PS[4 + j % 3]
                        P.op("tensor", lambda e, m=m, j=j, g=g, pss=pss: e.matmul(
                            out=pss[:], lhsT=kTs[m][:, j * 128:(j + 1) * 128], rhs=qTs[m][:, g * 512:(g + 1) * 512],
                            start=True, stop=True), reads=[("kTs", m), ("qTs", m)], writes=[pss])

                    emit_S(0)
                    emit_S(1)
                    for j in range(nkb):
                        if j + 2 < nkb:
                            emit_S(j + 2)
                        pss = self.PS[4 + j % 3]
                        et = ebuf[j % 3]
                        ev = et[:, 0:512]
                        if kind == "A":
                            bias = self.pbias[:, 0:1] if j < nprev else self.zero[:, 0:1]
                            breads = [self.pbias, self.zero]
                        else:
                            bcol = self.small[:, 48 + (j % 3):49 + (j % 3)]
                            P.op("vector", lambda e, bcol=bcol, g=g, j=j, h=h: e.tensor_tensor(
                                out=bcol, in0=self.anc[:, g, h:h + 1], in1=self.cum[:, j, h:h + 1], op=ALU.subtract),
                                reads=[("anc", g), ("cum", j)], writes=[("bcol", j % 3)])
                            if j < nprev:
                                P.op("vector", lambda e, bcol=bcol: e.tensor_tensor(out=bcol, in0=bcol, in1=self.pbias[:, 0:1], op=ALU.add),
                                     reads=[("bcol", j % 3), self.pbias], writes=[("bcol", j % 3)])
                            bias = bcol
                            breads = [("bcol", j % 3)]
                        P.op("scalar", lambda e, ev=ev, pss=pss, bias=bias: e.activation(out=ev, in_=pss[:], func=AF.Exp, bias=bias, scale=scale),
                             reads=[pss] + breads, writes=[et])
                        jo = j - nprev
                        for i in range(4):
                            qi = 4 * g + i
                            if jo > qi:
                                continue
                            if jo == qi:
                                P.op("gpsimd", lambda e, ev=ev, i=i: e.tensor_tensor(
                                    out=ev[:, i * 128:(i + 1) * 128], in0=ev[:, i * 128:(i + 1) * 128], in1=self.trib[:], op=ALU.mult),
                                    reads=[et, self.trib], writes=[et])
                            pso = self.PS[i]
                            last = nprev + qi
                            P.op("tensor", lambda e, ev=ev, i=i, j=j, pso=pso, last=last: e.matmul(
                                out=pso[:, 0:hv1], lhsT=ev[:, i * 128:(i + 1) * 128], rhs=v1[:, j, :],
                                start=(j == 0), stop=(j == last)), reads=[et, "v1"], writes=[pso])
                    st = self.small
                    for i in range(4):
                        pso = self.PS[i]
                        qi = 4 * g + i
                        P.op("vector", lambda e, pso=pso, i=i: e.reciprocal(out=st[:, 8 + i:9 + i], in_=pso[:, hv:hv1]), reads=[pso], writes=[st])
                        if kind == "A" and m == 0:
                            P.op("vector", lambda e, pso=pso, i=i: e.tensor_scalar(
                                out=o1n[:, i, :], in0=pso[:, 0:hv], scalar1=st[:, 8 + i:9 + i], scalar2=None, op0=ALU.mult),
                                reads=[pso, st], writes=[(self.W[2].key, "o1n", i)])
                        elif kind == "A":
                            ot = self.f32view(self.W[3], i * 256, 256)
                            okey = (self.W[3].key, "ot", i)
                            P.op("vector", lambda e, i=i: e.tensor_tensor(out=st[:, 12 + i:13 + i], in0=st[:, 8 + i:9 + i], in1=self.lam[:, 1:2], op=ALU.mult),
                                 reads=[st, self.lam], writes=[st])
                            P.op("vector", lambda e, pso=pso, i=i, ot=ot: e.scalar_tensor_tensor(
                                out=ot, in0=pso[:, 0:hv], scalar=st[:, 12 + i:13 + i], in1=o1n[:, i, :], op0=ALU.mult, op1=ALU.add),
                                reads=[pso, st, (self.W[2].key, "o1n", i)], writes=[okey])
                            sq = self.f32view(self.W[3], 1024 + i * 256, 256)
                            P.op("scalar", lambda e, ot=ot, sq=sq, i=i: e.activation(out=sq, in_=ot, func=AF.Square, accum_out=st[:, 16 + i:17 + i]),
                                 reads=[okey], writes=[(self.W[3].key, "sq", i), st])
                            P.op("vector", lambda e, i=i: e.tensor_scalar(out=st[:, 20 + i:21 + i], in0=st[:, 16 + i:17 + i], scalar1=1.0 / 256,
                                                                         scalar2=RMS_EPS, op0=ALU.mult, op1=ALU.add), reads=[st], writes=[st])
                            P.op("scalar", lambda e, i=i: e.activation(out=st[:, 24 + i:25 + i], in_=st[:, 20 + i:21 + i], func=AF.Sqrt), reads=[st], writes=[st])
                            P.op("vector", lambda e, i=i: e.reciprocal(out=st[:, 28 + i:29 + i], in_=st[:, 24 + i:25 + i]), reads=[st], writes=[st])
                            ob = self.W[3][:, 4096 + i * 256:4096 + (i + 1) * 256]
                            obk = (self.W[3].key, "ob", i)
                            P.op("vector", lambda e, ot=ot, ob=ob, i=i: e.scalar_tensor_tensor(
                                out=ob, in0=ot, scalar=st[:, 28 + i:29 + i], in1=self.subw[:], op0=ALU.mult, op1=ALU.mult),
                                reads=[okey, st, self.subw], writes=[obk])
                            self.dma("sync", sc["o"][qi * 128:(qi + 1) * 128, h * 256:(h + 1) * 256], ob, reads=[obk], writes=[("o", qi)])
                        else:
                            ob = self.W[3][:, 4096 + i * 128:4096 + (i + 1) * 128]
                            obk = (self.W[3].key, "ob", i)
                            P.op("vector", lambda e, pso=pso, ob=ob, i=i: e.tensor_scalar(
                                out=ob, in0=pso[:, 0:hv], scalar1=st[:, 8 + i:9 + i], scalar2=None, op0=ALU.mult),
                                reads=[pso, st], writes=[obk])
                            self.dma("sync", sc["o"][qi * 128:(qi + 1) * 128, h * 128:(h + 1) * 128], ob, reads=[obk], writes=[("o", qi)])

    def out_proj(self, l, w_out, x_ap, lng, lnb, sc, x1_ap):
        P = self.P
        T = self.T
        wo = [self.W[i] for i in range(4)]
        for n in range(4):
            self.dma("gpsimd", wo[n][:].rearrange("p (k n) -> p k n", k=16),
                     w_out[:, n * 512:(n + 1) * 512].rearrange("(k p) n -> p k n", p=128), writes=[wo[n]])
        self.dma("sync", self.BC[0][:], lng.partition_broadcast(128), writes=[self.BC[0]])
        self.dma("sync", self.BC[1][:], lnb.partition_broadcast(128), writes=[self.BC[1]])
        for t in range(T // 128):
            ob = self.BIGB[:, (t % 2) * 2048:(t % 2 + 1) * 2048]
            obk = ("ob", t % 2)
            self.dma("sync", ob, sc["o"][t * 128:(t + 1) * 128, :], reads=[("o", t)], writes=[obk])
            xt = self.f32view(self.BIGA, (t % 2) * 2048, 2048)
            xk = ("xt", t % 2)
            self.dma("sync", xt, x_ap[t * 128:(t + 1) * 128, :], writes=[xk])
            oT = self.BIGB[:, 4096 + (t % 2) * 2048:4096 + (t % 2 + 1) * 2048].rearrange("p (k n) -> p k n", k=16)
            oTk = ("oT", t % 2)
            for g in range(4):
                ps = self.PS[4 + g % 2]
                psb = ps.h.bitcast(BF16)
                for j in range(4):
                    k = g * 4 + j
                    P.op("tensor", lambda e, ob=ob, k=k, j=j, psb=psb: e.transpose(
                        out=psb[:, j * 128:(j + 1) * 128], in_=ob[:, k * 128:(k + 1) * 128], identity=self.identb[:]),
                        reads=[obk, self.identb], writes=[ps])
                P.op("scalar" if g % 2 else "vector",
                     (lambda e, oT=oT, psb=psb, g=g: e.activation(out=oT[:, g * 4:(g + 1) * 4, :], in_=psb[:, 0:512].rearrange("p (a b) -> p a b", a=4), func=AF.Copy))
                     if g % 2 else
                     (lambda e, oT=oT, psb=psb, g=g: e.tensor_copy(out=oT[:, g * 4:(g + 1) * 4, :], in_=psb[:, 0:512].rearrange("p (a b) -> p a b", a=4))),
                     reads=[ps], writes=[oTk])
            y = self.f32view(self.BIGA, 4096 + (t % 2) * 2048, 2048)
            yk = ("y", t % 2)
            for n in range(4):
                ps = self.PS[n]
                for k in range(16):
                    P.op("tensor", lambda e, n=n, k=k, oT=oT, ps=ps: e.matmul(
                        out=ps[:], lhsT=oT[:, k, :], rhs=wo[n][:, k * 512:(k + 1) * 512], start=(k == 0), stop=(k == 15)),
                        reads=[oTk, wo[n]], writes=[ps])
                P.op("vector", lambda e, n=n, ps=ps, y=y: e.tensor_tensor(
                    out=y[:, n * 512:(n + 1) * 512], in0=ps[:], in1=self.BC[2][:, n * 512:(n + 1) * 512], op=ALU.mult),
                    reads=[ps, self.BC[2]], writes=[yk])
            P.op("vector", lambda e, xt=xt, y=y: e.scalar_tensor_tensor(out=xt, in0=xt, scalar=ALPHA, in1=y, op0=ALU.mult, op1=ALU.add),
                 reads=[xk, yk], writes=[xk])
            self.layer_norm(xt, xk, self.BC[0], self.BC[1], y, yk)
            self.dma("sync", x1_ap[t * 128:(t + 1) * 128, :], xt, reads=[xk], writes=[("x1", t)])

    def moe(self, l, W, x1_ap, out_ap, outkey):
        P = self.P
        T = self.T
        TPASS = min(1024, T)
        npass = T // TPASS
        self.dma("sync", self.wr[:, :, 0:4], W["w_group"].rearrange("(k p) g -> p k g", p=128), writes=[self.wr])
        for g in range(4):
            self.dma("sync", self.wr[:, :, 4 + 8 * g:12 + 8 * g], W["w_router"][g].rearrange("(k p) e -> p k e", p=128), writes=[self.wr])
        self.dma("sync", self.brt[:, 0:4], W["b_group"].partition_broadcast(128), writes=[self.brt])
        self.dma("sync", self.brt[:, 4:36], W["b_router"].partition_broadcast(128), writes=[self.brt])
        if npass > 1:
            if not hasattr(self, "bcsave"):
                self.bcsave = self.dscr("s_bcsave", [2, 128, 2048], F32)
            self.dma("sync", self.bcsave[0], self.BC[0][:], reads=[self.BC[0]], writes=["bcsave0"])
            self.dma("sync", self.bcsave[1], self.BC[1][:], reads=[self.BC[1]], writes=["bcsave1"])
        ntile = TPASS // 128
        hT2 = self.BIGB[:, 0:16 * TPASS].rearrange("p (k t) -> p k t", k=16)
        accv = self.BIGA.h.bitcast(F32)
        hidbufs = [self.BC[0].h.bitcast(BF16)[:, i * 2048:(i + 1) * 2048].rearrange("p (f t) -> p f t", f=4) for i in range(2)]
        sgbufs = [self.BC[1][:, i * 512:(i + 1) * 512] for i in range(2)]
        for ps_i in range(npass):
            P.fence()
            tok0 = ps_i * TPASS
            if ps_i > 0:
                self.dma("sync", self.BC[0][:], self.bcsave[0], writes=[self.BC[0]])
                self.dma("sync", self.BC[1][:], self.bcsave[1], writes=[self.BC[1]])
            self.make_hT(x1_ap[tok0:tok0 + TPASS, :], TPASS, hT2, "hT2", router=dict())
            P.fence()
            hi = 0
            for ex in range(32):
                g, e = ex // 8, ex % 8
                wg = self.W[0 + ex % 2]
                wu = self.W[2 + ex % 2]
                wd = self.W[4]
                wgv = wg[:].rearrange("p (k n) -> p k n", k=16)
                wuv = wu[:].rearrange("p (k n) -> p k n", k=16)
                wdv = wd[:].rearrange("p (k n) -> p k n", k=4)
                self.dma("gpsimd", wgv, W["w_gate"][g, e].rearrange("(k p) n -> p k n", p=128), writes=[wg])
                self.dma("gpsimd", wuv, W["w_up"][g, e].rearrange("(k p) n -> p k n", p=128), writes=[wu])
                self.dma("gpsimd", wdv, W["w_down"][g, e].rearrange("(k p) n -> p k n", p=128), writes=[wd])
                for tb in range(TPASS // 512):
                    hb = hi % 2
                    hi += 1
                    hidb = hidbufs[hb]
                    for fc in range(4):
                        psg = self.PS[fc % 2]
                        psu = self.PS[2 + fc % 2]
                        for k in range(16):
                            P.op("tensor", lambda e_, k=k, fc=fc, tb=tb, psg=psg, wgv=wgv: e_.matmul(
                                out=psg[:], lhsT=wgv[:, k, fc * 128:(fc + 1) * 128], rhs=hT2[:, k, tb * 512:(tb + 1) * 512],
                                start=(k == 0), stop=(k == 15)), reads=[wg] + [("hT2", tb * 4 + i) for i in range(4)], writes=[psg])
                        for k in range(16):
                            P.op("tensor", lambda e_, k=k, fc=fc, tb=tb, psu=psu, wuv=wuv: e_.matmul(
                                out=psu[:], lhsT=wuv[:, k, fc * 128:(fc + 1) * 128], rhs=hT2[:, k, tb * 512:(tb + 1) * 512],
                                start=(k == 0), stop=(k == 15)), reads=[wu] + [("hT2", tb * 4 + i) for i in range(4)], writes=[psu])
                        sg = sgbufs[fc % 2]
                        sgk = ("sg", fc % 2)
                        P.op("scalar", lambda e_, sg=sg, psg=psg: e_.activation(out=sg, in_=psg[:], func=AF.Silu), reads=[psg], writes=[sgk])
                        P.op("vector", lambda e_, sg=sg, psu=psu, hidb=hidb, fc=fc: e_.tensor_tensor(
                            out=hidb[:, fc, :], in0=psu[:], in1=sg, op=ALU.mult), reads=[psu, sgk], writes=[("hid", hb, fc)])
                    for tt in range(4):
                        t = tb * 4 + tt
                        for dmb in range(4):
                            psd = self.PS[4 + (tt * 4 + dmb) % 4]
                            for fc in range(4):
                                P.op("tensor", lambda e_, fc=fc, tt=tt, dmb=dmb, psd=psd, hidb=hidb, wdv=wdv: e_.matmul(
                                    out=psd[:], lhsT=hidb[:, fc, tt * 128:(tt + 1) * 128], rhs=wdv[:, fc, dmb * 512:(dmb + 1) * 512],
                                    start=(fc == 0), stop=(fc == 3)), reads=[("hid", hb, fc), wd], writes=[psd])
                            av = accv[:, t * 2048 + dmb * 512:t * 2048 + (dmb + 1) * 512]
                            if ex == 0:
                                P.op("vector", lambda e_, psd=psd, av=av, t=t, ex=ex: e_.tensor_scalar(
                                    out=av, in0=psd[:], scalar1=self.w32[:, t, ex:ex + 1], scalar2=None, op0=ALU.mult),
                                    reads=[psd, ("w32", t)], writes=[("acc", t)])
                            else:
                                P.op("vector", lambda e_, psd=psd, av=av, t=t, ex=ex: e_.scalar_tensor_tensor(
                                    out=av, in0=psd[:], scalar=self.w32[:, t, ex:ex + 1], in1=av, op0=ALU.mult, op1=ALU.add),
                                    reads=[psd, ("w32", t), ("acc", t)], writes=[("acc", t)])
            P.fence()
            self.dma("sync", self.f32view(self.W[0], 0, 2048), W["ln_g"].partition_broadcast(128), writes=[self.W[0]])
            self.dma("sync", self.f32view(self.W[1], 0, 2048), W["ln_b"].partition_broadcast(128), writes=[self.W[1]])
            gt = _View(self.f32view(self.W[0], 0, 2048), self.W[0].key)
            bt = _View(self.f32view(self.W[1], 0, 2048), self.W[1].key)
            for t in range(ntile):
                z = accv[:, t * 2048:(t + 1) * 2048]
                xt = self.f32view(self.W[3], (t % 2) * 2048, 2048)
                xk = (self.W[3].key, "xt", t % 2)
                self.dma("sync", xt, x1_ap[tok0 + t * 128:tok0 + (t + 1) * 128, :], writes=[xk])
                P.op("vector", lambda e_, z=z: e_.tensor_tensor(out=z, in0=z, in1=self.BC[2][:], op=ALU.mult),
                     reads=[("acc", t), self.BC[2]], writes=[("acc", t)])
                P.op("vector", lambda e_, z=z, xt=xt: e_.scalar_tensor_tensor(out=z, in0=xt, scalar=ALPHA, in1=z, op0=ALU.mult, op1=ALU.add),
                     reads=[("acc", t), xk], writes=[("acc", t)])
                scr = self.f32view(self.W[2], (t % 2) * 2048, 2048)
                self.layer_norm(z, ("acc", t), gt, bt, scr, (self.W[2].key, "scr", t % 2))
                self.dma("sync", out_ap[tok0 + t * 128:tok0 + (t + 1) * 128, :], z, reads=[("acc", t)], writes=[(outkey, tok0 // 128 + t)])

    def rowhid(self, tb):
        o = 8192 + (tb % 2) * 2048
        return self.BIGB[:, o:o + 2048].rearrange("p (f t) -> p f t", f=4)

    def sgbuf(self, i):
        return self.BIGB.h.bitcast(F32)[:, 6144 + i * 512:6144 + (i + 1) * 512]


Buf.register = None


def _is_buf(x):
    return isinstance(x, (Buf, _View))


def _k(x):
    return x.key if isinstance(x, (Buf, _View)) else x


def build_program(T, TP, layers, ncores=8):
    B = Builder(T, TP, layers)
    nc, P = B.nc, B.P
    TC = T + TP
    B.load_consts()
    xo = B.din("xo", [T, D])
    xp = B.din("xp", [TP, D])
    cosT = B.din("c_cos", [128, TC])
    sinT = B.din("c_sin", [128, TC])
    out = nc.dram_tensor("out", [T, D], F32, kind="ExternalOutput").ap()
    sc = dict(
        qT=B.dscr("s_qT", [16, 128, T], BF16), kT=B.dscr("s_kT", [16, 128, TC], BF16),
        o=B.dscr("s_o", [T, D], BF16), cos=cosT, sin=sinT)
    vA = B.dscr("s_vA", [8, TC // 128, 128, 257], BF16)
    vB = B.dscr("s_vB", [16, TC // 128, 128, 129], BF16)
    x1 = B.dscr("s_x1", [T, D], F32)
    CH = 256
    nch = T // CH
    if len(layers) > 1:
        x2 = B.dscr("s_x2", [T, D], F32)
        gath = B.dscr("s_gath", [nch, 2 * CH, D], F32)
    cur_o, cur_p = xo, xp
    for li, l in enumerate(layers):
        kind = "A" if l % 2 == 0 else "B"
        last = (li == len(layers) - 1)
        sc["v"] = vA if kind == "A" else vB
        wmod = B.din("mix_mod_w%d" % l, [D, 6144])
        bmod = B.din("mix_mod_b%d" % l, [1, 6144])
        lng = B.din("mix_ln_g%d" % l, [1, D])
        lnb = B.din("mix_ln_b%d" % l, [1, D])
        lam_init = lamv = subw = fbias = None
        if kind == "A":
            w_in = B.din("a_w_in", [D, 6144])
            w_out = B.din("a_w_out", [D, D])
            lamv = [B.din("a_lam_q1", [1, 128]), B.din("a_lam_k1", [1, 128]),
                    B.din("a_lam_q2", [1, 128]), B.din("a_lam_k2", [1, 128])]
            subw = B.din("a_subln_w", [1, 256])
            lam_init = 0.8 - 0.6 * math.exp(-0.3 * l)
        else:
            w_in = B.din("b_w_in", [D, 6160])
            w_out = B.din("b_w_out", [D, D])
            fbias = B.din("b_forget_bias", [1, 16])
        Wm = dict(
            w_group=B.din("moe_w_group%d" % l, [D, 4]), b_group=B.din("moe_b_group%d" % l, [1, 4]),
            w_router=B.din("moe_w_router%d" % l, [4, D, 8]), b_router=B.din("moe_b_router%d" % l, [1, 32]),
            w_gate=B.din("moe_w_gate%d" % l, [4, 8, D, 512]), w_up=B.din("moe_w_up%d" % l, [4, 8, D, 512]),
            w_down=B.din("moe_w_down%d" % l, [4, 8, 512, D]),
            ln_g=B.din("ffn_ln_g%d" % l, [1, D]), ln_b=B.din("ffn_ln_b%d" % l, [1, D]))
        fwmod = B.din("ffn_mod_w%d" % l, [D, 6144])
        fbmod = B.din("ffn_mod_b%d" % l, [1, 6144])

        P.fence()
        B.modulation(wmod, bmod)
        B.mixer_params(kind, lam_init, lamv, subw, fbias, w_in)
        hT = B.BIGA[:, 0:16 * max(T, TP)].rearrange("p (k t) -> p k t", k=16)
        for (x_ap, ntok, tok0, with_q) in ((cur_p, TP, 0, False), (cur_o, T, TP, True)):
            P.fence()
            B.make_hT(x_ap, ntok, hT[:, :, 0:ntok], "hT")
            P.fence()
            B.qkv(l, kind, w_in, hT[:, :, 0:ntok], "hT", ntok, tok0, with_q, sc)
        P.fence()
        B.attention(l, kind, sc)
        P.fence()
        B.out_proj(l, w_out, cur_o, lng, lnb, sc, x1)
        P.fence()
        B.modulation(fwmod, fbmod)
        B.moe(l, Wm, x1, out if last else x2, "out" if last else "x2")
        if not last:
            P.fence()
            groups = [[2 * i, 2 * i + 1] for i in range(ncores // 2)]
            for ch in range(nch):
                if os.environ.get("K_NOCC"):
                    B.dma("sync", gath[ch, 0:CH, :], x2[ch * CH:(ch + 1) * CH, :], writes=[("gath", ch)])
                    continue
                P.op("gpsimd", lambda e, ch=ch: e.collective_compute(
                    "AllGather", ALU.bypass, replica_groups=groups,
                    ins=[x2[ch * CH:(ch + 1) * CH, :]], outs=[gath[ch]]),
                    writes=[("gath", ch)], dma=True, cc=True)
            P.fence()
            cur_o = x2
            cur_p = (lambda t: gath[t // 2, (t % 2) * 128:(t % 2 + 1) * 128, :])
    P.fence()
    P.op("sync", None, reads=[])
    P.emit()
    return nc, B


def _consts(T, TP, p):
    TC = T + TP
    ident = np.eye(128, dtype=np.float32)
    tri = np.triu(np.ones((128, 128), np.float32))
    R = np.zeros((128, 128), np.float32)
    for i in range(64):
        R[i, i + 64] = -1.0
        R[i + 64, i] = 1.0
    rotT = np.ascontiguousarray(R.T)
    pos = np.concatenate([np.arange(TP), p * T + np.arange(T)]).astype(np.float32)
    inv = np.power(np.float32(10000.0), -np.arange(0, 128, 2, dtype=np.float32) / 128).astype(np.float32)
    ang = pos[None, :] * np.concatenate([inv, inv])[:, None]
    return dict(c_ident=ident, c_tri=tri, c_rotT=rotT,
                c_pbias=np.full((128, 1), 0.0 if p == 1 else NEG, np.float32),
                c_cos=np.cos(ang).astype(np.float32), c_sin=np.sin(ang).astype(np.float32))


_PROG_CACHE = {}
_RUN_KW = {}
_LAST = {}


def run_layers(layers, x_in, inp, T, TP, ncores):
    key = (tuple(layers), T, TP, ncores)
    if key not in _PROG_CACHE:
        _PROG_CACHE[key] = build_program(T, TP, list(layers), ncores)
    nc, B = _PROG_CACHE[key]
    f = np.float32
    shared = {}
    for l in layers:
        j = l // 2
        shared["mix_mod_w%d" % l] = inp["mix_mod_w"][l]
        shared["mix_mod_b%d" % l] = inp["mix_mod_b"][l].reshape(1, -1)
        shared["mix_ln_g%d" % l] = inp["mix_ln_g"][l].reshape(1, -1)
        shared["mix_ln_b%d" % l] = inp["mix_ln_b"][l].reshape(1, -1)
        if l % 2 == 0:
            shared["a_w_in"] = inp["a_w_in"][j]
            shared["a_w_out"] = inp["a_w_out"][j]
            shared["a_lam_q1"] = inp["a_lam_q1"][j].reshape(1, -1)
            shared["a_lam_k1"] = inp["a_lam_k1"][j].reshape(1, -1)
            shared["a_lam_q2"] = inp["a_lam_q2"][j].reshape(1, -1)
            shared["a_lam_k2"] = inp["a_lam_k2"][j].reshape(1, -1)
            shared["a_subln_w"] = inp["a_subln_w"][j].reshape(1, -1)
        else:
            shared["b_w_in"] = inp["b_w_in"][j]
            shared["b_w_out"] = inp["b_w_out"][j]
            shared["b_forget_bias"] = inp["b_forget_bias"][j].reshape(1, -1)
        shared["ffn_mod_w%d" % l] = inp["ffn_mod_w"][l]
        shared["ffn_mod_b%d" % l] = inp["ffn_mod_b"][l].reshape(1, -1)
        shared["ffn_ln_g%d" % l] = inp["ffn_ln_g"][l].reshape(1, -1)
        shared["ffn_ln_b%d" % l] = inp["ffn_ln_b"][l].reshape(1, -1)
        shared["moe_w_group%d" % l] = inp["moe_w_group"][l]
        shared["moe_b_group%d" % l] = inp["moe_b_group"][l].reshape(1, -1)
        shared["moe_w_router%d" % l] = inp["moe_w_router"][l]
        shared["moe_b_router%d" % l] = inp["moe_b_router"][l].reshape(1, -1)
        shared["moe_w_gate%d" % l] = inp["moe_w_gate"][l]
        shared["moe_w_up%d" % l] = inp["moe_w_up"][l]
        shared["moe_w_down%d" % l] = inp["moe_w_down"][l]
    shared = {k: np.ascontiguousarray(np.asarray(v, dtype=f)) for k, v in shared.items() if k in B.inputs}
    in_maps = []
    for c in range(ncores):
        b, p = c // 2, c % 2
        m = _consts(T, TP, p)
        m["xo"] = x_in[b, p * T:(p + 1) * T]
        m["xp"] = x_in[b, 0:TP]
        m["cvec_in"] = inp["c"][b].reshape(16, 128).T
        m = {k: np.ascontiguousarray(np.asarray(v, dtype=f)) for k, v in m.items() if k in B.inputs}
        m.update(shared)
        in_maps.append(m)
    res = run_bass_kernel_spmd(nc, in_maps, core_ids=list(range(ncores)), **_RUN_KW)
    _LAST['res'] = res
    out = np.empty_like(x_in)
    for c in range(ncores):
        b, p = c // 2, c % 2
        out[b, p * T:(p + 1) * T] = res.results[c]["out"]
    return out


def run_layer(l, x_in, inp, T, TP, ncores, stop=99):
    return run_layers([l], x_in, inp, T, TP, ncores)


def kernel(**inputs):
    inp = {k: np.asarray(v) for k, v in inputs.items()}
    x = np.asarray(inp["x"], dtype=np.float32)
    Bn, S, _ = x.shape
    T = S // 2
    ncores = Bn * 2
    return run_layers(list(range(DEPTH)), x, inp, T, T, ncores)
```

```python
import math
import os
import numpy as np
import concourse.bass as bass
import concourse.mybir as mybir
from concourse.bass_utils import run_bass_kernel_spmd

F32 = mybir.dt.float32
BF16 = mybir.dt.bfloat16
AF = mybir.ActivationFunctionType
ALU = mybir.AluOpType
AX = mybir.AxisListType

D = 2048
KC = 16
DEPTH = 2
ALPHA = (2.0 * DEPTH) ** 0.25
LN_EPS = 1e-5
RMS_EPS = 1e-5
NEG = -30000.0
COMPUTE = ("tensor", "vector", "scalar", "gpsimd")
NDMASEM = 8


class Buf:
    _n = 0

    def __init__(self, h, name, psum=False):
        self.h = h
        self.psum = psum
        Buf._n += 1
        self.key = name + "#" + str(Buf._n)

    def __getitem__(self, idx):
        return self.h[idx]


def _k(x):
    return x.key if isinstance(x, Buf) else x


class _View:
    def __init__(self, ap, key):
        self.ap = ap
        self.key = key

    def __getitem__(self, idx):
        return self.ap


class Prog:
    def __init__(self, nc):
        self.nc = nc
        self.ops = []
        self.last_w = {}
        self.readers = {}
        self.last_eng = {}
        self.last_dma = {}
        self.cc_ops = []

    def sbuf(self, name, shape, dtype):
        return Buf(self.nc.alloc_sbuf_tensor(name, list(shape), dtype), name)

    def psum(self, name, shape, dtype=F32):
        return Buf(self.nc.alloc_psum_tensor(name, list(shape), dtype), name, psum=True)

    def op(self, eng, fn, reads=(), writes=(), dma=False, cc=False):
        i = len(self.ops)
        deps = set()
        pr = [r for r in reads if isinstance(r, Buf) and r.psum]
        if pr:
            reads = [r for r in reads if not (isinstance(r, Buf) and r.psum)]
            writes = list(writes) + [r for r in pr if r not in writes]
        for r in reads:
            k = _k(r)
            if k in self.last_w:
                deps.add(self.last_w[k])
        for w in writes:
            k = _k(w)
            if k in self.last_w:
                deps.add(self.last_w[k])
            for rd in self.readers.get(k, ()):
                deps.add(rd)
        for r in reads:
            self.readers.setdefault(_k(r), []).append(i)
        for w in writes:
            k = _k(w)
            self.last_w[k] = i
            self.readers[k] = []
        deps.discard(i)
        self.ops.append(dict(eng=eng, fn=fn, deps=deps, dma=dma, signal=False, cc=cc))
        if cc:
            self.cc_ops.append(i)
        elif dma:
            self.last_dma.setdefault(eng, []).append(i)
            self.last_dma[eng] = self.last_dma[eng][-NDMASEM:]
        else:
            self.last_eng[eng] = i
        return i

    def fence(self):
        deps = set(self.last_eng.values())
        for v in self.last_dma.values():
            deps.update(v)
        deps.update(self.cc_ops[-1:])
        for e in COMPUTE + ("sync",):
            self.ops.append(dict(eng=e, fn=None, deps=set(deps), dma=False, signal=False, cc=False))
        self.last_w = {}
        self.readers = {}

    def emit(self):
        nc = self.nc
        ops = self.ops

        def dom(o):
            if o["cc"]:
                return "cc"
            return ("dma", o["eng"]) if o["dma"] else o["eng"]

        ccsem = nc.alloc_semaphore(name="s_cc") if self.cc_ops else None
        ncc = 0
        for o in ops:
            if o["cc"]:
                ncc += 1
            o["ncc_before"] = ncc
        for o in ops:
            for d in o["deps"]:
                od = ops[d]
                if od["cc"]:
                    continue
                if (not od["dma"]) and (not o["dma"]) and od["eng"] == o["eng"] == "tensor":
                    continue
                od["signal"] = True
        cnt = {}
        dma_idx = {}
        for o in ops:
            dm = dom(o)
            if o["cc"]:
                continue
            if o["dma"]:
                n = dma_idx.get(dm, 0)
                o["dslot"] = n % NDMASEM
                o["dround"] = n // NDMASEM
                dma_idx[dm] = n + 1
            elif o["signal"] and o["fn"] is not None:
                cnt[dm] = cnt.get(dm, 0) + 1
                o["cnt"] = cnt[dm]
        sems = {e: nc.alloc_semaphore(name="s_" + e) for e in COMPUTE}
        dsems = {dm: [nc.alloc_semaphore(name="d_%s_%d" % (dm[1], k)) for k in range(NDMASEM)]
                 for dm in dma_idx}
        streams = {}
        for i, o in enumerate(ops):
            streams.setdefault(o["eng"], []).append(i)

        def run_stream(eng_name, engine):
            waited = {}

            def wait(sem, key, val):
                if waited.get(key, -1) >= val:
                    return
                engine.wait_ge(sem, val)
                waited[key] = val

            for i in streams.get(eng_name, []):
                o = ops[i]
                for d in sorted(o["deps"]):
                    od = ops[d]
                    if od["cc"]:
                        wait(ccsem, "cc", o["ncc_before"] - (1 if o["cc"] else 0))
                        continue
                    if od["dma"]:
                        dm = dom(od)
                        wait(dsems[dm][od["dslot"]], (dm, od["dslot"]), 16 * (od["dround"] + 1))
                    else:
                        if "cnt" not in od:
                            continue
                        if od["eng"] == eng_name == "tensor" and not o["dma"]:
                            continue
                        wait(sems[od["eng"]], od["eng"], od["cnt"])
                if o["dma"] and not o["cc"] and o["dround"] > 0:
                    dm = dom(o)
                    wait(dsems[dm][o["dslot"]], (dm, o["dslot"]), 16 * o["dround"])
                if o["fn"] is None:
                    continue
                ins = o["fn"](engine)
                if o["cc"]:
                    ins.then_inc(ccsem, 1)
                elif o["dma"]:
                    ins.then_inc(dsems[dom(o)][o["dslot"]], 16)
                elif "cnt" in o:
                    ins.then_inc(sems[eng_name], 1)

        with nc.Block() as block:
            @block.tensor
            def _(e):
                run_stream("tensor", e)

            @block.vector
            def _(e):
                run_stream("vector", e)

            @block.scalar
            def _(e):
                run_stream("scalar", e)

            @block.gpsimd
            def _(e):
                run_stream("gpsimd", e)

            @block.sync
            def _(e):
                run_stream("sync", e)


class Builder:
    def __init__(self, T, TP, layers, n_exp_groups=4):
        self.T, self.TP, self.TC = T, TP, T + TP
        self.layers = layers
        nc = self.nc = bass.Bass("TRN2", target_bir_lowering=False)
        P = self.P = Prog(nc)
        self.inputs = {}
        self.BIGA = P.sbuf("BIGA", [128, 32768], BF16)
        self.BIGB = P.sbuf("BIGB", [128, 16384], BF16)
        self.W = [P.sbuf("W%d" % i, [128, 8192], BF16) for i in range(5)]
        self.BC = [P.sbuf("BC%d" % i, [128, 2048], F32) for i in range(3)]
        self.ident = P.sbuf("ident", [128, 128], F32)
        self.identb = P.sbuf("identb", [128, 128], BF16)
        self.tri = P.sbuf("tri", [128, 128], F32)
        self.trib = P.sbuf("trib", [128, 128], BF16)
        self.rotT = P.sbuf("rotT", [128, 128], BF16)
        self.ones = P.sbuf("ones", [128, 128], F32)
        self.pbias = P.sbuf("pbias", [128, 1], F32)
        self.zero = P.sbuf("zero", [128, 1], F32)
        self.cvec = P.sbuf("cvec", [128, 16], F32)
        self.rowbuf = _View(self.BIGA.h.bitcast(F32)[0:1, 0:6144], self.BIGA.key)
        self.small = P.sbuf("small", [128, 64], F32)
        self.lam = P.sbuf("lam", [128, 8], F32)
        self.subw = P.sbuf("subw", [128, 256], F32)
        self.w32 = P.sbuf("w32", [128, 8, 32], F32)
        self.rt = P.sbuf("rt", [128, 128], F32)
        self.wr = P.sbuf("wr", [128, 16, 36], F32)
        self.brt = P.sbuf("brt", [128, 36], F32)
        bf = self.BIGB.h.bitcast(F32)
        self.lf = bf[:, 0:512].rearrange("p (t h) -> p t h", h=16)
        self.cum = bf[:, 512:1024].rearrange("p (t h) -> p t h", h=16)
        self.anc = bf[:, 1024:1088].rearrange("p (t h) -> p t h", h=16)
        self.fb = P.sbuf("fb", [128, 16], F32)
        self.wf = _View(self.BIGB[:, 4096:4352].rearrange("p (k n) -> p k n", k=16), "wfkey")
        self.PS = [P.psum("ps%d" % i, [128, 512], F32) for i in range(8)]

    def din(self, name, shape, dt=F32):
        t = self.nc.dram_tensor(name, list(shape), dt, kind="ExternalInput").ap()
        self.inputs[name] = t
        return t

    def dscr(self, name, shape, dt):
        return self.nc.dram_tensor(name, list(shape), dt).ap()

    def dma(self, eng, out, in_, reads=(), writes=()):
        self.P.op(eng, lambda e: e.dma_start(out=out, in_=in_), reads=reads, writes=writes, dma=True)

    def f32view(self, buf, off_f32, n):
        return buf.h.bitcast(F32)[:, off_f32:off_f32 + n]

    def load_consts(self):
        P = self.P
        c_ident = self.din("c_ident", [128, 128])
        c_tri = self.din("c_tri", [128, 128])
        c_rotT = self.din("c_rotT", [128, 128])
        c_pbias = self.din("c_pbias", [128, 1])
        self.dma("sync", self.ident[:], c_ident, writes=[self.ident])
        self.dma("sync", self.tri[:], c_tri, writes=[self.tri])
        self.dma("sync", self.pbias[:], c_pbias, writes=[self.pbias])
        self.dma("gpsimd", self.identb[:], c_ident, writes=[self.identb])
        self.dma("gpsimd", self.trib[:], c_tri, writes=[self.trib])
        self.dma("gpsimd", self.rotT[:], c_rotT, writes=[self.rotT])
        P.op("vector", lambda e: e.memset(self.ones[:], 1.0), writes=[self.ones])
        P.op("vector", lambda e: e.memset(self.zero[:], 0.0), writes=[self.zero])
        cv = self.din("cvec_in", [128, 16])
        self.dma("sync", self.cvec[:], cv, writes=[self.cvec])
        P.op("scalar", lambda e: e.activation(out=self.cvec[:], in_=self.cvec[:], func=AF.Silu),
             reads=[self.cvec], writes=[self.cvec])

    def modulation(self, wmod, bmod):
        P = self.P
        wst = [self.f32view(self.W[0], 0, 4096), self.f32view(self.W[1], 0, 4096),
               self.f32view(self.W[2], 0, 4096), self.f32view(self.W[3], 0, 4096)]
        wk = [self.W[0], self.W[1], self.W[2], self.W[3]]
        ps = self.PS[0]
        self.dma("sync", self.rowbuf.ap, bmod, writes=[self.rowbuf])
        for n in range(12):
            a, b = (0, 1) if n % 2 == 0 else (2, 3)
            src = wmod[:, n * 512:(n + 1) * 512].rearrange("(k p) n -> p k n", p=128)
            self.dma("sync", wst[a].rearrange("p (k n) -> p k n", k=8), src[:, 0:8, :], writes=[wk[a]])
            self.dma("sync", wst[b].rearrange("p (k n) -> p k n", k=8), src[:, 8:16, :], writes=[wk[b]])
            for k in range(16):
                wb = a if k < 8 else b
                kk = k % 8
                P.op("tensor", lambda e, wb=wb, kk=kk, k=k: e.matmul(
                    out=ps[0:1, :], lhsT=self.cvec[:, k:k + 1], rhs=wst[wb][:, kk * 512:(kk + 1) * 512],
                    start=(k == 0), stop=(k == 15)), reads=[self.cvec, wk[wb]], writes=[ps])
            addc = 0.0 if n < 4 else 1.0
            P.op("vector", lambda e, n=n, addc=addc: e.scalar_tensor_tensor(
                out=self.rowbuf.ap[0:1, n * 512:(n + 1) * 512], in0=ps[0:1, :], scalar=addc,
                in1=self.rowbuf.ap[0:1, n * 512:(n + 1) * 512], op0=ALU.add, op1=ALU.add),
                reads=[ps, self.rowbuf], writes=[self.rowbuf])
        self.bcast_mod()

    def bcast_mod(self):
        P = self.P
        for n in range(12):
            dst = [self.BC[1], self.BC[0], self.BC[2]][n // 4]
            psb = self.PS[1 + n % 2]
            P.op("tensor", lambda e, n=n, psb=psb: e.matmul(
                out=psb[:], lhsT=self.ones[0:1, :], rhs=self.rowbuf.ap[0:1, n * 512:(n + 1) * 512],
                start=True, stop=True), reads=[self.ones, self.rowbuf], writes=[psb])
            P.op("scalar", lambda e, n=n, psb=psb, dst=dst: e.activation(
                out=dst[:, (n % 4) * 512:(n % 4 + 1) * 512], in_=psb[:], func=AF.Copy),
                reads=[psb], writes=[dst])

    def make_hT(self, x_ap, ntok, hT, hT_key, router=None):
        P = self.P
        xt = [self.f32view(self.W[0], 0, 2048), self.f32view(self.W[1], 0, 2048)]
        xk = [self.W[0], self.W[1]]
        htf = self.f32view(self.W[2], 0, 2048)
        for t in range(ntok // 128):
            xb, xkk = xt[t % 2], xk[t % 2]
            self.dma("sync", xb, (x_ap(t) if callable(x_ap) else x_ap[t * 128:(t + 1) * 128, :]), writes=[xkk])
            if router is not None and "acc" in router:
                acc = router["acc"](t)
                P.op("scalar", lambda e, xb=xb, acc=acc: e.activation(out=acc, in_=xb, func=AF.Copy, scale=ALPHA),
                     reads=[xkk], writes=[(router["acckey"], t)])
            P.op("vector", lambda e, xb=xb: e.tensor_tensor(out=xb, in0=xb, in1=self.BC[0][:], op=ALU.mult),
                 reads=[xkk, self.BC[0]], writes=[xkk])
            P.op("vector", lambda e, xb=xb: e.tensor_tensor(out=xb, in0=xb, in1=self.BC[1][:], op=ALU.add),
                 reads=[xkk, self.BC[1]], writes=[xkk])
            for g in range(4):
                ps = self.PS[g % 4]
                for j in range(4):
                    k = g * 4 + j
                    P.op("tensor", lambda e, xb=xb, k=k, j=j, ps=ps: e.transpose(
                        out=ps[:, j * 128:(j + 1) * 128], in_=xb[:, k * 128:(k + 1) * 128], identity=self.ident[:]),
                        reads=[xkk, self.ident], writes=[ps])
                eng = "vector" if g % 2 == 0 else "scalar"
                dst = hT[:, g * 4:(g + 1) * 4, t * 128:(t + 1) * 128]
                src = ps[:].rearrange("p (a b) -> p a b", a=4)
                if eng == "vector":
                    P.op("vector", lambda e, dst=dst, src=src: e.tensor_copy(out=dst, in_=src),
                         reads=[ps], writes=[(hT_key, t)])
                else:
                    P.op("scalar", lambda e, dst=dst, src=src: e.activation(out=dst, in_=src, func=AF.Copy),
                         reads=[ps], writes=[(hT_key, t)])
                if router is not None:
                    P.op("gpsimd" if False else "vector", lambda e, src=src, g=g: e.tensor_copy(
                        out=htf[:, g * 512:(g + 1) * 512].rearrange("p (a b) -> p a b", a=4), in_=src),
                        reads=[ps], writes=[self.W[2]])
            if router is not None:
                self.route(t, htf, router)

    def route(self, t, htf, router):
        P = self.P
        ps = self.PS[4]
        for k in range(16):
            P.op("tensor", lambda e, k=k: e.matmul(out=ps[:, 0:36], lhsT=htf[:, k * 128:(k + 1) * 128],
                                                   rhs=self.wr[:, k, :], start=(k == 0), stop=(k == 15)),
                 reads=[self.W[2], self.wr], writes=[ps])
        rt = self.rt
        V = "vector"

        def v(fn, reads=(), writes=()):
            P.op(V, fn, reads=[rt] + list(reads), writes=[rt] + list(writes))
        v(lambda e: e.tensor_tensor(out=rt[:, 0:36], in0=ps[:, 0:36], in1=self.brt[:], op=ALU.add), reads=[ps, self.brt])
        v(lambda e: e.reduce_max(out=rt[:, 36:37], in_=rt[:, 0:4], axis=AX.X))
        v(lambda e: e.tensor_scalar(out=rt[:, 40:44], in0=rt[:, 0:4], scalar1=rt[:, 36:37], scalar2=None, op0=ALU.is_ge))
        v(lambda e: e.tensor_scalar(out=rt[:, 44:48], in0=rt[:, 0:4], scalar1=rt[:, 36:37], scalar2=None, op0=ALU.subtract))
        P.op("scalar", lambda e: e.activation(out=rt[:, 44:48], in_=rt[:, 44:48], func=AF.Exp, accum_out=rt[:, 37:38]),
             reads=[rt], writes=[rt])
        v(lambda e: e.reciprocal(out=rt[:, 38:39], in_=rt[:, 37:38]))
        v(lambda e: e.tensor_scalar(out=rt[:, 48:56], in0=rt[:, 4:12], scalar1=rt[:, 40:41], scalar2=None, op0=ALU.mult))
        for g in range(1, 4):
            v(lambda e, g=g: e.scalar_tensor_tensor(out=rt[:, 48:56], in0=rt[:, 4 + 8 * g:12 + 8 * g],
                                                    scalar=rt[:, 40 + g:41 + g], in1=rt[:, 48:56],
                                                    op0=ALU.mult, op1=ALU.add))
        v(lambda e: e.reduce_max(out=rt[:, 56:57], in_=rt[:, 48:56], axis=AX.X))
        v(lambda e: e.tensor_scalar(out=rt[:, 64:72], in0=rt[:, 48:56], scalar1=rt[:, 56:57], scalar2=None, op0=ALU.is_ge))
        v(lambda e: e.scalar_tensor_tensor(out=rt[:, 72:80], in0=rt[:, 64:72], scalar=NEG, in1=rt[:, 48:56],
                                           op0=ALU.mult, op1=ALU.add))
        v(lambda e: e.reduce_max(out=rt[:, 57:58], in_=rt[:, 72:80], axis=AX.X))
        v(lambda e: e.tensor_scalar(out=rt[:, 80:88], in0=rt[:, 72:80], scalar1=rt[:, 57:58], scalar2=None, op0=ALU.is_ge))
        v(lambda e: e.tensor_tensor(out=rt[:, 58:59], in0=rt[:, 57:58], in1=rt[:, 56:57], op=ALU.subtract))
        P.op("scalar", lambda e: e.activation(out=rt[:, 59:60], in_=rt[:, 58:59], func=AF.Exp), reads=[rt], writes=[rt])
        v(lambda e: e.tensor_scalar(out=rt[:, 60:61], in0=rt[:, 59:60], scalar1=1.0, scalar2=None, op0=ALU.add))
        v(lambda e: e.reciprocal(out=rt[:, 61:62], in_=rt[:, 60:61]))
        v(lambda e: e.tensor_tensor(out=rt[:, 62:63], in0=rt[:, 59:60], in1=rt[:, 61:62], op=ALU.mult))
        v(lambda e: e.tensor_scalar(out=rt[:, 64:72], in0=rt[:, 64:72], scalar1=rt[:, 61:62], scalar2=None, op0=ALU.mult))
        v(lambda e: e.scalar_tensor_tensor(out=rt[:, 64:72], in0=rt[:, 80:88], scalar=rt[:, 62:63], in1=rt[:, 64:72],
                                           op0=ALU.mult, op1=ALU.add))
        v(lambda e: e.tensor_scalar(out=rt[:, 64:72], in0=rt[:, 64:72], scalar1=rt[:, 38:39], scalar2=None, op0=ALU.mult))
        for g in range(4):
            P.op(V, lambda e, g=g: e.tensor_scalar(out=self.w32[:, t, g * 8:(g + 1) * 8], in0=rt[:, 64:72],
                                                   scalar1=rt[:, 40 + g:41 + g], scalar2=None, op0=ALU.mult),
                 reads=[rt], writes=[("w32", t)])

    def layer_norm(self, z, zkey, gt, bt, scr, scrkey):
        P = self.P
        st = self.small
        P.op("vector", lambda e: e.reduce_sum(out=st[:, 0:1], in_=z, axis=AX.X), reads=[zkey], writes=[st])
        P.op("vector", lambda e: e.tensor_scalar(out=st[:, 1:2], in0=st[:, 0:1], scalar1=-1.0 / D, scalar2=None, op0=ALU.mult),
             reads=[st], writes=[st])
        P.op("scalar", lambda e: e.activation(out=scr, in_=z, func=AF.Square, bias=st[:, 1:2], scale=1.0, accum_out=st[:, 2:3]),
             reads=[zkey, st], writes=[scrkey, st])
        P.op("vector", lambda e: e.tensor_scalar(out=st[:, 3:4], in0=st[:, 2:3], scalar1=1.0 / D, scalar2=LN_EPS, op0=ALU.mult, op1=ALU.add),
             reads=[st], writes=[st])
        P.op("scalar", lambda e: e.activation(out=st[:, 5:6], in_=st[:, 3:4], func=AF.Sqrt), reads=[st], writes=[st])
        P.op("vector", lambda e: e.reciprocal(out=st[:, 4:5], in_=st[:, 5:6]), reads=[st], writes=[st])
        P.op("vector", lambda e: e.tensor_scalar(out=z, in0=z, scalar1=st[:, 1:2], scalar2=st[:, 4:5], op0=ALU.add, op1=ALU.mult),
             reads=[zkey, st], writes=[zkey])
        P.op("vector", lambda e: e.tensor_tensor(out=z, in0=z, in1=gt[:], op=ALU.mult), reads=[zkey, gt], writes=[zkey])
        P.op("vector", lambda e: e.tensor_tensor(out=z, in0=z, in1=bt[:], op=ALU.add), reads=[zkey, bt], writes=[zkey])

    def mixer_params(self, kind, lam_init, lamv, subw, fbias, w_in):
        B = self
        P = self.P
        if kind == "A":
            lt = [B.f32view(B.W[4], i * 128, 128) for i in range(4)]
            for i in range(4):
                B.dma("sync", lt[i], lamv[i].partition_broadcast(128), writes=[B.W[4]])
            P.op("vector", lambda e: e.tensor_tensor(out=lt[0], in0=lt[0], in1=lt[1], op=ALU.mult), reads=[B.W[4]], writes=[B.W[4]])
            P.op("vector", lambda e: e.tensor_tensor(out=lt[2], in0=lt[2], in1=lt[3], op=ALU.mult), reads=[B.W[4]], writes=[B.W[4]])
            P.op("vector", lambda e: e.reduce_sum(out=B.lam[:, 2:3], in_=lt[0], axis=AX.X), reads=[B.W[4]], writes=[B.lam])
            P.op("vector", lambda e: e.reduce_sum(out=B.lam[:, 3:4], in_=lt[2], axis=AX.X), reads=[B.W[4]], writes=[B.lam])
            P.op("scalar", lambda e: e.activation(out=B.lam[:, 4:6], in_=B.lam[:, 2:4], func=AF.Exp), reads=[B.lam], writes=[B.lam])
            P.op("vector", lambda e: e.tensor_tensor(out=B.lam[:, 0:1], in0=B.lam[:, 4:5], in1=B.lam[:, 5:6], op=ALU.subtract), reads=[B.lam], writes=[B.lam])
            P.op("vector", lambda e: e.tensor_scalar(out=B.lam[:, 0:1], in0=B.lam[:, 0:1], scalar1=lam_init, scalar2=None, op0=ALU.add), reads=[B.lam], writes=[B.lam])
            P.op("vector", lambda e: e.tensor_scalar(out=B.lam[:, 1:2], in0=B.lam[:, 0:1], scalar1=-1.0, scalar2=None, op0=ALU.mult), reads=[B.lam], writes=[B.lam])
            B.dma("sync", B.subw[:], subw.partition_broadcast(128), writes=[B.subw])
            P.op("vector", lambda e: e.tensor_scalar(out=B.subw[:], in0=B.subw[:], scalar1=1.0 - lam_init, scalar2=None, op0=ALU.mult), reads=[B.subw], writes=[B.subw])
        else:
            B.dma("sync", B.fb[:], fbias.partition_broadcast(128), writes=[B.fb])
            B.dma("gpsimd", B.wf.ap, w_in[:, 6144:6160].rearrange("(k p) n -> p k n", p=128), writes=[B.wf])

    def qk_post(self, kind, mi, n, tb, tok0, isq, cs, sc):
        P = self.P
        m = (n % 4) * 4 + mi
        ps = self.PS[mi % 2]
        ob = self.W[3][:, (mi % 2) * 512:(mi % 2) * 512 + 512]
        okey = (self.W[3].key, "o", mi % 2)
        if kind == "A":
            qraw = self.W[3][:, 1024 + (mi % 2) * 512:1024 + (mi % 2) * 512 + 512]
            qkey = (self.W[3].key, "q", mi % 2)
            ps2 = self.PS[2 + mi % 2]
            t1 = self.f32view(self.W[3], 1024 + (mi % 2) * 1024, 512)
            t2 = self.f32view(self.W[3], 1024 + (mi % 2) * 1024 + 512, 512)
            tkey = (self.W[3].key, "t", mi % 2)
            P.op("scalar", lambda e: e.activation(out=qraw, in_=ps[:], func=AF.Copy), reads=[ps], writes=[qkey])
            P.op("tensor", lambda e: e.matmul(out=ps2[:], lhsT=self.rotT[:], rhs=qraw, start=True, stop=True),
                 reads=[qkey, self.rotT], writes=[ps2])
            P.op("vector", lambda e: e.tensor_tensor(out=t1, in0=ps[:], in1=cs[0], op=ALU.mult),
                 reads=[ps, (self.W[2].key, "c")], writes=[(tkey, 1)])
            P.op("vector", lambda e: e.tensor_tensor(out=t2, in0=ps2[:], in1=cs[1], op=ALU.mult),
                 reads=[ps2, (self.W[2].key, "s")], writes=[(tkey, 2)])
            P.op("vector", lambda e: e.tensor_tensor(out=ob, in0=t1, in1=t2, op=ALU.add),
                 reads=[(tkey, 1), (tkey, 2)], writes=[okey])
        else:
            P.op("scalar", lambda e: e.activation(out=ob, in_=ps[:], func=AF.Copy), reads=[ps], writes=[okey])
        if isq:
            dst = sc["qT"][m, :, tb * 512:(tb + 1) * 512]
            dk = ("qT", m)
        else:
            dst = sc["kT"][m, :, tok0 + tb * 512:tok0 + (tb + 1) * 512]
            dk = ("kT", m)
        self.dma("sync", dst, ob, reads=[okey], writes=[dk])

    def qkv(self, l, kind, w_in, hT, hT_key, ntok, tok0, with_q, sc):
        P = self.P
        wb = [self.W[0], self.W[1]]
        nblk = 12
        first = 0 if with_q else 4
        bi = 0
        import os
        skip = os.environ.get("QKV_SKIP", "")
        for n in range(first, nblk):
            if (skip == "v" and n >= 8) or (skip == "qk" and n < 8):
                continue
            wt = wb[bi % 2]
            bi += 1
            wv = wt[:].rearrange("p (k n) -> p k n", k=16)
            self.dma("gpsimd", wv, w_in[:, n * 512:(n + 1) * 512].rearrange("(k p) n -> p k n", p=128), writes=[wt])
            if n < 8:
                isq = n < 4
                for tb in range(ntok // 512):
                    if kind == "A":
                        cs = [self.f32view(self.W[2], 0, 512), self.f32view(self.W[2], 512, 512)]
                        t0 = tok0 + tb * 512
                        self.dma("sync", cs[0], sc["cos"][:, t0:t0 + 512], writes=[(self.W[2].key, "c")])
                        self.dma("sync", cs[1], sc["sin"][:, t0:t0 + 512], writes=[(self.W[2].key, "s")])
                    pend = None
                    for mi in range(5):
                        if mi < 4:
                            ps = self.PS[mi % 2]
                            for k in range(16):
                                P.op("tensor", lambda e, k=k, mi=mi, tb=tb, ps=ps, wv=wv: e.matmul(
                                    out=ps[:], lhsT=wv[:, k, mi * 128:(mi + 1) * 128], rhs=hT[:, k, tb * 512:(tb + 1) * 512],
                                    start=(k == 0), stop=(k == 15)), reads=[wt] + [(hT_key, tb * 4 + i) for i in range(4)], writes=[ps])
                        if pend is not None:
                            self.qk_post(kind, pend, n, tb, tok0, isq, cs if kind == "A" else None, sc)
                        pend = mi if mi < 4 else None
            else:
                hv = 256 if kind == "A" else 128
                nh = 512 // hv
                for tt in range(ntok // 128):
                    ps = self.PS[4 + tt % 2]
                    for k in range(16):
                        P.op("tensor", lambda e, k=k, tt=tt, ps=ps, wv=wv: e.matmul(
                            out=ps[:], lhsT=hT[:, k, tt * 128:(tt + 1) * 128], rhs=wv[:, k, :],
                            start=(k == 0), stop=(k == 15)), reads=[wt, (hT_key, tt)], writes=[ps])
                    vb = self.W[2][:, 2048 + (tt % 2) * 1024:2048 + (tt % 2) * 1024 + nh * (hv + 1)].rearrange("p (h c) -> p h c", h=nh)
                    vkey = (self.W[2].key, "v", tt % 2)
                    P.op("scalar" if tt % 2 == 0 else "vector",
                         (lambda e, vb=vb, ps=ps: e.activation(out=vb[:, :, 0:hv], in_=ps[:].rearrange("p (h c) -> p h c", h=nh), func=AF.Copy))
                         if tt % 2 == 0 else
                         (lambda e, vb=vb, ps=ps: e.tensor_copy(out=vb[:, :, 0:hv], in_=ps[:].rearrange("p (h c) -> p h c", h=nh))),
                         reads=[ps], writes=[vkey])
                    P.op("gpsimd", lambda e, vb=vb: e.memset(vb[:, :, hv:hv + 1], 1.0), writes=[vkey])
                    h0 = (n - 8) * nh
                    ct = (tok0 // 128) + tt
                    self.dma("sync", sc["v"][h0:h0 + nh, ct, :, :].rearrange("h p c -> p h c"), vb, reads=[vkey], writes=[("v", h0)])
        if kind == "B":
            P.op("sync", None, reads=[self.wf])
            for tt in range(ntok // 128):
                ps = self.PS[6]
                ct = (tok0 // 128) + tt
                for k in range(16):
                    P.op("tensor", lambda e, k=k, tt=tt: e.matmul(out=ps[:, 0:16], lhsT=hT[:, k, tt * 128:(tt + 1) * 128],
                                                                  rhs=self.wf.ap[:, k, :], start=(k == 0), stop=(k == 15)),
                         reads=[self.wf, (hT_key, tt)], writes=[ps])
                st = self.small
                P.op("vector", lambda e: e.tensor_tensor(out=st[:, 16:32], in0=ps[:, 0:16], in1=self.fb[:], op=ALU.add),
                     reads=[ps, self.fb], writes=[st])
                P.op("scalar", lambda e: e.activation(out=st[:, 32:48], in_=st[:, 16:32], func=AF.Exp, scale=-1.0), reads=[st], writes=[st])
                P.op("scalar", lambda e: e.activation(out=st[:, 32:48], in_=st[:, 32:48], func=AF.Ln, bias=1.0, scale=1.0), reads=[st], writes=[st])
                P.op("vector", lambda e, ct=ct: e.tensor_scalar(out=self.lf[:, ct, :], in0=st[:, 32:48], scalar1=-1.0, scalar2=None, op0=ALU.mult),
                     reads=[st], writes=[("lf", ct)])

    def attention(self, l, kind, sc):
        P = self.P
        T, TP, TC = self.T, self.TP, self.TC
        nprev = TP // 128
        nq = T // 512
        scale = 128 ** -0.5
        if kind == "A":
            nheads, nmaps, hv = 8, 2, 256
        else:
            nheads, nmaps, hv = 16, 1, 128
        hv1 = hv + 1
        kTs = [self.BIGA[:, m * TC:(m + 1) * TC] for m in range(nmaps)]
        o0 = nmaps * TC
        qTs = [self.BIGA[:, o0 + m * T:o0 + (m + 1) * T] for m in range(nmaps)]
        o1 = o0 + nmaps * T
        v1 = self.BIGA[:, o1:o1 + (TC // 128) * hv1].rearrange("p (t c) -> p t c", c=hv1)
        assert o1 + (TC // 128) * hv1 <= 32768
        if kind == "B":
            ps = self.PS[7]
            for t in range(TC // 128):
                for t2 in range(t + 1):
                    P.op("tensor", lambda e, t=t, t2=t2: e.matmul(
                        out=ps[:, 0:16], lhsT=(self.tri[:] if t2 == t else self.ones[:]), rhs=self.lf[:, t2, :],
                        start=(t2 == 0), stop=(t2 == t)), reads=[("lf", t2), self.tri, self.ones], writes=[ps])
                P.op("vector", lambda e, t=t: e.tensor_copy(out=self.cum[:, t, :], in_=ps[:, 0:16]), reads=[ps], writes=[("cum", t)])
            for g in range(nq):
                ta = (TP + g * 512 + 256) // 128
                for t2 in range(ta):
                    P.op("tensor", lambda e, t2=t2, ta=ta: e.matmul(out=ps[:, 0:16], lhsT=self.ones[:], rhs=self.lf[:, t2, :],
                                                                    start=(t2 == 0), stop=(t2 == ta - 1)),
                         reads=[("lf", t2), self.ones], writes=[ps])
                P.op("vector", lambda e, g=g: e.tensor_copy(out=self.anc[:, g, :], in_=ps[:, 0:16]), reads=[ps], writes=[("anc", g)])
        ebuf = [self.W[0], self.W[1], self.W[4]]
        for h in range(nheads):
            for m in range(nmaps):
                mm = h * nmaps + m
                self.dma("sync", kTs[m], sc["kT"][mm, :, :], reads=[("kT", mm)], writes=[("kTs", m)])
                self.dma("sync", qTs[m], sc["qT"][mm, :, :], reads=[("qT", mm)], writes=[("qTs", m)])
            self.dma("sync", v1, sc["v"][h, :, :, :].rearrange("t p c -> p t c"), reads=[("v", (h // (512 // hv)) * (512 // hv))], writes=["v1"])
            for g in range(nq):
                nkb = nprev + 4 * g + 4
                o1n = self.f32view(self.W[2], 0, 1024).rearrange("p (i c) -> p i c", i=4)
                for m in range(nmaps):
                    def emit_S(j, m=m, g=g):
                        pss = self.PS[4 + j % 3]
                        P.op("tensor", lambda e, m=m, j=j, g=g, pss=pss: e.matmul(
                            out=pss[:], lhsT=kTs[m][:, j * 128:(j + 1) * 128], rhs=qTs[m][:, g * 512:(g + 1) * 512],
                            start=True, stop=True), reads=[("kTs", m), ("qTs", m)], writes=[pss])

                    emit_S(0)
                    emit_S(1)
                    for j in range(nkb):
                        if j + 2 < nkb:
                            emit_S(j + 2)
                        pss = self.PS[4 + j % 3]
                        et = ebuf[j % 3]
                        ev = et[:, 0:512]
                        if kind == "A":
                            bias = self.pbias[:, 0:1] if j < nprev else self.zero[:, 0:1]
                            breads = [self.pbias, self.zero]
                        else:
                            bcol = self.small[:, 48 + (j % 3):49 + (j % 3)]
                            P.op("vector", lambda e, bcol=bcol, g=g, j=j, h=h: e.tensor_tensor(
                                out=bcol, in0=self.anc[:, g, h:h + 1], in1=self.cum[:, j, h:h + 1], op=ALU.subtract),
                                reads=[("anc", g), ("cum", j)], writes=[("bcol", j % 3)])
                            if j < nprev:
                                P.op("vector", lambda e, bcol=bcol: e.tensor_tensor(out=bcol, in0=bcol, in1=self.pbias[:, 0:1], op=ALU.add),
                                     reads=[("bcol", j % 3), self.pbias], writes=[("bcol", j % 3)])
                            bias = bcol
                            breads = [("bcol", j % 3)]
                        P.op("scalar", lambda e, ev=ev, pss=pss, bias=bias: e.activation(out=ev, in_=pss[:], func=AF.Exp, bias=bias, scale=scale),
                             reads=[pss] + breads, writes=[et])
                        jo = j - nprev
                        for i in range(4):
                            qi = 4 * g + i
                            if jo > qi:
                                continue
                            if jo == qi:
                                P.op("gpsimd", lambda e, ev=ev, i=i: e.tensor_tensor(
                                    out=ev[:, i * 128:(i + 1) * 128], in0=ev[:, i * 128:(i + 1) * 128], in1=self.trib[:], op=ALU.mult),
                                    reads=[et, self.trib], writes=[et])
                            pso = self.PS[i]
                            last = nprev + qi
                            P.op("tensor", lambda e, ev=ev, i=i, j=j, pso=pso, last=last: e.matmul(
                                out=pso[:, 0:hv1], lhsT=ev[:, i * 128:(i + 1) * 128], rhs=v1[:, j, :],
                                start=(j == 0), stop=(j == last)), reads=[et, "v1"], writes=[pso])
                    st = self.small
                    for i in range(4):
                        pso = self.PS[i]
                        qi = 4 * g + i
                        P.op("vector", lambda e, pso=pso, i=i: e.reciprocal(out=st[:, 8 + i:9 + i], in_=pso[:, hv:hv1]), reads=[pso], writes=[st])
                        if kind == "A" and m == 0:
                            P.op("vector", lambda e, pso=pso, i=i: e.tensor_scalar(
                                out=o1n[:, i, :], in0=pso[:, 0:hv], scalar1=st[:, 8 + i:9 + i], scalar2=None, op0=ALU.mult),
                                reads=[pso, st], writes=[(self.W[2].key, "o1n", i)])
                        elif kind == "A":
                            ot = self.f32view(self.W[3], i * 256, 256)
                            okey = (self.W[3].key, "ot", i)
                            P.op("vector", lambda e, i=i: e.tensor_tensor(out=st[:, 12 + i:13 + i], in0=st[:, 8 + i:9 + i], in1=self.lam[:, 1:2], op=ALU.mult),
                                 reads=[st, self.lam], writes=[st])
                            P.op("vector", lambda e, pso=pso, i=i, ot=ot: e.scalar_tensor_tensor(
                                out=ot, in0=pso[:, 0:hv], scalar=st[:, 12 + i:13 + i], in1=o1n[:, i, :], op0=ALU.mult, op1=ALU.add),
                                reads=[pso, st, (self.W[2].key, "o1n", i)], writes=[okey])
                            sq = self.f32view(self.W[3], 1024 + i * 256, 256)
                            P.op("scalar", lambda e, ot=ot, sq=sq, i=i: e.activation(out=sq, in_=ot, func=AF.Square, accum_out=st[:, 16 + i:17 + i]),
                                 reads=[okey], writes=[(self.W[3].key, "sq", i), st])
                            P.op("vector", lambda e, i=i: e.tensor_scalar(out=st[:, 20 + i:21 + i], in0=st[:, 16 + i:17 + i], scalar1=1.0 / 256,
                                                                         scalar2=RMS_EPS, op0=ALU.mult, op1=ALU.add), reads=[st], writes=[st])
                            P.op("scalar", lambda e, i=i: e.activation(out=st[:, 24 + i:25 + i], in_=st[:, 20 + i:21 + i], func=AF.Sqrt), reads=[st], writes=[st])
                            P.op("vector", lambda e, i=i: e.reciprocal(out=st[:, 28 + i:29 + i], in_=st[:, 24 + i:25 + i]), reads=[st], writes=[st])
                            ob = self.W[3][:, 4096 + i * 256:4096 + (i + 1) * 256]
                            obk = (self.W[3].key, "ob", i)
                            P.op("vector", lambda e, ot=ot, ob=ob, i=i: e.scalar_tensor_tensor(
                                out=ob, in0=ot, scalar=st[:, 28 + i:29 + i], in1=self.subw[:], op0=ALU.mult, op1=ALU.mult),
                                reads=[okey, st, self.subw], writes=[obk])
                            self.dma("sync", sc["o"][qi * 128:(qi + 1) * 128, h * 256:(h + 1) * 256], ob, reads=[obk], writes=[("o", qi)])
                        else:
                            ob = self.W[3][:, 4096 + i * 128:4096 + (i + 1) * 128]
                            obk = (self.W[3].key, "ob", i)
                            P.op("vector", lambda e, pso=pso, ob=ob, i=i: e.tensor_scalar(
                                out=ob, in0=pso[:, 0:hv], scalar1=st[:, 8 + i:9 + i], scalar2=None, op0=ALU.mult),
                                reads=[pso, st], writes=[obk])
                            self.dma("sync", sc["o"][qi * 128:(qi + 1) * 128, h * 128:(h + 1) * 128], ob, reads=[obk], writes=[("o", qi)])

    def out_proj(self, l, w_out, x_ap, lng, lnb, sc, x1_ap):
        P = self.P
        T = self.T
        wo = [self.W[i] for i in range(4)]
        for n in range(4):
            self.dma("gpsimd", wo[n][:].rearrange("p (k n) -> p k n", k=16),
                     w_out[:, n * 512:(n + 1) * 512].rearrange("(k p) n -> p k n", p=128), writes=[wo[n]])
        self.dma("sync", self.BC[0][:], lng.partition_broadcast(128), writes=[self.BC[0]])
        self.dma("sync", self.BC[1][:], lnb.partition_broadcast(128), writes=[self.BC[1]])
        for t in range(T // 128):
            ob = self.BIGB[:, (t % 2) * 2048:(t % 2 + 1) * 2048]
            obk = ("ob", t % 2)
            self.dma("sync", ob, sc["o"][t * 128:(t + 1) * 128, :], reads=[("o", t)], writes=[obk])
            xt = self.f32view(self.BIGA, (t % 2) * 2048, 2048)
            xk = ("xt", t % 2)
            self.dma("sync", xt, x_ap[t * 128:(t + 1) * 128, :], writes=[xk])
            oT = self.BIGB[:, 4096 + (t % 2) * 2048:4096 + (t % 2 + 1) * 2048].rearrange("p (k n) -> p k n", k=16)
            oTk = ("oT", t % 2)
            for g in range(4):
                ps = self.PS[4 + g % 2]
                psb = ps.h.bitcast(BF16)
                for j in range(4):
                    k = g * 4 + j
                    P.op("tensor", lambda e, ob=ob, k=k, j=j, psb=psb: e.transpose(
                        out=psb[:, j * 128:(j + 1) * 128], in_=ob[:, k * 128:(k + 1) * 128], identity=self.identb[:]),
                        reads=[obk, self.identb], writes=[ps])
                P.op("scalar" if g % 2 else "vector",
                     (lambda e, oT=oT, psb=psb, g=g: e.activation(out=oT[:, g * 4:(g + 1) * 4, :], in_=psb[:, 0:512].rearrange("p (a b) -> p a b", a=4), func=AF.Copy))
                     if g % 2 else
                     (lambda e, oT=oT, psb=psb, g=g: e.tensor_copy(out=oT[:, g * 4:(g + 1) * 4, :], in_=psb[:, 0:512].rearrange("p (a b) -> p a b", a=4))),
                     reads=[ps], writes=[oTk])
            y = self.f32view(self.BIGA, 4096 + (t % 2) * 2048, 2048)
            yk = ("y", t % 2)
            for n in range(4):
                ps = self.PS[n]
                for k in range(16):
                    P.op("tensor", lambda e, n=n, k=k, oT=oT, ps=ps: e.matmul(
                        out=ps[:], lhsT=oT[:, k, :], rhs=wo[n][:, k * 512:(k + 1) * 512], start=(k == 0), stop=(k == 15)),
                        reads=[oTk, wo[n]], writes=[ps])
                P.op("vector", lambda e, n=n, ps=ps, y=y: e.tensor_tensor(
                    out=y[:, n * 512:(n + 1) * 512], in0=ps[:], in1=self.BC[2][:, n * 512:(n + 1) * 512], op=ALU.mult),
                    reads=[ps, self.BC[2]], writes=[yk])
            P.op("vector", lambda e, xt=xt, y=y: e.scalar_tensor_tensor(out=xt, in0=xt, scalar=ALPHA, in1=y, op0=ALU.mult, op1=ALU.add),
                 reads=[xk, yk], writes=[xk])
            self.layer_norm(xt, xk, self.BC[0], self.BC[1], y, yk)
            self.dma("sync", x1_ap[t * 128:(t + 1) * 128, :], xt, reads=[xk], writes=[("x1", t)])

    def moe(self, l, W, x1_ap, out_ap, outkey):
        P = self.P
        T = self.T
        TPASS = min(1024, T)
        npass = T // TPASS
        self.dma("sync", self.wr[:, :, 0:4], W["w_group"].rearrange("(k p) g -> p k g", p=128), writes=[self.wr])
        for g in range(4):
            self.dma("sync", self.wr[:, :, 4 + 8 * g:12 + 8 * g], W["w_router"][g].rearrange("(k p) e -> p k e", p=128), writes=[self.wr])
        self.dma("sync", self.brt[:, 0:4], W["b_group"].partition_broadcast(128), writes=[self.brt])
        self.dma("sync", self.brt[:, 4:36], W["b_router"].partition_broadcast(128), writes=[self.brt])
        if npass > 1:
            if not hasattr(self, "bcsave"):
                self.bcsave = self.dscr("s_bcsave", [2, 128, 2048], F32)
            self.dma("sync", self.bcsave[0], self.BC[0][:], reads=[self.BC[0]], writes=["bcsave0"])
            self.dma("sync", self.bcsave[1], self.BC[1][:], reads=[self.BC[1]], writes=["bcsave1"])
        ntile = TPASS // 128
        hT2 = self.BIGB[:, 0:16 * TPASS].rearrange("p (k t) -> p k t", k=16)
        accv = self.BIGA.h.bitcast(F32)
        hidbufs = [self.BC[0].h.bitcast(BF16)[:, i * 2048:(i + 1) * 2048].rearrange("p (f t) -> p f t", f=4) for i in range(2)]
        sgbufs = [self.BC[1][:, i * 512:(i + 1) * 512] for i in range(2)]
        for ps_i in range(npass):
            P.fence()
            tok0 = ps_i * TPASS
            if ps_i > 0:
                self.dma("sync", self.BC[0][:], self.bcsave[0], writes=[self.BC[0]])
                self.dma("sync", self.BC[1][:], self.bcsave[1], writes=[self.BC[1]])
            self.make_hT(x1_ap[tok0:tok0 + TPASS, :], TPASS, hT2, "hT2", router=dict())
            P.fence()
            hi = 0
            for ex in range(32):
                g, e = ex // 8, ex % 8
                wg = self.W[0 + ex % 2]
                wu = self.W[2 + ex % 2]
                wd = self.W[4]
                wgv = wg[:].rearrange("p (k n) -> p k n", k=16)
                wuv = wu[:].rearrange("p (k n) -> p k n", k=16)
                wdv = wd[:].rearrange("p (k n) -> p k n", k=4)
                self.dma("gpsimd", wgv, W["w_gate"][g, e].rearrange("(k p) n -> p k n", p=128), writes=[wg])
                self.dma("gpsimd", wuv, W["w_up"][g, e].rearrange("(k p) n -> p k n", p=128), writes=[wu])
                self.dma("gpsimd", wdv, W["w_down"][g, e].rearrange("(k p) n -> p k n", p=128), writes=[wd])
                for tb in range(TPASS // 512):
                    hb = hi % 2
                    hi += 1
                    hidb = hidbufs[hb]
                    for fc in range(4):
                        psg = self.PS[fc % 2]
                        psu = self.PS[2 + fc % 2]
                        for k in range(16):
                            P.op("tensor", lambda e_, k=k, fc=fc, tb=tb, psg=psg, wgv=wgv: e_.matmul(
                                out=psg[:], lhsT=wgv[:, k, fc * 128:(fc + 1) * 128], rhs=hT2[:, k, tb * 512:(tb + 1) * 512],
                                start=(k == 0), stop=(k == 15)), reads=[wg] + [("hT2", tb * 4 + i) for i in range(4)], writes=[psg])
                        for k in range(16):
                            P.op("tensor", lambda e_, k=k, fc=fc, tb=tb, psu=psu, wuv=wuv: e_.matmul(
                                out=psu[:], lhsT=wuv[:, k, fc * 128:(fc + 1) * 128], rhs=hT2[:, k, tb * 512:(tb + 1) * 512],
                                start=(k == 0), stop=(k == 15)), reads=[wu] + [("hT2", tb * 4 + i) for i in range(4)], writes=[psu])
                        sg = sgbufs[fc % 2]
                        sgk = ("sg", fc % 2)
                        P.op("scalar", lambda e_, sg=sg, psg=psg: e_.activation(out=sg, in_=psg[:], func=AF.Silu), reads=[psg], writes=[sgk])
                        P.op("vector", lambda e_, sg=sg, psu=psu, hidb=hidb, fc=fc: e_.tensor_tensor(
                            out=hidb[:, fc, :], in0=psu[:], in1=sg, op=ALU.mult), reads=[psu, sgk], writes=[("hid", hb, fc)])
                    for tt in range(4):
                        t = tb * 4 + tt
                        for dmb in range(4):
                            psd = self.PS[4 + (tt * 4 + dmb) % 4]
                            for fc in range(4):
                                P.op("tensor", lambda e_, fc=fc, tt=tt, dmb=dmb, psd=psd, hidb=hidb, wdv=wdv: e_.matmul(
                                    out=psd[:], lhsT=hidb[:, fc, tt * 128:(tt + 1) * 128], rhs=wdv[:, fc, dmb * 512:(dmb + 1) * 512],
                                    start=(fc == 0), stop=(fc == 3)), reads=[("hid", hb, fc), wd], writes=[psd])
                            av = accv[:, t * 2048 + dmb * 512:t * 2048 + (dmb + 1) * 512]
                            if ex == 0:
                                P.op("vector", lambda e_, psd=psd, av=av, t=t, ex=ex: e_.tensor_scalar(
                                    out=av, in0=psd[:], scalar1=self.w32[:, t, ex:ex + 1], scalar2=None, op0=ALU.mult),
                                    reads=[psd, ("w32", t)], writes=[("acc", t)])
                            else:
                                P.op("vector", lambda e_, psd=psd, av=av, t=t, ex=ex: e_.scalar_tensor_tensor(
                                    out=av, in0=psd[:], scalar=self.w32[:, t, ex:ex + 1], in1=av, op0=ALU.mult, op1=ALU.add),
                                    reads=[psd, ("w32", t), ("acc", t)], writes=[("acc", t)])
            P.fence()
            self.dma("sync", self.f32view(self.W[0], 0, 2048), W["ln_g"].partition_broadcast(128), writes=[self.W[0]])
            self.dma("sync", self.f32view(self.W[1], 0, 2048), W["ln_b"].partition_broadcast(128), writes=[self.W[1]])
            gt = _View(self.f32view(self.W[0], 0, 2048), self.W[0].key)
            bt = _View(self.f32view(self.W[1], 0, 2048), self.W[1].key)
            for t in range(ntile):
                z = accv[:, t * 2048:(t + 1) * 2048]
                xt = self.f32view(self.W[3], (t % 2) * 2048, 2048)
                xk = (self.W[3].key, "xt", t % 2)
                self.dma("sync", xt, x1_ap[tok0 + t * 128:tok0 + (t + 1) * 128, :], writes=[xk])
                P.op("vector", lambda e_, z=z: e_.tensor_tensor(out=z, in0=z, in1=self.BC[2][:], op=ALU.mult),
                     reads=[("acc", t), self.BC[2]], writes=[("acc", t)])
                P.op("vector", lambda e_, z=z, xt=xt: e_.scalar_tensor_tensor(out=z, in0=xt, scalar=ALPHA, in1=z, op0=ALU.mult, op1=ALU.add),
                     reads=[("acc", t), xk], writes=[("acc", t)])
                scr = self.f32view(self.W[2], (t % 2) * 2048, 2048)
                self.layer_norm(z, ("acc", t), gt, bt, scr, (self.W[2].key, "scr", t % 2))
                self.dma("sync", out_ap[tok0 + t * 128:tok0 + (t + 1) * 128, :], z, reads=[("acc", t)], writes=[(outkey, tok0 // 128 + t)])

    def rowhid(self, tb):
        o = 8192 + (tb % 2) * 2048
        return self.BIGB[:, o:o + 2048].rearrange("p (f t) -> p f t", f=4)

    def sgbuf(self, i):
        return self.BIGB.h.bitcast(F32)[:, 6144 + i * 512:6144 + (i + 1) * 512]


Buf.register = None


def _is_buf(x):
    return isinstance(x, (Buf, _View))


def _k(x):
    return x.key if isinstance(x, (Buf, _View)) else x


def build_program(T, TP, layers, ncores=8):
    B = Builder(T, TP, layers)
    nc, P = B.nc, B.P
    TC = T + TP
    B.load_consts()
    xo = B.din("xo", [T, D])
    xp = B.din("xp", [TP, D])
    cosT = B.din("c_cos", [128, TC])
    sinT = B.din("c_sin", [128, TC])
    out = nc.dram_tensor("out", [T, D], F32, kind="ExternalOutput").ap()
    sc = dict(
        qT=B.dscr("s_qT", [16, 128, T], BF16), kT=B.dscr("s_kT", [16, 128, TC], BF16),
        o=B.dscr("s_o", [T, D], BF16), cos=cosT, sin=sinT)
    vA = B.dscr("s_vA", [8, TC // 128, 128, 257], BF16)
    vB = B.dscr("s_vB", [16, TC // 128, 128, 129], BF16)
    x1 = B.dscr("s_x1", [T, D], F32)
    CH = 256
    nch = T // CH
    if len(layers) > 1:
        x2 = B.dscr("s_x2", [T, D], F32)
        gath = B.dscr("s_gath", [nch, 2 * CH, D], F32)
    cur_o, cur_p = xo, xp
    for li, l in enumerate(layers):
        kind = "A" if l % 2 == 0 else "B"
        last = (li == len(layers) - 1)
        sc["v"] = vA if kind == "A" else vB
        wmod = B.din("mix_mod_w%d" % l, [D, 6144])
        bmod = B.din("mix_mod_b%d" % l, [1, 6144])
        lng = B.din("mix_ln_g%d" % l, [1, D])
        lnb = B.din("mix_ln_b%d" % l, [1, D])
        lam_init = lamv = subw = fbias = None
        if kind == "A":
            w_in = B.din("a_w_in", [D, 6144])
            w_out = B.din("a_w_out", [D, D])
            lamv = [B.din("a_lam_q1", [1, 128]), B.din("a_lam_k1", [1, 128]),
                    B.din("a_lam_q2", [1, 128]), B.din("a_lam_k2", [1, 128])]
            subw = B.din("a_subln_w", [1, 256])
            lam_init = 0.8 - 0.6 * math.exp(-0.3 * l)
        else:
            w_in = B.din("b_w_in", [D, 6160])
            w_out = B.din("b_w_out", [D, D])
            fbias = B.din("b_forget_bias", [1, 16])
        Wm = dict(
            w_group=B.din("moe_w_group%d" % l, [D, 4]), b_group=B.din("moe_b_group%d" % l, [1, 4]),
            w_router=B.din("moe_w_router%d" % l, [4, D, 8]), b_router=B.din("moe_b_router%d" % l, [1, 32]),
            w_gate=B.din("moe_w_gate%d" % l, [4, 8, D, 512]), w_up=B.din("moe_w_up%d" % l, [4, 8, D, 512]),
            w_down=B.din("moe_w_down%d" % l, [4, 8, 512, D]),
            ln_g=B.din("ffn_ln_g%d" % l, [1, D]), ln_b=B.din("ffn_ln_b%d" % l, [1, D]))
        fwmod = B.din("ffn_mod_w%d" % l, [D, 6144])
        fbmod = B.din("ffn_mod_b%d" % l, [1, 6144])

        if li == 0:
            P.fence()
        B.modulation(wmod, bmod)
        B.mixer_params(kind, lam_init, lamv, subw, fbias, w_in)
        hT = B.BIGA[:, 0:16 * max(T, TP)].rearrange("p (k t) -> p k t", k=16)
        for (x_ap, ntok, tok0, with_q) in ((cur_p, TP, 0, False), (cur_o, T, TP, True)):
            P.fence()
            B.make_hT(x_ap, ntok, hT[:, :, 0:ntok], "hT")
            P.fence()
            B.qkv(l, kind, w_in, hT[:, :, 0:ntok], "hT", ntok, tok0, with_q, sc)
        P.fence()
        B.attention(l, kind, sc)
        P.fence()
        B.out_proj(l, w_out, cur_o, lng, lnb, sc, x1)
        P.fence()
        B.modulation(fwmod, fbmod)
        B.moe(l, Wm, x1, out if last else x2, "out" if last else "x2")
        if not last:
            P.fence()
            groups = [[2 * i, 2 * i + 1] for i in range(ncores // 2)]
            for ch in range(nch):
                if os.environ.get("K_NOCC"):
                    B.dma("sync", gath[ch, 0:CH, :], x2[ch * CH:(ch + 1) * CH, :], writes=[("gath", ch)])
                    continue
                P.op("gpsimd", lambda e, ch=ch: e.collective_compute(
                    "AllGather", ALU.bypass, replica_groups=groups,
                    ins=[x2[ch * CH:(ch + 1) * CH, :]], outs=[gath[ch]]),
                    writes=[("gath", ch)], dma=True, cc=True)
            cur_o = x2
            cur_p = (lambda t: gath[t // 2, (t % 2) * 128:(t % 2 + 1) * 128, :])
    P.fence()
    P.op("sync", None, reads=[])
    P.emit()
    return nc, B


def _consts(T, TP, p):
    TC = T + TP
    ident = np.eye(128, dtype=np.float32)
    tri = np.triu(np.ones((128, 128), np.float32))
    R = np.zeros((128, 128), np.float32)
    for i in range(64):
        R[i, i + 64] = -1.0
        R[i + 64, i] = 1.0
    rotT = np.ascontiguousarray(R.T)
    pos = np.concatenate([np.arange(TP), p * T + np.arange(T)]).astype(np.float32)
    inv = np.power(np.float32(10000.0), -np.arange(0, 128, 2, dtype=np.float32) / 128).astype(np.float32)
    ang = pos[None, :] * np.concatenate([inv, inv])[:, None]
    return dict(c_ident=ident, c_tri=tri, c_rotT=rotT,
                c_pbias=np.full((128, 1), 0.0 if p == 1 else NEG, np.float32),
                c_cos=np.cos(ang).astype(np.float32), c_sin=np.sin(ang).astype(np.float32))


_PROG_CACHE = {}
_RUN_KW = {}
_LAST = {}


def run_layers(layers, x_in, inp, T, TP, ncores):
    key = (tuple(layers), T, TP, ncores)
    if key not in _PROG_CACHE:
        _PROG_CACHE[key] = build_program(T, TP, list(layers), ncores)
    nc, B = _PROG_CACHE[key]
    f = np.float32
    shared = {}
    for l in layers:
        j = l // 2
        shared["mix_mod_w%d" % l] = inp["mix_mod_w"][l]
        shared["mix_mod_b%d" % l] = inp["mix_mod_b"][l].reshape(1, -1)
        shared["mix_ln_g%d" % l] = inp["mix_ln_g"][l].reshape(1, -1)
        shared["mix_ln_b%d" % l] = inp["mix_ln_b"][l].reshape(1, -1)
        if l % 2 == 0:
            shared["a_w_in"] = inp["a_w_in"][j]
            shared["a_w_out"] = inp["a_w_out"][j]
            shared["a_lam_q1"] = inp["a_lam_q1"][j].reshape(1, -1)
            shared["a_lam_k1"] = inp["a_lam_k1"][j].reshape(1, -1)
            shared["a_lam_q2"] = inp["a_lam_q2"][j].reshape(1, -1)
            shared["a_lam_k2"] = inp["a_lam_k2"][j].reshape(1, -1)
            shared["a_subln_w"] = inp["a_subln_w"][j].reshape(1, -1)
        else:
            shared["b_w_in"] = inp["b_w_in"][j]
            shared["b_w_out"] = inp["b_w_out"][j]
            shared["b_forget_bias"] = inp["b_forget_bias"][j].reshape(1, -1)
        shared["ffn_mod_w%d" % l] = inp["ffn_mod_w"][l]
        shared["ffn_mod_b%d" % l] = inp["ffn_mod_b"][l].reshape(1, -1)
        shared["ffn_ln_g%d" % l] = inp["ffn_ln_g"][l].reshape(1, -1)
        shared["ffn_ln_b%d" % l] = inp["ffn_ln_b"][l].reshape(1, -1)
        shared["moe_w_group%d" % l] = inp["moe_w_group"][l]
        shared["moe_b_group%d" % l] = inp["moe_b_group"][l].reshape(1, -1)
        shared["moe_w_router%d" % l] = inp["moe_w_router"][l]
        shared["moe_b_router%d" % l] = inp["moe_b_router"][l].reshape(1, -1)
        shared["moe_w_gate%d" % l] = inp["moe_w_gate"][l]
        shared["moe_w_up%d" % l] = inp["moe_w_up"][l]
        shared["moe_w_down%d" % l] = inp["moe_w_down"][l]
    shared = {k: np.ascontiguousarray(np.asarray(v, dtype=f)) for k, v in shared.items() if k in B.inputs}
    in_maps = []
    for c in range(ncores):
        b, p = c // 2, c % 2
        m = _consts(T, TP, p)
        m["xo"] = x_in[b, p * T:(p + 1) * T]
        m["xp"] = x_in[b, 0:TP]
        m["cvec_in"] = inp["c"][b].reshape(16, 128).T
        m = {k: np.ascontiguousarray(np.asarray(v, dtype=f)) for k, v in m.items() if k in B.inputs}
        m.update(shared)
        in_maps.append(m)
    res = run_bass_kernel_spmd(nc, in_maps, core_ids=list(range(ncores)), **_RUN_KW)
    _LAST['res'] = res
    out = np.empty_like(x_in)
    for c in range(ncores):
        b, p = c // 2, c % 2
        out[b, p * T:(p + 1) * T] = res.results[c]["out"]
    return out


def run_layer(l, x_in, inp, T, TP, ncores, stop=99):
    return run_layers([l], x_in, inp, T, TP, ncores)


def kernel(**inputs):
    inp = {k: np.asarray(v) for k, v in inputs.items()}
    x = np.asarray(inp["x"], dtype=np.float32)
    Bn, S, _ = x.shape
    T = S // 2
    ncores = Bn * 2
    return run_layers(list(range(DEPTH)), x, inp, T, T, ncores)
```

```python
import math
import os
import numpy as np
import concourse.bass as bass
import concourse.mybir as mybir
from concourse.bass_utils import run_bass_kernel_spmd

F32 = mybir.dt.float32
BF16 = mybir.dt.bfloat16
AF = mybir.ActivationFunctionType
ALU = mybir.AluOpType
AX = mybir.AxisListType

D = 2048
KC = 16
DEPTH = 2
ALPHA = (2.0 * DEPTH) ** 0.25
LN_EPS = 1e-5
RMS_EPS = 1e-5
NEG = -30000.0
COMPUTE = ("tensor", "vector", "scalar", "gpsimd")
NDMASEM = 8


class Buf:
    _n = 0

    def __init__(self, h, name, psum=False):
        self.h = h
        self.psum = psum
        Buf._n += 1
        self.key = name + "#" + str(Buf._n)

    def __getitem__(self, idx):
        return self.h[idx]


def _k(x):
    return x.key if isinstance(x, Buf) else x


class _View:
    def __init__(self, ap, key):
        self.ap = ap
        self.key = key

    def __getitem__(self, idx):
        return self.ap


class Prog:
    def __init__(self, nc):
        self.nc = nc
        self.ops = []
        self.last_w = {}
        self.readers = {}
        self.last_eng = {}
        self.last_dma = {}
        self.cc_ops = []

    def sbuf(self, name, shape, dtype):
        return Buf(self.nc.alloc_sbuf_tensor(name, list(shape), dtype), name)

    def psum(self, name, shape, dtype=F32):
        return Buf(self.nc.alloc_psum_tensor(name, list(shape), dtype), name, psum=True)

    def op(self, eng, fn, reads=(), writes=(), dma=False, cc=False):
        i = len(self.ops)
        deps = set()
        pr = [r for r in reads if isinstance(r, Buf) and r.psum]
        if pr:
            reads = [r for r in reads if not (isinstance(r, Buf) and r.psum)]
            writes = list(writes) + [r for r in pr if r not in writes]
        for r in reads:
            k = _k(r)
            if k in self.last_w:
                deps.add(self.last_w[k])
        for w in writes:
            k = _k(w)
            if k in self.last_w:
                deps.add(self.last_w[k])
            for rd in self.readers.get(k, ()):
                deps.add(rd)
        for r in reads:
            self.readers.setdefault(_k(r), []).append(i)
        for w in writes:
            k = _k(w)
            self.last_w[k] = i
            self.readers[k] = []
        deps.discard(i)
        self.ops.append(dict(eng=eng, fn=fn, deps=deps, dma=dma, signal=False, cc=cc))
        if cc:
            self.cc_ops.append(i)
        elif dma:
            self.last_dma.setdefault(eng, []).append(i)
            self.last_dma[eng] = self.last_dma[eng][-NDMASEM:]
        else:
            self.last_eng[eng] = i
        return i

    def fence(self):
        deps = set(self.last_eng.values())
        for v in self.last_dma.values():
            deps.update(v)
        deps.update(self.cc_ops[-1:])
        for e in COMPUTE + ("sync",):
            self.ops.append(dict(eng=e, fn=None, deps=set(deps), dma=False, signal=False, cc=False))
        self.last_w = {}
        self.readers = {}

    def emit(self):
        nc = self.nc
        ops = self.ops

        def dom(o):
            if o["cc"]:
                return "cc"
            return ("dma", o["eng"]) if o["dma"] else o["eng"]

        ccsem = nc.alloc_semaphore(name="s_cc") if self.cc_ops else None
        ncc = 0
        for o in ops:
            if o["cc"]:
                ncc += 1
            o["ncc_before"] = ncc
        for o in ops:
            for d in o["deps"]:
                od = ops[d]
                if od["cc"]:
                    continue
                if (not od["dma"]) and (not o["dma"]) and od["eng"] == o["eng"] == "tensor":
                    continue
                od["signal"] = True
        cnt = {}
        dma_idx = {}
        for o in ops:
            dm = dom(o)
            if o["cc"]:
                continue
            if o["dma"]:
                n = dma_idx.get(dm, 0)
                o["dslot"] = n % NDMASEM
                o["dround"] = n // NDMASEM
                dma_idx[dm] = n + 1
            elif o["signal"] and o["fn"] is not None:
                cnt[dm] = cnt.get(dm, 0) + 1
                o["cnt"] = cnt[dm]
        sems = {e: nc.alloc_semaphore(name="s_" + e) for e in COMPUTE}
        dsems = {dm: [nc.alloc_semaphore(name="d_%s_%d" % (dm[1], k)) for k in range(NDMASEM)]
                 for dm in dma_idx}
        streams = {}
        for i, o in enumerate(ops):
            streams.setdefault(o["eng"], []).append(i)

        def run_stream(eng_name, engine):
            waited = {}

            def wait(sem, key, val):
                if waited.get(key, -1) >= val:
                    return
                engine.wait_ge(sem, val)
                waited[key] = val

            for i in streams.get(eng_name, []):
                o = ops[i]
                for d in sorted(o["deps"]):
                    od = ops[d]
                    if od["cc"]:
                        wait(ccsem, "cc", o["ncc_before"] - (1 if o["cc"] else 0))
                        continue
                    if od["dma"]:
                        dm = dom(od)
                        wait(dsems[dm][od["dslot"]], (dm, od["dslot"]), 16 * (od["dround"] + 1))
                    else:
                        if "cnt" not in od:
                            continue
                        if od["eng"] == eng_name == "tensor" and not o["dma"]:
                            continue
                        wait(sems[od["eng"]], od["eng"], od["cnt"])
                if o["dma"] and not o["cc"] and o["dround"] > 0:
                    dm = dom(o)
                    wait(dsems[dm][o["dslot"]], (dm, o["dslot"]), 16 * o["dround"])
                if o["fn"] is None:
                    continue
                ins = o["fn"](engine)
                if o["cc"]:
                    ins.then_inc(ccsem, 1)
                elif o["dma"]:
                    ins.then_inc(dsems[dom(o)][o["dslot"]], 16)
                elif "cnt" in o:
                    ins.then_inc(sems[eng_name], 1)

        with nc.Block() as block:
            @block.tensor
            def _(e):
                run_stream("tensor", e)

            @block.vector
            def _(e):
                run_stream("vector", e)

            @block.scalar
            def _(e):
                run_stream("scalar", e)

            @block.gpsimd
            def _(e):
                run_stream("gpsimd", e)

            @block.sync
            def _(e):
                run_stream("sync", e)


class Builder:
    def __init__(self, T, TP, layers, n_exp_groups=4):
        self.T, self.TP, self.TC = T, TP, T + TP
        self.layers = layers
        nc = self.nc = bass.Bass("TRN2", target_bir_lowering=False)
        P = self.P = Prog(nc)
        self.inputs = {}
        self.BIGA = P.sbuf("BIGA", [128, 32768], BF16)
        self.BIGB = P.sbuf("BIGB", [128, 16384], BF16)
        self.W = [P.sbuf("W%d" % i, [128, 8192], BF16) for i in range(5)]
        self.BC = [P.sbuf("BC%d" % i, [128, 2048], F32) for i in range(3)]
        self.ident = P.sbuf("ident", [128, 128], F32)
        self.identb = P.sbuf("identb", [128, 128], BF16)
        self.tri = P.sbuf("tri", [128, 128], F32)
        self.trib = P.sbuf("trib", [128, 128], BF16)
        self.rotT = P.sbuf("rotT", [128, 128], BF16)
        self.ones = P.sbuf("ones", [128, 128], F32)
        self.pbias = P.sbuf("pbias", [128, 1], F32)
        self.zero = P.sbuf("zero", [128, 1], F32)
        self.cvec = P.sbuf("cvec", [128, 16], F32)
        self.rowbuf = _View(self.BIGA.h.bitcast(F32)[0:1, 0:6144], self.BIGA.key)
        self.small = P.sbuf("small", [128, 64], F32)
        self.lam = P.sbuf("lam", [128, 8], F32)
        self.subw = P.sbuf("subw", [128, 256], F32)
        self.w32 = P.sbuf("w32", [128, 8, 32], F32)
        self.rt = P.sbuf("rt", [128, 128], F32)
        self.wr = P.sbuf("wr", [128, 16, 36], F32)
        self.brt = P.sbuf("brt", [128, 36], F32)
        bf = self.BIGB.h.bitcast(F32)
        self.lf = bf[:, 0:512].rearrange("p (t h) -> p t h", h=16)
        self.cum = bf[:, 512:1024].rearrange("p (t h) -> p t h", h=16)
        self.anc = bf[:, 1024:1088].rearrange("p (t h) -> p t h", h=16)
        self.fb = P.sbuf("fb", [128, 16], F32)
        self.wf = _View(self.BIGB[:, 4096:4352].rearrange("p (k n) -> p k n", k=16), "wfkey")
        self.PS = [P.psum("ps%d" % i, [128, 512], F32) for i in range(8)]

    def din(self, name, shape, dt=F32):
        t = self.nc.dram_tensor(name, list(shape), dt, kind="ExternalInput").ap()
        self.inputs[name] = t
        return t

    def dscr(self, name, shape, dt):
        return self.nc.dram_tensor(name, list(shape), dt).ap()

    def dma(self, eng, out, in_, reads=(), writes=()):
        self.P.op(eng, lambda e: e.dma_start(out=out, in_=in_), reads=reads, writes=writes, dma=True)

    def f32view(self, buf, off_f32, n):
        return buf.h.bitcast(F32)[:, off_f32:off_f32 + n]

    def load_consts(self):
        P = self.P
        c_ident = self.din("c_ident", [128, 128])
        c_tri = self.din("c_tri", [128, 128])
        c_rotT = self.din("c_rotT", [128, 128])
        c_pbias = self.din("c_pbias", [128, 1])
        self.dma("sync", self.ident[:], c_ident, writes=[self.ident])
        self.dma("sync", self.tri[:], c_tri, writes=[self.tri])
        self.dma("sync", self.pbias[:], c_pbias, writes=[self.pbias])
        self.dma("gpsimd", self.identb[:], c_ident, writes=[self.identb])
        self.dma("gpsimd", self.trib[:], c_tri, writes=[self.trib])
        self.dma("gpsimd", self.rotT[:], c_rotT, writes=[self.rotT])
        P.op("vector", lambda e: e.memset(self.ones[:], 1.0), writes=[self.ones])
        P.op("vector", lambda e: e.memset(self.zero[:], 0.0), writes=[self.zero])
        cv = self.din("cvec_in", [128, 16])
        self.dma("sync", self.cvec[:], cv, writes=[self.cvec])
        P.op("scalar", lambda e: e.activation(out=self.cvec[:], in_=self.cvec[:], func=AF.Silu),
             reads=[self.cvec], writes=[self.cvec])

    def modulation(self, wmod, bmod):
        P = self.P
        wst = [self.f32view(self.W[0], 0, 4096), self.f32view(self.W[1], 0, 4096),
               self.f32view(self.W[2], 0, 4096), self.f32view(self.W[3], 0, 4096)]
        wk = [self.W[0], self.W[1], self.W[2], self.W[3]]
        ps = self.PS[0]
        self.dma("sync", self.rowbuf.ap, bmod, writes=[self.rowbuf])
        for n in range(12):
            a, b = (0, 1) if n % 2 == 0 else (2, 3)
            src = wmod[:, n * 512:(n + 1) * 512].rearrange("(k p) n -> p k n", p=128)
            self.dma("sync", wst[a].rearrange("p (k n) -> p k n", k=8), src[:, 0:8, :], writes=[wk[a]])
            self.dma("sync", wst[b].rearrange("p (k n) -> p k n", k=8), src[:, 8:16, :], writes=[wk[b]])
            for k in range(16):
                wb = a if k < 8 else b
                kk = k % 8
                P.op("tensor", lambda e, wb=wb, kk=kk, k=k: e.matmul(
                    out=ps[0:1, :], lhsT=self.cvec[:, k:k + 1], rhs=wst[wb][:, kk * 512:(kk + 1) * 512],
                    start=(k == 0), stop=(k == 15)), reads=[self.cvec, wk[wb]], writes=[ps])
            addc = 0.0 if n < 4 else 1.0
            P.op("vector", lambda e, n=n, addc=addc: e.scalar_tensor_tensor(
                out=self.rowbuf.ap[0:1, n * 512:(n + 1) * 512], in0=ps[0:1, :], scalar=addc,
                in1=self.rowbuf.ap[0:1, n * 512:(n + 1) * 512], op0=ALU.add, op1=ALU.add),
                reads=[ps, self.rowbuf], writes=[self.rowbuf])
        self.bcast_mod()

    def bcast_mod(self):
        P = self.P
        for n in range(12):
            dst = [self.BC[1], self.BC[0], self.BC[2]][n // 4]
            psb = self.PS[1 + n % 2]
            P.op("tensor", lambda e, n=n, psb=psb: e.matmul(
                out=psb[:], lhsT=self.ones[0:1, :], rhs=self.rowbuf.ap[0:1, n * 512:(n + 1) * 512],
                start=True, stop=True), reads=[self.ones, self.rowbuf], writes=[psb])
            P.op("scalar", lambda e, n=n, psb=psb, dst=dst: e.activation(
                out=dst[:, (n % 4) * 512:(n % 4 + 1) * 512], in_=psb[:], func=AF.Copy),
                reads=[psb], writes=[dst])

    def make_hT(self, x_ap, ntok, hT, hT_key, router=None):
        P = self.P
        xt = [self.f32view(self.W[0], 0, 2048), self.f32view(self.W[1], 0, 2048)]
        xk = [self.W[0], self.W[1]]
        htf = self.f32view(self.W[2], 0, 2048)
        for t in range(ntok // 128):
            xb, xkk = xt[t % 2], xk[t % 2]
            self.dma("sync", xb, (x_ap(t) if callable(x_ap) else x_ap[t * 128:(t + 1) * 128, :]), writes=[xkk])
            if router is not None and "acc" in router:
                acc = router["acc"](t)
                P.op("scalar", lambda e, xb=xb, acc=acc: e.activation(out=acc, in_=xb, func=AF.Copy, scale=ALPHA),
                     reads=[xkk], writes=[(router["acckey"], t)])
            P.op("vector", lambda e, xb=xb: e.tensor_tensor(out=xb, in0=xb, in1=self.BC[0][:], op=ALU.mult),
                 reads=[xkk, self.BC[0]], writes=[xkk])
            P.op("vector", lambda e, xb=xb: e.tensor_tensor(out=xb, in0=xb, in1=self.BC[1][:], op=ALU.add),
                 reads=[xkk, self.BC[1]], writes=[xkk])
            for g in range(4):
                ps = self.PS[g % 4]
                for j in range(4):
                    k = g * 4 + j
                    P.op("tensor", lambda e, xb=xb, k=k, j=j, ps=ps: e.transpose(
                        out=ps[:, j * 128:(j + 1) * 128], in_=xb[:, k * 128:(k + 1) * 128], identity=self.ident[:]),
                        reads=[xkk, self.ident], writes=[ps])
                eng = "vector" if g % 2 == 0 else "scalar"
                dst = hT[:, g * 4:(g + 1) * 4, t * 128:(t + 1) * 128]
                src = ps[:].rearrange("p (a b) -> p a b", a=4)
                if eng == "vector":
                    P.op("vector", lambda e, dst=dst, src=src: e.tensor_copy(out=dst, in_=src),
                         reads=[ps], writes=[(hT_key, t)])
                else:
                    P.op("scalar", lambda e, dst=dst, src=src: e.activation(out=dst, in_=src, func=AF.Copy),
                         reads=[ps], writes=[(hT_key, t)])
                if router is not None:
                    P.op("gpsimd" if False else "vector", lambda e, src=src, g=g: e.tensor_copy(
                        out=htf[:, g * 512:(g + 1) * 512].rearrange("p (a b) -> p a b", a=4), in_=src),
                        reads=[ps], writes=[self.W[2]])
            if router is not None:
                self.route(t, htf, router)

    def route(self, t, htf, router):
        P = self.P
        ps = self.PS[4]
        for k in range(16):
            P.op("tensor", lambda e, k=k: e.matmul(out=ps[:, 0:36], lhsT=htf[:, k * 128:(k + 1) * 128],
                                                   rhs=self.wr[:, k, :], start=(k == 0), stop=(k == 15)),
                 reads=[self.W[2], self.wr], writes=[ps])
        rt = self.rt
        V = "vector"

        def v(fn, reads=(), writes=()):
            P.op(V, fn, reads=[rt] + list(reads), writes=[rt] + list(writes))
        v(lambda e: e.tensor_tensor(out=rt[:, 0:36], in0=ps[:, 0:36], in1=self.brt[:], op=ALU.add), reads=[ps, self.brt])
        v(lambda e: e.reduce_max(out=rt[:, 36:37], in_=rt[:, 0:4], axis=AX.X))
        v(lambda e: e.tensor_scalar(out=rt[:, 40:44], in0=rt[:, 0:4], scalar1=rt[:, 36:37], scalar2=None, op0=ALU.is_ge))
        v(lambda e: e.tensor_scalar(out=rt[:, 44:48], in0=rt[:, 0:4], scalar1=rt[:, 36:37], scalar2=None, op0=ALU.subtract))
        P.op("scalar", lambda e: e.activation(out=rt[:, 44:48], in_=rt[:, 44:48], func=AF.Exp, accum_out=rt[:, 37:38]),
             reads=[rt], writes=[rt])
        v(lambda e: e.reciprocal(out=rt[:, 38:39], in_=rt[:, 37:38]))
        v(lambda e: e.tensor_scalar(out=rt[:, 48:56], in0=rt[:, 4:12], scalar1=rt[:, 40:41], scalar2=None, op0=ALU.mult))
        for g in range(1, 4):
            v(lambda e, g=g: e.scalar_tensor_tensor(out=rt[:, 48:56], in0=rt[:, 4 + 8 * g:12 + 8 * g],
                                                    scalar=rt[:, 40 + g:41 + g], in1=rt[:, 48:56],
                                                    op0=ALU.mult, op1=ALU.add))
        v(lambda e: e.reduce_max(out=rt[:, 56:57], in_=rt[:, 48:56], axis=AX.X))
        v(lambda e: e.tensor_scalar(out=rt[:, 64:72], in0=rt[:, 48:56], scalar1=rt[:, 56:57], scalar2=None, op0=ALU.is_ge))
        v(lambda e: e.scalar_tensor_tensor(out=rt[:, 72:80], in0=rt[:, 64:72], scalar=NEG, in1=rt[:, 48:56],
                                           op0=ALU.mult, op1=ALU.add))
        v(lambda e: e.reduce_max(out=rt[:, 57:58], in_=rt[:, 72:80], axis=AX.X))
        v(lambda e: e.tensor_scalar(out=rt[:, 80:88], in0=rt[:, 72:80], scalar1=rt[:, 57:58], scalar2=None, op0=ALU.is_ge))
        v(lambda e: e.tensor_tensor(out=rt[:, 58:59], in0=rt[:, 57:58], in1=rt[:, 56:57], op=ALU.subtract))
        P.op("scalar", lambda e: e.activation(out=rt[:, 59:60], in_=rt[:, 58:59], func=AF.Exp), reads=[rt], writes=[rt])
        v(lambda e: e.tensor_scalar(out=rt[:, 60:61], in0=rt[:, 59:60], scalar1=1.0, scalar2=None, op0=ALU.add))
        v(lambda e: e.reciprocal(out=rt[:, 61:62], in_=rt[:, 60:61]))
        v(lambda e: e.tensor_tensor(out=rt[:, 62:63], in0=rt[:, 59:60], in1=rt[:, 61:62], op=ALU.mult))
        v(lambda e: e.tensor_scalar(out=rt[:, 64:72], in0=rt[:, 64:72], scalar1=rt[:, 61:62], scalar2=None, op0=ALU.mult))
        v(lambda e: e.scalar_tensor_tensor(out=rt[:, 64:72], in0=rt[:, 80:88], scalar=rt[:, 62:63], in1=rt[:, 64:72],
                                           op0=ALU.mult, op1=ALU.add))
        v(lambda e: e.tensor_scalar(out=rt[:, 64:72], in0=rt[:, 64:72], scalar1=rt[:, 38:39], scalar2=None, op0=ALU.mult))
        for g in range(4):
            P.op(V, lambda e, g=g: e.tensor_scalar(out=self.w32[:, t, g * 8:(g + 1) * 8], in0=rt[:, 64:72],
                                                   scalar1=rt[:, 40 + g:41 + g], scalar2=None, op0=ALU.mult),
                 reads=[rt], writes=[("w32", t)])

    def layer_norm(self, z, zkey, gt, bt, scr, scrkey):
        P = self.P
        st = self.small
        P.op("vector", lambda e: e.reduce_sum(out=st[:, 0:1], in_=z, axis=AX.X), reads=[zkey], writes=[st])
        P.op("vector", lambda e: e.tensor_scalar(out=st[:, 1:2], in0=st[:, 0:1], scalar1=-1.0 / D, scalar2=None, op0=ALU.mult),
             reads=[st], writes=[st])
        P.op("scalar", lambda e: e.activation(out=scr, in_=z, func=AF.Square, bias=st[:, 1:2], scale=1.0, accum_out=st[:, 2:3]),
             reads=[zkey, st], writes=[scrkey, st])
        P.op("vector", lambda e: e.tensor_scalar(out=st[:, 3:4], in0=st[:, 2:3], scalar1=1.0 / D, scalar2=LN_EPS, op0=ALU.mult, op1=ALU.add),
             reads=[st], writes=[st])
        P.op("scalar", lambda e: e.activation(out=st[:, 5:6], in_=st[:, 3:4], func=AF.Sqrt), reads=[st], writes=[st])
        P.op("vector", lambda e: e.reciprocal(out=st[:, 4:5], in_=st[:, 5:6]), reads=[st], writes=[st])
        P.op("vector", lambda e: e.tensor_scalar(out=z, in0=z, scalar1=st[:, 1:2], scalar2=st[:, 4:5], op0=ALU.add, op1=ALU.mult),
             reads=[zkey, st], writes=[zkey])
        P.op("vector", lambda e: e.tensor_tensor(out=z, in0=z, in1=gt[:], op=ALU.mult), reads=[zkey, gt], writes=[zkey])
        P.op("vector", lambda e: e.tensor_tensor(out=z, in0=z, in1=bt[:], op=ALU.add), reads=[zkey, bt], writes=[zkey])

    def mixer_params(self, kind, lam_init, lamv, subw, fbias, w_in):
        B = self
        P = self.P
        if kind == "A":
            lt = [B.f32view(B.W[4], i * 128, 128) for i in range(4)]
            for i in range(4):
                B.dma("sync", lt[i], lamv[i].partition_broadcast(128), writes=[B.W[4]])
            P.op("vector", lambda e: e.tensor_tensor(out=lt[0], in0=lt[0], in1=lt[1], op=ALU.mult), reads=[B.W[4]], writes=[B.W[4]])
            P.op("vector", lambda e: e.tensor_tensor(out=lt[2], in0=lt[2], in1=lt[3], op=ALU.mult), reads=[B.W[4]], writes=[B.W[4]])
            P.op("vector", lambda e: e.reduce_sum(out=B.lam[:, 2:3], in_=lt[0], axis=AX.X), reads=[B.W[4]], writes=[B.lam])
            P.op("vector", lambda e: e.reduce_sum(out=B.lam[:, 3:4], in_=lt[2], axis=AX.X), reads=[B.W[4]], writes=[B.lam])
            P.op("scalar", lambda e: e.activation(out=B.lam[:, 4:6], in_=B.lam[:, 2:4], func=AF.Exp), reads=[B.lam], writes=[B.lam])
            P.op("vector", lambda e: e.tensor_tensor(out=B.lam[:, 0:1], in0=B.lam[:, 4:5], in1=B.lam[:, 5:6], op=ALU.subtract), reads=[B.lam], writes=[B.lam])
            P.op("vector", lambda e: e.tensor_scalar(out=B.lam[:, 0:1], in0=B.lam[:, 0:1], scalar1=lam_init, scalar2=None, op0=ALU.add), reads=[B.lam], writes=[B.lam])
            P.op("vector", lambda e: e.tensor_scalar(out=B.lam[:, 1:2], in0=B.lam[:, 0:1], scalar1=-1.0, scalar2=None, op0=ALU.mult), reads=[B.lam], writes=[B.lam])
            B.dma("sync", B.subw[:], subw.partition_broadcast(128), writes=[B.subw])
            P.op("vector", lambda e: e.tensor_scalar(out=B.subw[:], in0=B.subw[:], scalar1=1.0 - lam_init, scalar2=None, op0=ALU.mult), reads=[B.subw], writes=[B.subw])
        else:
            B.dma("sync", B.fb[:], fbias.partition_broadcast(128), writes=[B.fb])
            B.dma("gpsimd", B.wf.ap, w_in[:, 6144:6160].rearrange("(k p) n -> p k n", p=128), writes=[B.wf])

    def qk_post(self, kind, mi, n, tb, tok0, isq, cs, sc):
        P = self.P
        m = (n % 4) * 4 + mi
        ps = self.PS[mi % 2]
        ob = self.W[3][:, (mi % 2) * 512:(mi % 2) * 512 + 512]
        okey = (self.W[3].key, "o", mi % 2)
        if kind == "A":
            qraw = self.W[3][:, 1024 + (mi % 2) * 512:1024 + (mi % 2) * 512 + 512]
            qkey = (self.W[3].key, "q", mi % 2)
            ps2 = self.PS[2 + mi % 2]
            t1 = self.f32view(self.W[3], 1024 + (mi % 2) * 1024, 512)
            t2 = self.f32view(self.W[3], 1024 + (mi % 2) * 1024 + 512, 512)
            tkey = (self.W[3].key, "t", mi % 2)
            P.op("scalar", lambda e: e.activation(out=qraw, in_=ps[:], func=AF.Copy), reads=[ps], writes=[qkey])
            P.op("tensor", lambda e: e.matmul(out=ps2[:], lhsT=self.rotT[:], rhs=qraw, start=True, stop=True),
                 reads=[qkey, self.rotT], writes=[ps2])
            P.op("vector", lambda e: e.tensor_tensor(out=t1, in0=ps[:], in1=cs[0], op=ALU.mult),
                 reads=[ps, (self.W[2].key, "c")], writes=[(tkey, 1)])
            P.op("vector", lambda e: e.tensor_tensor(out=t2, in0=ps2[:], in1=cs[1], op=ALU.mult),
                 reads=[ps2, (self.W[2].key, "s")], writes=[(tkey, 2)])
            P.op("vector", lambda e: e.tensor_tensor(out=ob, in0=t1, in1=t2, op=ALU.add),
                 reads=[(tkey, 1), (tkey, 2)], writes=[okey])
        else:
            P.op("scalar", lambda e: e.activation(out=ob, in_=ps[:], func=AF.Copy), reads=[ps], writes=[okey])
        if isq:
            dst = sc["qT"][m, :, tb * 512:(tb + 1) * 512]
            dk = ("qT", m)
        else:
            dst = sc["kT"][m, :, tok0 + tb * 512:tok0 + (tb + 1) * 512]
            dk = ("kT", m)
        self.dma("sync", dst, ob, reads=[okey], writes=[dk])

    def qkv(self, l, kind, w_in, hT, hT_key, ntok, tok0, with_q, sc):
        P = self.P
        wb = [self.W[0], self.W[1]]
        nblk = 12
        first = 0 if with_q else 4
        bi = 0
        import os
        skip = os.environ.get("QKV_SKIP", "")
        for n in range(first, nblk):
            if (skip == "v" and n >= 8) or (skip == "qk" and n < 8):
                continue
            wt = wb[bi % 2]
            bi += 1
            wv = wt[:].rearrange("p (k n) -> p k n", k=16)
            self.dma("gpsimd", wv, w_in[:, n * 512:(n + 1) * 512].rearrange("(k p) n -> p k n", p=128), writes=[wt])
            if n < 8:
                isq = n < 4
                for tb in range(ntok // 512):
                    if kind == "A":
                        cs = [self.f32view(self.W[2], 0, 512), self.f32view(self.W[2], 512, 512)]
                        t0 = tok0 + tb * 512
                        self.dma("sync", cs[0], sc["cos"][:, t0:t0 + 512], writes=[(self.W[2].key, "c")])
                        self.dma("sync", cs[1], sc["sin"][:, t0:t0 + 512], writes=[(self.W[2].key, "s")])
                    pend = None
                    for mi in range(5):
                        if mi < 4:
                            ps = self.PS[mi % 2]
                            for k in range(16):
                                P.op("tensor", lambda e, k=k, mi=mi, tb=tb, ps=ps, wv=wv: e.matmul(
                                    out=ps[:], lhsT=wv[:, k, mi * 128:(mi + 1) * 128], rhs=hT[:, k, tb * 512:(tb + 1) * 512],
                                    start=(k == 0), stop=(k == 15)), reads=[wt] + [(hT_key, tb * 4 + i) for i in range(4)], writes=[ps])
                        if pend is not None:
                            self.qk_post(kind, pend, n, tb, tok0, isq, cs if kind == "A" else None, sc)
                        pend = mi if mi < 4 else None
            else:
                hv = 256 if kind == "A" else 128
                nh = 512 // hv
                for tt in range(ntok // 128):
                    ps = self.PS[4 + tt % 2]
                    for k in range(16):
                        P.op("tensor", lambda e, k=k, tt=tt, ps=ps, wv=wv: e.matmul(
                            out=ps[:], lhsT=hT[:, k, tt * 128:(tt + 1) * 128], rhs=wv[:, k, :],
                            start=(k == 0), stop=(k == 15)), reads=[wt, (hT_key, tt)], writes=[ps])
                    vb = self.W[2][:, 2048 + (tt % 2) * 1024:2048 + (tt % 2) * 1024 + nh * (hv + 1)].rearrange("p (h c) -> p h c", h=nh)
                    vkey = (self.W[2].key, "v", tt % 2)
                    P.op("scalar" if tt % 2 == 0 else "vector",
                         (lambda e, vb=vb, ps=ps: e.activation(out=vb[:, :, 0:hv], in_=ps[:].rearrange("p (h c) -> p h c", h=nh), func=AF.Copy))
                         if tt % 2 == 0 else
                         (lambda e, vb=vb, ps=ps: e.tensor_copy(out=vb[:, :, 0:hv], in_=ps[:].rearrange("p (h c) -> p h c", h=nh))),
                         reads=[ps], writes=[vkey])
                    P.op("gpsimd", lambda e, vb=vb: e.memset(vb[:, :, hv:hv + 1], 1.0), writes=[vkey])
                    h0 = (n - 8) * nh
                    ct = (tok0 // 128) + tt
                    self.dma("sync", sc["v"][h0:h0 + nh, ct, :, :].rearrange("h p c -> p h c"), vb, reads=[vkey], writes=[("v", h0)])
        if kind == "B":
            P.op("sync", None, reads=[self.wf])
            for tt in range(ntok // 128):
                ps = self.PS[6]
                ct = (tok0 // 128) + tt
                for k in range(16):
                    P.op("tensor", lambda e, k=k, tt=tt: e.matmul(out=ps[:, 0:16], lhsT=hT[:, k, tt * 128:(tt + 1) * 128],
                                                                  rhs=self.wf.ap[:, k, :], start=(k == 0), stop=(k == 15)),
                         reads=[self.wf, (hT_key, tt)], writes=[ps])
                st = self.small
                P.op("vector", lambda e: e.tensor_tensor(out=st[:, 16:32], in0=ps[:, 0:16], in1=self.fb[:], op=ALU.add),
                     reads=[ps, self.fb], writes=[st])
                P.op("scalar", lambda e: e.activation(out=st[:, 32:48], in_=st[:, 16:32], func=AF.Exp, scale=-1.0), reads=[st], writes=[st])
                P.op("scalar", lambda e: e.activation(out=st[:, 32:48], in_=st[:, 32:48], func=AF.Ln, bias=1.0, scale=1.0), reads=[st], writes=[st])
                P.op("vector", lambda e, ct=ct: e.tensor_scalar(out=self.lf[:, ct, :], in0=st[:, 32:48], scalar1=-1.0, scalar2=None, op0=ALU.mult),
                     reads=[st], writes=[("lf", ct)])

    def attention(self, l, kind, sc):
        P = self.P
        T, TP, TC = self.T, self.TP, self.TC
        nprev = TP // 128
        nq = T // 512
        scale = 128 ** -0.5
        if kind == "A":
            nheads, nmaps, hv = 8, 2, 256
        else:
            nheads, nmaps, hv = 16, 1, 128
        hv1 = hv + 1
        kqsize = nmaps * (TC + T)
        vsize = (TC // 128) * hv1
        sets = []
        for par in range(2):
            if kind == "A":
                kqb, kq0 = (self.BIGA, 0) if par == 0 else (self.BIGB, 0)
                vb, v0 = self.BIGA, kqsize + par * vsize
                assert kqsize <= 16384 and kqsize + 2 * vsize <= 32768
            else:
                kqb, kq0 = self.BIGA, par * (kqsize + vsize)
                vb, v0 = self.BIGA, par * (kqsize + vsize) + kqsize
                assert 2 * (kqsize + vsize) <= 32768
            kT_ = [kqb[:, kq0 + m * TC:kq0 + (m + 1) * TC] for m in range(nmaps)]
            qT_ = [kqb[:, kq0 + nmaps * TC + m * T:kq0 + nmaps * TC + (m + 1) * T] for m in range(nmaps)]
            v_ = vb[:, v0:v0 + vsize].rearrange("p (t c) -> p t c", c=hv1)
            sets.append((kT_, qT_, v_))

        def load_head(h):
            par = h % 2
            kT_, qT_, v_ = sets[par]
            for m in range(nmaps):
                mm = h * nmaps + m
                self.dma("sync", kT_[m], sc["kT"][mm, :, :], reads=[("kT", mm)], writes=[("kTs", m, par)])
                self.dma("sync", qT_[m], sc["qT"][mm, :, :], reads=[("qT", mm)], writes=[("qTs", m, par)])
            self.dma("sync", v_, sc["v"][h, :, :, :].rearrange("t p c -> p t c"),
                     reads=[("v", (h // (512 // hv)) * (512 // hv))], writes=[("v1", par)])
        if kind == "B":
            ps = self.PS[7]
            for t in range(TC // 128):
                for t2 in range(t + 1):
                    P.op("tensor", lambda e, t=t, t2=t2: e.matmul(
                        out=ps[:, 0:16], lhsT=(self.tri[:] if t2 == t else self.ones[:]), rhs=self.lf[:, t2, :],
                        start=(t2 == 0), stop=(t2 == t)), reads=[("lf", t2), self.tri, self.ones], writes=[ps])
                P.op("vector", lambda e, t=t: e.tensor_copy(out=self.cum[:, t, :], in_=ps[:, 0:16]), reads=[ps], writes=[("cum", t)])
            for g in range(nq):
                ta = (TP + g * 512 + 256) // 128
                for t2 in range(ta):
                    P.op("tensor", lambda e, t2=t2, ta=ta: e.matmul(out=ps[:, 0:16], lhsT=self.ones[:], rhs=self.lf[:, t2, :],
                                                                    start=(t2 == 0), stop=(t2 == ta - 1)),
                         reads=[("lf", t2), self.ones], writes=[ps])
                P.op("vector", lambda e, g=g: e.tensor_copy(out=self.anc[:, g, :], in_=ps[:, 0:16]), reads=[ps], writes=[("anc", g)])
        ebuf = [self.W[0], self.W[1], self.W[4]]
        for h in range(nheads):
            if h == 0:
                load_head(0)
            if h + 1 < nheads:
                load_head(h + 1)
            par = h % 2
            kTs, qTs, v1 = sets[par]
            for g in range(nq):
                nkb = nprev + 4 * g + 4
                o1n = self.f32view(self.W[2], 0, 1024).rearrange("p (i c) -> p i c", i=4)
                for m in range(nmaps):
                    def emit_S(j, m=m, g=g):
                        pss = self.PS[4 + j % 3]
                        P.op("tensor", lambda e, j=j, g=g, pss=pss, kT=kTs[m], qT=qTs[m]: e.matmul(
                            out=pss[:], lhsT=kT[:, j * 128:(j + 1) * 128], rhs=qT[:, g * 512:(g + 1) * 512],
                            start=True, stop=True), reads=[("kTs", m, par), ("qTs", m, par)], writes=[pss])

                    emit_S(0)
                    emit_S(1)
                    for j in range(nkb):
                        if j + 2 < nkb:
                            emit_S(j + 2)
                        pss = self.PS[4 + j % 3]
                        et = ebuf[j % 3]
                        ev = et[:, 0:512]
                        if kind == "A":
                            bias = self.pbias[:, 0:1] if j < nprev else self.zero[:, 0:1]
                            breads = [self.pbias, self.zero]
                        else:
                            bcol = self.small[:, 48 + (j % 3):49 + (j % 3)]
                            P.op("vector", lambda e, bcol=bcol, g=g, j=j, h=h: e.tensor_tensor(
                                out=bcol, in0=self.anc[:, g, h:h + 1], in1=self.cum[:, j, h:h + 1], op=ALU.subtract),
                                reads=[("anc", g), ("cum", j)], writes=[("bcol", j % 3)])
                            if j < nprev:
                                P.op("vector", lambda e, bcol=bcol: e.tensor_tensor(out=bcol, in0=bcol, in1=self.pbias[:, 0:1], op=ALU.add),
                                     reads=[("bcol", j % 3), self.pbias], writes=[("bcol", j % 3)])
                            bias = bcol
                            breads = [("bcol", j % 3)]
                        P.op("scalar", lambda e, ev=ev, pss=pss, bias=bias: e.activation(out=ev, in_=pss[:], func=AF.Exp, bias=bias, scale=scale),
                             reads=[pss] + breads, writes=[et])
                        jo = j - nprev
                        for i in range(4):
                            qi = 4 * g + i
                            if jo > qi:
                                continue
                            if jo == qi:
                                P.op("gpsimd", lambda e, ev=ev, i=i: e.tensor_tensor(
                                    out=ev[:, i * 128:(i + 1) * 128], in0=ev[:, i * 128:(i + 1) * 128], in1=self.trib[:], op=ALU.mult),
                                    reads=[et, self.trib], writes=[et])
                            pso = self.PS[i]
                            last = nprev + qi
                            P.op("tensor", lambda e, ev=ev, i=i, j=j, pso=pso, last=last, vv=v1: e.matmul(
                                out=pso[:, 0:hv1], lhsT=ev[:, i * 128:(i + 1) * 128], rhs=vv[:, j, :],
                                start=(j == 0), stop=(j == last)), reads=[et, ("v1", par)], writes=[pso])
                    st = self.small
                    for i in range(4):
                        pso = self.PS[i]
                        qi = 4 * g + i
                        P.op("vector", lambda e, pso=pso, i=i: e.reciprocal(out=st[:, 8 + i:9 + i], in_=pso[:, hv:hv1]), reads=[pso], writes=[st])
                        if kind == "A" and m == 0:
                            P.op("vector", lambda e, pso=pso, i=i: e.tensor_scalar(
                                out=o1n[:, i, :], in0=pso[:, 0:hv], scalar1=st[:, 8 + i:9 + i], scalar2=None, op0=ALU.mult),
                                reads=[pso, st], writes=[(self.W[2].key, "o1n", i)])
                        elif kind == "A":
                            ot = self.f32view(self.W[3], i * 256, 256)
                            okey = (self.W[3].key, "ot", i)
                            P.op("vector", lambda e, i=i: e.tensor_tensor(out=st[:, 12 + i:13 + i], in0=st[:, 8 + i:9 + i], in1=self.lam[:, 1:2], op=ALU.mult),
                                 reads=[st, self.lam], writes=[st])
                            P.op("vector", lambda e, pso=pso, i=i, ot=ot: e.scalar_tensor_tensor(
                                out=ot, in0=pso[:, 0:hv], scalar=st[:, 12 + i:13 + i], in1=o1n[:, i, :], op0=ALU.mult, op1=ALU.add),
                                reads=[pso, st, (self.W[2].key, "o1n", i)], writes=[okey])
                            sq = self.f32view(self.W[3], 1024 + i * 256, 256)
                            P.op("scalar", lambda e, ot=ot, sq=sq, i=i: e.activation(out=sq, in_=ot, func=AF.Square, accum_out=st[:, 16 + i:17 + i]),
                                 reads=[okey], writes=[(self.W[3].key, "sq", i), st])
                            P.op("vector", lambda e, i=i: e.tensor_scalar(out=st[:, 20 + i:21 + i], in0=st[:, 16 + i:17 + i], scalar1=1.0 / 256,
                                                                         scalar2=RMS_EPS, op0=ALU.mult, op1=ALU.add), reads=[st], writes=[st])
                            P.op("scalar", lambda e, i=i: e.activation(out=st[:, 24 + i:25 + i], in_=st[:, 20 + i:21 + i], func=AF.Sqrt), reads=[st], writes=[st])
                            P.op("vector", lambda e, i=i: e.reciprocal(out=st[:, 28 + i:29 + i], in_=st[:, 24 + i:25 + i]), reads=[st], writes=[st])
                            ob = self.W[3][:, 4096 + i * 256:4096 + (i + 1) * 256]
                            obk = (self.W[3].key, "ob", i)
                            P.op("vector", lambda e, ot=ot, ob=ob, i=i: e.scalar_tensor_tensor(
                                out=ob, in0=ot, scalar=st[:, 28 + i:29 + i], in1=self.subw[:], op0=ALU.mult, op1=ALU.mult),
                                reads=[okey, st, self.subw], writes=[obk])
                            self.dma("sync", sc["o"][qi * 128:(qi + 1) * 128, h * 256:(h + 1) * 256], ob, reads=[obk], writes=[("o", qi)])
                        else:
                            ob = self.W[3][:, 4096 + i * 128:4096 + (i + 1) * 128]
                            obk = (self.W[3].key, "ob", i)
                            P.op("vector", lambda e, pso=pso, ob=ob, i=i: e.tensor_scalar(
                                out=ob, in0=pso[:, 0:hv], scalar1=st[:, 8 + i:9 + i], scalar2=None, op0=ALU.mult),
                                reads=[pso, st], writes=[obk])
                            self.dma("sync", sc["o"][qi * 128:(qi + 1) * 128, h * 128:(h + 1) * 128], ob, reads=[obk], writes=[("o", qi)])

    def out_proj(self, l, w_out, x_ap, lng, lnb, sc, x1_ap):
        P = self.P
        T = self.T
        wo = [self.W[i] for i in range(4)]
        for n in range(4):
            self.dma("gpsimd", wo[n][:].rearrange("p (k n) -> p k n", k=16),
                     w_out[:, n * 512:(n + 1) * 512].rearrange("(k p) n -> p k n", p=128), writes=[wo[n]])
        self.dma("sync", self.BC[0][:], lng.partition_broadcast(128), writes=[self.BC[0]])
        self.dma("sync", self.BC[1][:], lnb.partition_broadcast(128), writes=[self.BC[1]])
        for t in range(T // 128):
            ob = self.BIGB[:, (t % 2) * 2048:(t % 2 + 1) * 2048]
            obk = ("ob", t % 2)
            self.dma("sync", ob, sc["o"][t * 128:(t + 1) * 128, :], reads=[("o", t)], writes=[obk])
            xt = self.f32view(self.BIGA, (t % 2) * 2048, 2048)
            xk = ("xt", t % 2)
            self.dma("sync", xt, x_ap[t * 128:(t + 1) * 128, :], writes=[xk])
            oT = self.BIGB[:, 4096 + (t % 2) * 2048:4096 + (t % 2 + 1) * 2048].rearrange("p (k n) -> p k n", k=16)
            oTk = ("oT", t % 2)
            for g in range(4):
                ps = self.PS[4 + g % 2]
                psb = ps.h.bitcast(BF16)
                for j in range(4):
                    k = g * 4 + j
                    P.op("tensor", lambda e, ob=ob, k=k, j=j, psb=psb: e.transpose(
                        out=psb[:, j * 128:(j + 1) * 128], in_=ob[:, k * 128:(k + 1) * 128], identity=self.identb[:]),
                        reads=[obk, self.identb], writes=[ps])
                P.op("scalar" if g % 2 else "vector",
                     (lambda e, oT=oT, psb=psb, g=g: e.activation(out=oT[:, g * 4:(g + 1) * 4, :], in_=psb[:, 0:512].rearrange("p (a b) -> p a b", a=4), func=AF.Copy))
                     if g % 2 else
                     (lambda e, oT=oT, psb=psb, g=g: e.tensor_copy(out=oT[:, g * 4:(g + 1) * 4, :], in_=psb[:, 0:512].rearrange("p (a b) -> p a b", a=4))),
                     reads=[ps], writes=[oTk])
            y = self.f32view(self.BIGA, 4096 + (t % 2) * 2048, 2048)
            yk = ("y", t % 2)
            for n in range(4):
                ps = self.PS[n]
                for k in range(16):
                    P.op("tensor", lambda e, n=n, k=k, oT=oT, ps=ps: e.matmul(
                        out=ps[:], lhsT=oT[:, k, :], rhs=wo[n][:, k * 512:(k + 1) * 512], start=(k == 0), stop=(k == 15)),
                        reads=[oTk, wo[n]], writes=[ps])
                P.op("vector", lambda e, n=n, ps=ps, y=y: e.tensor_tensor(
                    out=y[:, n * 512:(n + 1) * 512], in0=ps[:], in1=self.BC[2][:, n * 512:(n + 1) * 512], op=ALU.mult),
                    reads=[ps, self.BC[2]], writes=[yk])
            P.op("vector", lambda e, xt=xt, y=y: e.scalar_tensor_tensor(out=xt, in0=xt, scalar=ALPHA, in1=y, op0=ALU.mult, op1=ALU.add),
                 reads=[xk, yk], writes=[xk])
            self.layer_norm(xt, xk, self.BC[0], self.BC[1], y, yk)
            self.dma("sync", x1_ap[t * 128:(t + 1) * 128, :], xt, reads=[xk], writes=[("x1", t)])

    def moe(self, l, W, x1_ap, out_ap, outkey):
        P = self.P
        T = self.T
        TPASS = min(1024, T)
        npass = T // TPASS
        self.dma("sync", self.wr[:, :, 0:4], W["w_group"].rearrange("(k p) g -> p k g", p=128), writes=[self.wr])
        for g in range(4):
            self.dma("sync", self.wr[:, :, 4 + 8 * g:12 + 8 * g], W["w_router"][g].rearrange("(k p) e -> p k e", p=128), writes=[self.wr])
        self.dma("sync", self.brt[:, 0:4], W["b_group"].partition_broadcast(128), writes=[self.brt])
        self.dma("sync", self.brt[:, 4:36], W["b_router"].partition_broadcast(128), writes=[self.brt])
        if npass > 1:
            if not hasattr(self, "bcsave"):
                self.bcsave = self.dscr("s_bcsave", [2, 128, 2048], F32)
            self.dma("sync", self.bcsave[0], self.BC[0][:], reads=[self.BC[0]], writes=["bcsave0"])
            self.dma("sync", self.bcsave[1], self.BC[1][:], reads=[self.BC[1]], writes=["bcsave1"])
        ntile = TPASS // 128
        hT2 = self.BIGB[:, 0:16 * TPASS].rearrange("p (k t) -> p k t", k=16)
        accv = self.BIGA.h.bitcast(F32)
        hidbufs = [self.BC[0].h.bitcast(BF16)[:, i * 2048:(i + 1) * 2048].rearrange("p (f t) -> p f t", f=4) for i in range(2)]
        sgbufs = [self.BC[1][:, i * 512:(i + 1) * 512] for i in range(2)]
        for ps_i in range(npass):
            P.fence()
            tok0 = ps_i * TPASS
            if ps_i > 0:
                self.dma("sync", self.BC[0][:], self.bcsave[0], writes=[self.BC[0]])
                self.dma("sync", self.BC[1][:], self.bcsave[1], writes=[self.BC[1]])
            self.make_hT(x1_ap[tok0:tok0 + TPASS, :], TPASS, hT2, "hT2", router=dict())
            P.fence()
            hi = 0
            for ex in range(32):
                g, e = ex // 8, ex % 8
                wg = self.W[0 + ex % 2]
                wu = self.W[2 + ex % 2]
                wd = self.W[4]
                wgv = wg[:].rearrange("p (k n) -> p k n", k=16)
                wuv = wu[:].rearrange("p (k n) -> p k n", k=16)
                wdv = wd[:].rearrange("p (k n) -> p k n", k=4)
                self.dma("gpsimd", wgv, W["w_gate"][g, e].rearrange("(k p) n -> p k n", p=128), writes=[wg])
                self.dma("gpsimd", wuv, W["w_up"][g, e].rearrange("(k p) n -> p k n", p=128), writes=[wu])
                self.dma("gpsimd", wdv, W["w_down"][g, e].rearrange("(k p) n -> p k n", p=128), writes=[wd])
                for tb in range(TPASS // 512):
                    hb = hi % 2
                    hi += 1
                    hidb = hidbufs[hb]
                    for fc in range(4):
                        psg = self.PS[fc % 2]
                        psu = self.PS[2 + fc % 2]
                        for k in range(16):
                            P.op("tensor", lambda e_, k=k, fc=fc, tb=tb, psg=psg, wgv=wgv: e_.matmul(
                                out=psg[:], lhsT=wgv[:, k, fc * 128:(fc + 1) * 128], rhs=hT2[:, k, tb * 512:(tb + 1) * 512],
                                start=(k == 0), stop=(k == 15)), reads=[wg] + [("hT2", tb * 4 + i) for i in range(4)], writes=[psg])
                        for k in range(16):
                            P.op("tensor", lambda e_, k=k, fc=fc, tb=tb, psu=psu, wuv=wuv: e_.matmul(
                                out=psu[:], lhsT=wuv[:, k, fc * 128:(fc + 1) * 128], rhs=hT2[:, k, tb * 512:(tb + 1) * 512],
                                start=(k == 0), stop=(k == 15)), reads=[wu] + [("hT2", tb * 4 + i) for i in range(4)], writes=[psu])
                        sg = sgbufs[fc % 2]
                        sgk = ("sg", fc % 2)
                        P.op("scalar", lambda e_, sg=sg, psg=psg: e_.activation(out=sg, in_=psg[:], func=AF.Silu), reads=[psg], writes=[sgk])
                        P.op("vector", lambda e_, sg=sg, psu=psu, hidb=hidb, fc=fc: e_.tensor_tensor(
                            out=hidb[:, fc, :], in0=psu[:], in1=sg, op=ALU.mult), reads=[psu, sgk], writes=[("hid", hb, fc)])
                    for tt in range(4):
                        t = tb * 4 + tt
                        for dmb in range(4):
                            psd = self.PS[4 + (tt * 4 + dmb) % 4]
                            for fc in range(4):
                                P.op("tensor", lambda e_, fc=fc, tt=tt, dmb=dmb, psd=psd, hidb=hidb, wdv=wdv: e_.matmul(
                                    out=psd[:], lhsT=hidb[:, fc, tt * 128:(tt + 1) * 128], rhs=wdv[:, fc, dmb * 512:(dmb + 1) * 512],
                                    start=(fc == 0), stop=(fc == 3)), reads=[("hid", hb, fc), wd], writes=[psd])
                            av = accv[:, t * 2048 + dmb * 512:t * 2048 + (dmb + 1) * 512]
                            if ex == 0:
                                P.op("vector", lambda e_, psd=psd, av=av, t=t, ex=ex: e_.tensor_scalar(
                                    out=av, in0=psd[:], scalar1=self.w32[:, t, ex:ex + 1], scalar2=None, op0=ALU.mult),
                                    reads=[psd, ("w32", t)], writes=[("acc", t)])
                            else:
                                P.op("vector", lambda e_, psd=psd, av=av, t=t, ex=ex: e_.scalar_tensor_tensor(
                                    out=av, in0=psd[:], scalar=self.w32[:, t, ex:ex + 1], in1=av, op0=ALU.mult, op1=ALU.add),
                                    reads=[psd, ("w32", t), ("acc", t)], writes=[("acc", t)])
            P.fence()
            self.dma("sync", self.f32view(self.W[0], 0, 2048), W["ln_g"].partition_broadcast(128), writes=[self.W[0]])
            self.dma("sync", self.f32view(self.W[1], 0, 2048), W["ln_b"].partition_broadcast(128), writes=[self.W[1]])
            gt = _View(self.f32view(self.W[0], 0, 2048), self.W[0].key)
            bt = _View(self.f32view(self.W[1], 0, 2048), self.W[1].key)
            for t in range(ntile):
                z = accv[:, t * 2048:(t + 1) * 2048]
                xt = self.f32view(self.W[3], (t % 2) * 2048, 2048)
                xk = (self.W[3].key, "xt", t % 2)
                self.dma("sync", xt, x1_ap[tok0 + t * 128:tok0 + (t + 1) * 128, :], writes=[xk])
                P.op("vector", lambda e_, z=z: e_.tensor_tensor(out=z, in0=z, in1=self.BC[2][:], op=ALU.mult),
                     reads=[("acc", t), self.BC[2]], writes=[("acc", t)])
                P.op("vector", lambda e_, z=z, xt=xt: e_.scalar_tensor_tensor(out=z, in0=xt, scalar=ALPHA, in1=z, op0=ALU.mult, op1=ALU.add),
                     reads=[("acc", t), xk], writes=[("acc", t)])
                scr = self.f32view(self.W[2], (t % 2) * 2048, 2048)
                self.layer_norm(z, ("acc", t), gt, bt, scr, (self.W[2].key, "scr", t % 2))
                self.dma("sync", out_ap[tok0 + t * 128:tok0 + (t + 1) * 128, :], z, reads=[("acc", t)], writes=[(outkey, tok0 // 128 + t)])

    def rowhid(self, tb):
        o = 8192 + (tb % 2) * 2048
        return self.BIGB[:, o:o + 2048].rearrange("p (f t) -> p f t", f=4)

    def sgbuf(self, i):
        return self.BIGB.h.bitcast(F32)[:, 6144 + i * 512:6144 + (i + 1) * 512]


Buf.register = None


def _is_buf(x):
    return isinstance(x, (Buf, _View))


def _k(x):
    return x.key if isinstance(x, (Buf, _View)) else x


def build_program(T, TP, layers, ncores=8):
    B = Builder(T, TP, layers)
    nc, P = B.nc, B.P
    TC = T + TP
    B.load_consts()
    xo = B.din("xo", [T, D])
    xp = B.din("xp", [TP, D])
    cosT = B.din("c_cos", [128, TC])
    sinT = B.din("c_sin", [128, TC])
    out = nc.dram_tensor("out", [T, D], F32, kind="ExternalOutput").ap()
    sc = dict(
        qT=B.dscr("s_qT", [16, 128, T], BF16), kT=B.dscr("s_kT", [16, 128, TC], BF16),
        o=B.dscr("s_o", [T, D], BF16), cos=cosT, sin=sinT)
    vA = B.dscr("s_vA", [8, TC // 128, 128, 257], BF16)
    vB = B.dscr("s_vB", [16, TC // 128, 128, 129], BF16)
    x1 = B.dscr("s_x1", [T, D], F32)
    CH = 256
    nch = T // CH
    if len(layers) > 1:
        x2 = B.dscr("s_x2", [T, D], F32)
        gath = B.dscr("s_gath", [nch, 2 * CH, D], F32)
    cur_o, cur_p = xo, xp
    for li, l in enumerate(layers):
        kind = "A" if l % 2 == 0 else "B"
        last = (li == len(layers) - 1)
        sc["v"] = vA if kind == "A" else vB
        wmod = B.din("mix_mod_w%d" % l, [D, 6144])
        bmod = B.din("mix_mod_b%d" % l, [1, 6144])
        lng = B.din("mix_ln_g%d" % l, [1, D])
        lnb = B.din("mix_ln_b%d" % l, [1, D])
        lam_init = lamv = subw = fbias = None
        if kind == "A":
            w_in = B.din("a_w_in", [D, 6144])
            w_out = B.din("a_w_out", [D, D])
            lamv = [B.din("a_lam_q1", [1, 128]), B.din("a_lam_k1", [1, 128]),
                    B.din("a_lam_q2", [1, 128]), B.din("a_lam_k2", [1, 128])]
            subw = B.din("a_subln_w", [1, 256])
            lam_init = 0.8 - 0.6 * math.exp(-0.3 * l)
        else:
            w_in = B.din("b_w_in", [D, 6160])
            w_out = B.din("b_w_out", [D, D])
            fbias = B.din("b_forget_bias", [1, 16])
        Wm = dict(
            w_group=B.din("moe_w_group%d" % l, [D, 4]), b_group=B.din("moe_b_group%d" % l, [1, 4]),
            w_router=B.din("moe_w_router%d" % l, [4, D, 8]), b_router=B.din("moe_b_router%d" % l, [1, 32]),
            w_gate=B.din("moe_w_gate%d" % l, [4, 8, D, 512]), w_up=B.din("moe_w_up%d" % l, [4, 8, D, 512]),
            w_down=B.din("moe_w_down%d" % l, [4, 8, 512, D]),
            ln_g=B.din("ffn_ln_g%d" % l, [1, D]), ln_b=B.din("ffn_ln_b%d" % l, [1, D]))
        fwmod = B.din("ffn_mod_w%d" % l, [D, 6144])
        fbmod = B.din("ffn_mod_b%d" % l, [1, 6144])

        if li == 0:
            P.fence()
        B.modulation(wmod, bmod)
        B.mixer_params(kind, lam_init, lamv, subw, fbias, w_in)
        hT = B.BIGA[:, 0:16 * max(T, TP)].rearrange("p (k t) -> p k t", k=16)
        for (x_ap, ntok, tok0, with_q) in ((cur_p, TP, 0, False), (cur_o, T, TP, True)):
            P.fence()
            B.make_hT(x_ap, ntok, hT[:, :, 0:ntok], "hT")
            P.fence()
            B.qkv(l, kind, w_in, hT[:, :, 0:ntok], "hT", ntok, tok0, with_q, sc)
        P.fence()
        B.attention(l, kind, sc)
        P.fence()
        B.out_proj(l, w_out, cur_o, lng, lnb, sc, x1)
        P.fence()
        B.modulation(fwmod, fbmod)
        B.moe(l, Wm, x1, out if last else x2, "out" if last else "x2")
        if not last:
            P.fence()
            groups = [[2 * i, 2 * i + 1] for i in range(ncores // 2)]
            for ch in range(nch):
                if os.environ.get("K_NOCC"):
                    B.dma("sync", gath[ch, 0:CH, :], x2[ch * CH:(ch + 1) * CH, :], writes=[("gath", ch)])
                    continue
                P.op("gpsimd", lambda e, ch=ch: e.collective_compute(
                    "AllGather", ALU.bypass, replica_groups=groups,
                    ins=[x2[ch * CH:(ch + 1) * CH, :]], outs=[gath[ch]]),
                    writes=[("gath", ch)], dma=True, cc=True)
            cur_o = x2
            cur_p = (lambda t: gath[t // 2, (t % 2) * 128:(t % 2 + 1) * 128, :])
    P.fence()
    P.op("sync", None, reads=[])
    P.emit()
    return nc, B


def _consts(T, TP, p):
    TC = T + TP
    ident = np.eye(128, dtype=np.float32)
    tri = np.triu(np.ones((128, 128), np.float32))
    R = np.zeros((128, 128), np.float32)
    for i in range(64):
        R[i, i + 64] = -1.0
        R[i + 64, i] = 1.0
    rotT = np.ascontiguousarray(R.T)
    pos = np.concatenate([np.arange(TP), p * T + np.arange(T)]).astype(np.float32)
    inv = np.power(np.float32(10000.0), -np.arange(0, 128, 2, dtype=np.float32) / 128).astype(np.float32)
    ang = pos[None, :] * np.concatenate([inv, inv])[:, None]
    return dict(c_ident=ident, c_tri=tri, c_rotT=rotT,
                c_pbias=np.full((128, 1), 0.0 if p == 1 else NEG, np.float32),
                c_cos=np.cos(ang).astype(np.float32), c_sin=np.sin(ang).astype(np.float32))


_PROG_CACHE = {}
_RUN_KW = {}
_LAST = {}


def run_layers(layers, x_in, inp, T, TP, ncores):
    key = (tuple(layers), T, TP, ncores)
    if key not in _PROG_CACHE:
        _PROG_CACHE[key] = build_program(T, TP, list(layers), ncores)
    nc, B = _PROG_CACHE[key]
    f = np.float32
    shared = {}
    for l in layers:
        j = l // 2
        shared["mix_mod_w%d" % l] = inp["mix_mod_w"][l]
        shared["mix_mod_b%d" % l] = inp["mix_mod_b"][l].reshape(1, -1)
        shared["mix_ln_g%d" % l] = inp["mix_ln_g"][l].reshape(1, -1)
        shared["mix_ln_b%d" % l] = inp["mix_ln_b"][l].reshape(1, -1)
        if l % 2 == 0:
            shared["a_w_in"] = inp["a_w_in"][j]
            shared["a_w_out"] = inp["a_w_out"][j]
            shared["a_lam_q1"] = inp["a_lam_q1"][j].reshape(1, -1)
            shared["a_lam_k1"] = inp["a_lam_k1"][j].reshape(1, -1)
            shared["a_lam_q2"] = inp["a_lam_q2"][j].reshape(1, -1)
            shared["a_lam_k2"] = inp["a_lam_k2"][j].reshape(1, -1)
            shared["a_subln_w"] = inp["a_subln_w"][j].reshape(1, -1)
        else:
            shared["b_w_in"] = inp["b_w_in"][j]
            shared["b_w_out"] = inp["b_w_out"][j]
            shared["b_forget_bias"] = inp["b_forget_bias"][j].reshape(1, -1)
        shared["ffn_mod_w%d" % l] = inp["ffn_mod_w"][l]
        shared["ffn_mod_b%d" % l] = inp["ffn_mod_b"][l].reshape(1, -1)
        shared["ffn_ln_g%d" % l] = inp["ffn_ln_g"][l].reshape(1, -1)
        shared["ffn_ln_b%d" % l] = inp["ffn_ln_b"][l].reshape(1, -1)
        shared["moe_w_group%d" % l] = inp["moe_w_group"][l]
        shared["moe_b_group%d" % l] = inp["moe_b_group"][l].reshape(1, -1)
        shared["moe_w_router%d" % l] = inp["moe_w_router"][l]
        shared["moe_b_router%d" % l] = inp["moe_b_router"][l].reshape(1, -1)
        shared["moe_w_gate%d" % l] = inp["moe_w_gate"][l]
        shared["moe_w_up%d" % l] = inp["moe_w_up"][l]
        shared["moe_w_down%d" % l] = inp["moe_w_down"][l]
    shared = {k: np.ascontiguousarray(np.asarray(v, dtype=f)) for k, v in shared.items() if k in B.inputs}
    in_maps = []
    for c in range(ncores):
        b, p = c // 2, c % 2
        m = _consts(T, TP, p)
        m["xo"] = x_in[b, p * T:(p + 1) * T]
        m["xp"] = x_in[b, 0:TP]
        m["cvec_in"] = inp["c"][b].reshape(16, 128).T
        m = {k: np.ascontiguousarray(np.asarray(v, dtype=f)) for k, v in m.items() if k in B.inputs}
        m.update(shared)
        in_maps.append(m)
    res = run_bass_kernel_spmd(nc, in_maps, core_ids=list(range(ncores)), **_RUN_KW)
    _LAST['res'] = res
    out = np.empty_like(x_in)
    for c in range(ncores):
        b, p = c // 2, c % 2
        out[b, p * T:(p + 1) * T] = res.results[c]["out"]
    return out


def run_layer(l, x_in, inp, T, TP, ncores, stop=99):
    return run_layers([l], x_in, inp, T, TP, ncores)


def kernel(**inputs):
    inp = {k: np.asarray(v) for k, v in inputs.items()}
    x = np.asarray(inp["x"], dtype=np.float32)
    Bn, S, _ = x.shape
    T = S // 2
    ncores = Bn * 2
    return run_layers(list(range(DEPTH)), x, inp, T, T, ncores)
```
